# Optimizing a Trainium2 kernel written in Bass

```python
import math
import jax, jax.numpy as jnp
from jax import lax
import numpy as np

D_MODEL = 1024
BATCH = 4
SEQ = 4096
DEPTH = 2

GRID_W = 64
CTX_LEN = 256
GLA_HEADS = 4
GLA_DK = 32
GLA_DV = 64
GLA_GATE_RANK = 16
GLA_GATE_NORM = 16.0
GLA_CHUNK = 64
GQA_HEADS = 8
GQA_KV_HEADS = 2
GQA_GROUP = GQA_HEADS // GQA_KV_HEADS
GQA_HD = 64
DIFF_HEADS = 4
DIFF_QK = 32
DIFF_V = 64

ROPE_THETA = 10000.0
Q_BLOCK = 128
NORM_EPS = 1e-6
MIX_WIDTH = GLA_HEADS * GLA_DV + GQA_HEADS * GQA_HD + DIFF_HEADS * DIFF_V
IN_WIDTHS = (GLA_HEADS * GLA_DK, GLA_HEADS * GLA_DK, GLA_HEADS * GLA_DV, GLA_HEADS * GLA_DV,
             GLA_GATE_RANK, GLA_GATE_RANK,
             GQA_HEADS * GQA_HD, GQA_KV_HEADS * GQA_HD, GQA_KV_HEADS * GQA_HD,
             DIFF_HEADS * 2 * DIFF_QK, DIFF_HEADS * 2 * DIFF_QK, DIFF_HEADS * DIFF_V)
IN_WIDTH = sum(IN_WIDTHS)
D_FF = 2816
N_EXPERTS = 8
TOP_K = 2
D_FF_EXPERT = 3584
N_DENSE = (DEPTH + 1) // 2
N_MOE = DEPTH // 2

kernel_name = "hybrid_gla_gqa_diffattn_moe_dit_block"


def rms_norm(x, w):
    xf = x.astype(jnp.float32)
    y = xf * lax.rsqrt(jnp.mean(xf * xf, axis=-1, keepdims=True) + NORM_EPS)
    return (y * w.astype(jnp.float32)).astype(x.dtype)


def modulate(h, shift, scale):
    return h * (1.0 + scale) + shift


def rope_1d(x, pos):
    L, d = x.shape[1], x.shape[-1]
    half = d // 2
    freqs = ROPE_THETA ** (-jnp.arange(half, dtype=jnp.float32) / half)
    ang = pos[:, None] * freqs[None, :]
    bshape = (L,) + (1,) * (x.ndim - 3) + (half,)
    cos = jnp.cos(ang).reshape(bshape)
    sin = jnp.sin(ang).reshape(bshape)
    x1 = x[..., :half].astype(jnp.float32)
    x2 = x[..., half:].astype(jnp.float32)
    return jnp.concatenate([x1 * cos - x2 * sin, x1 * sin + x2 * cos], axis=-1).astype(x.dtype)


def axial_rope(x, rows, cols):
    d = x.shape[-1]
    return jnp.concatenate([rope_1d(x[..., : d // 2], rows), rope_1d(x[..., d // 2:], cols)], axis=-1)


def gla_chunk_scan(q, k, v, log_a, s0):
    B, L, H, dk = q.shape
    dv = v.shape[-1]
    n = L // GLA_CHUNK

    def chunked(t):
        return t.reshape(B, n, GLA_CHUNK, H, t.shape[-1]).transpose(1, 0, 3, 2, 4).astype(jnp.float32)

    qc, kc, vc, gc = chunked(q * GLA_DK ** -0.5), chunked(k), chunked(v), chunked(log_a)
    b = jnp.cumsum(gc, axis=-2)
    b_last = b[..., -1:, :]
    q_dec = qc * jnp.exp(b)
    causal = jnp.tril(jnp.ones((GLA_CHUNK, GLA_CHUNK), dtype=bool))
    A = jnp.einsum("nbhcd,nbhsd->nbhcs", q_dec, kc * jnp.exp(-b))
    A = jnp.where(causal, A, 0.0)
    o_intra = jnp.einsum("nbhcs,nbhse->nbhce", A, vc)
    kv = jnp.einsum("nbhcd,nbhce->nbhde", kc * jnp.exp(b_last - b), vc)
    decay = jnp.exp(b[..., -1, :])

    def step(S, inp):
        dec, kv_n = inp
        return dec[..., None] * S + kv_n, S

    s_final, s_in = lax.scan(step, s0.astype(jnp.float32), (decay, kv))
    o_inter = jnp.einsum("nbhcd,nbhde->nbhce", q_dec, s_in)
    o = (o_intra + o_inter).transpose(1, 0, 3, 2, 4).reshape(B, L, H, dv)
    return o.astype(v.dtype), s_final


def gla_bidirectional(q, k, v, la_f, la_b, s0_f, s0_b):
    o_f, s_f = gla_chunk_scan(q, k, v, la_f, s0_f)
    flip = lambda t: jnp.flip(t, axis=1)
    o_b, s_b = gla_chunk_scan(flip(q), flip(k), flip(v), flip(la_b), s0_b)
    return o_f + flip(o_b), s_f, s_b


def gqa_attend(q, k, v):
    s = jnp.einsum("bqkgd,bskd->bkgqs", q, k).astype(jnp.float32) * q.shape[-1] ** -0.5
    p = jax.nn.softmax(s, axis=-1).astype(v.dtype)
    return jnp.einsum("bkgqs,bskd->bqkgd", p, v)


def diff_attend(q, k, v, lam):
    s = jnp.einsum("bqhmd,bshmd->bhmqs", q, k).astype(jnp.float32) * q.shape[-1] ** -0.5
    p = jax.nn.softmax(s, axis=-1)
    p = p[:, :, 0] - lam * p[:, :, 1]
    return jnp.einsum("bhqs,bshd->bqhd", p.astype(v.dtype), v)


def sweep_query_blocks(attend, q):
    B, L = q.shape[:2]
    nb = L // Q_BLOCK
    qb = jnp.moveaxis(q.reshape((B, nb, Q_BLOCK) + q.shape[2:]), 1, 0)
    ob = lax.map(attend, qb)
    return jnp.moveaxis(ob, 0, 1).reshape((B, L) + ob.shape[3:])


def swiglu(h, w_gate, w_up, w_down):
    return (jax.nn.silu(h @ w_gate) * (h @ w_up)) @ w_down


def moe_swiglu(h, w_router, w_gate, w_up, w_down):
    logits = (h @ w_router).astype(jnp.float32)
    top_v, top_i = lax.top_k(logits, TOP_K)
    top_w = jax.nn.softmax(top_v, axis=-1)
    combine = jnp.sum(jax.nn.one_hot(top_i, N_EXPERTS, dtype=jnp.float32) * top_w[..., None], axis=-2)
    combine = combine.astype(h.dtype)
    out = jnp.zeros_like(h)
    for e in range(N_EXPERTS):
        out = out + combine[..., e:e + 1] * swiglu(h, w_gate[e], w_up[e], w_down[e])
    return out


def hybrid_mixer(h, hc, rows, cols, w_in, w_out, gla_a2_f, gla_ab_f, gla_a2_b, gla_ab_b, gla_norm,
                 q_norm, k_norm, lq1, lk1, lq2, lk2, diff_norm, lambda_init, need_ctx_out):
    splits = [int(s) for s in np.cumsum(IN_WIDTHS)[:-1]]
    P = jnp.split(h @ w_in, splits, axis=-1)
    Pc = jnp.split(hc @ w_in, splits, axis=-1)
    heads = lambda t, *shape: t.reshape(t.shape[:2] + shape)

    def gla_in(p):
        q = heads(p[0], GLA_HEADS, GLA_DK)
        k = heads(p[1], GLA_HEADS, GLA_DK)
        v = heads(p[2], GLA_HEADS, GLA_DV)
        la_f = heads(jax.nn.log_sigmoid(p[4] @ gla_a2_f + gla_ab_f) / GLA_GATE_NORM, GLA_HEADS, GLA_DK)
        la_b = heads(jax.nn.log_sigmoid(p[5] @ gla_a2_b + gla_ab_b) / GLA_GATE_NORM, GLA_HEADS, GLA_DK)
        return q, k, v, la_f, la_b

    zero_state = jnp.zeros((hc.shape[0], GLA_HEADS, GLA_DK, GLA_DV), jnp.float32)
    gla_c, s_f, s_b = gla_bidirectional(*gla_in(Pc), zero_state, zero_state)
    gla_l, _, _ = gla_bidirectional(*gla_in(P), s_f, s_b)
    gla_out = lambda o, r: rms_norm(o, gla_norm).reshape(r.shape) * jax.nn.silu(r)

    def gqa_in(p, rope):
        q = rms_norm(heads(p[6], GQA_KV_HEADS, GQA_GROUP, GQA_HD), q_norm)
        k = rms_norm(heads(p[7], GQA_KV_HEADS, GQA_HD), k_norm)
        v = heads(p[8], GQA_KV_HEADS, GQA_HD)
        if rope:
            q, k = axial_rope(q, rows, cols), axial_rope(k, rows, cols)
        return q, k, v

    gq, gk, gv = gqa_in(P, True)
    gqc, gkc, gvc = gqa_in(Pc, False)
    gk_all = jnp.concatenate([gk, gkc], axis=1)
    gv_all = jnp.concatenate([gv, gvc], axis=1)
    gqa_l = sweep_query_blocks(lambda qb: gqa_attend(qb, gk_all, gv_all), gq)
    gqa_l = gqa_l.reshape(gqa_l.shape[:2] + (-1,))

    lam = (jnp.exp(jnp.sum((lq1 * lk1).astype(jnp.float32)))
           - jnp.exp(jnp.sum((lq2 * lk2).astype(jnp.float32))) + lambda_init)

    def diff_in(p, rope):
        q = heads(p[9], DIFF_HEADS, 2, DIFF_QK)
        k = heads(p[10], DIFF_HEADS, 2, DIFF_QK)
        v = heads(p[11], DIFF_HEADS, DIFF_V)
        if rope:
            q, k = axial_rope(q, rows, cols), axial_rope(k, rows, cols)
        return q, k, v

    dq, dk, dv = diff_in(P, True)
    dqc, dkc, dvc = diff_in(Pc, False)
    dk_all = jnp.concatenate([dk, dkc], axis=1)
    dv_all = jnp.concatenate([dv, dvc], axis=1)
    diff_l = sweep_query_blocks(lambda qb: diff_attend(qb, dk_all, dv_all, lam), dq)
    diff_out = lambda o: (rms_norm(o, diff_norm) * (1.0 - lambda_init)).reshape(o.shape[:2] + (-1,))

    y = jnp.concatenate([gla_out(gla_l, P[3]), gqa_l, diff_out(diff_l)], axis=-1) @ w_out
    yc = None
    if need_ctx_out:
        gqa_c = gqa_attend(gqc, gkc, gvc)
        gqa_c = gqa_c.reshape(gqa_c.shape[:2] + (-1,))
        diff_c = diff_attend(dqc, dkc, dvc, lam)
        yc = jnp.concatenate([gla_out(gla_c, Pc[3]), gqa_c, diff_out(diff_c)], axis=-1) @ w_out
    return y, yc


def setup_inputs(seed: int = 0) -> dict:
    key = jax.random.key(seed)
    ks = iter(jax.random.split(key, 40))
    nrm = lambda shape, scale: jax.random.normal(next(ks), shape, jnp.float32) * scale
    gain = lambda shape: 1.0 + nrm(shape, 0.02)
    D = D_MODEL
    return {
        "x": nrm((BATCH, SEQ, D), 1.0),
        "c": nrm((BATCH, D), 1.0),
        "ctx": nrm((BATCH, CTX_LEN, D), 1.0),
        "c_ctx": nrm((D,), 1.0),
        "w_mod": nrm((DEPTH, D, 6 * D), 0.5 * D ** -0.5),
        "b_mod": nrm((DEPTH, 6 * D), 0.02),
        "norm_mix": gain((DEPTH, D)),
        "norm_ffn": gain((DEPTH, D)),
        "w_in": nrm((DEPTH, D, IN_WIDTH), D ** -0.5),
        "w_out": nrm((DEPTH, MIX_WIDTH, D), MIX_WIDTH ** -0.5),
        "gla_a2_f": nrm((DEPTH, GLA_GATE_RANK, GLA_HEADS * GLA_DK), GLA_GATE_RANK ** -0.5),
        "gla_ab_f": nrm((DEPTH, GLA_HEADS * GLA_DK), 0.1),
        "gla_a2_b": nrm((DEPTH, GLA_GATE_RANK, GLA_HEADS * GLA_DK), GLA_GATE_RANK ** -0.5),
        "gla_ab_b": nrm((DEPTH, GLA_HEADS * GLA_DK), 0.1),
        "gla_norm": gain((DEPTH, GLA_DV)),
        "gqa_q_norm": gain((DEPTH, GQA_HD)),
        "gqa_k_norm": gain((DEPTH, GQA_HD)),
        "diff_lam_q1": nrm((DEPTH, DIFF_QK), 0.1),
        "diff_lam_k1": nrm((DEPTH, DIFF_QK), 0.1),
        "diff_lam_q2": nrm((DEPTH, DIFF_QK), 0.1),
        "diff_lam_k2": nrm((DEPTH, DIFF_QK), 0.1),
        "diff_norm": gain((DEPTH, DIFF_V)),
        "ffn_gate": nrm((N_DENSE, D, D_FF), D ** -0.5),
        "ffn_up": nrm((N_DENSE, D, D_FF), D ** -0.5),
        "ffn_down": nrm((N_DENSE, D_FF, D), D_FF ** -0.5),
        "moe_router": nrm((N_MOE, D, N_EXPERTS), D ** -0.5),
        "moe_gate": nrm((N_MOE, N_EXPERTS, D, D_FF_EXPERT), D ** -0.5),
        "moe_up": nrm((N_MOE, N_EXPERTS, D, D_FF_EXPERT), D ** -0.5),
        "moe_down": nrm((N_MOE, N_EXPERTS, D_FF_EXPERT, D), D_FF_EXPERT ** -0.5),
        "norm_f": gain((D,)),
    }


def reference(x, c, ctx, c_ctx, w_mod, b_mod, norm_mix, norm_ffn, w_in, w_out,
              gla_a2_f, gla_ab_f, gla_a2_b, gla_ab_b, gla_norm, gqa_q_norm, gqa_k_norm,
              diff_lam_q1, diff_lam_k1, diff_lam_q2, diff_lam_k2, diff_norm,
              ffn_gate, ffn_up, ffn_down, moe_router, moe_gate, moe_up, moe_down, norm_f):
    L = x.shape[1]
    ROWS = L // GRID_W
    rows = jnp.repeat(jnp.arange(ROWS, dtype=jnp.float32), GRID_W)
    cols = jnp.tile(jnp.arange(GRID_W, dtype=jnp.float32), ROWS)
    xc = ctx
    for l in range(DEPTH):
        need_ctx = l < DEPTH - 1
        mod = jax.nn.silu(c) @ w_mod[l] + b_mod[l]
        mod_c = jax.nn.silu(c_ctx) @ w_mod[l] + b_mod[l]
        sh_a, sc_a, g_a, sh_f, sc_f, g_f = jnp.split(mod[:, None, :], 6, axis=-1)
        csh_a, csc_a, cg_a, csh_f, csc_f, cg_f = jnp.split(mod_c, 6, axis=-1)

        h = modulate(rms_norm(x, norm_mix[l]), sh_a, sc_a)
        hc = modulate(rms_norm(xc, norm_mix[l]), csh_a, csc_a)
        y, yc = hybrid_mixer(h, hc, rows, cols, w_in[l], w_out[l],
                             gla_a2_f[l], gla_ab_f[l], gla_a2_b[l], gla_ab_b[l], gla_norm[l],
                             gqa_q_norm[l], gqa_k_norm[l],
                             diff_lam_q1[l], diff_lam_k1[l], diff_lam_q2[l], diff_lam_k2[l], diff_norm[l],
                             0.8 - 0.6 * math.exp(-0.3 * l), need_ctx)
        x = x + g_a * y

        i = l // 2
        if l % 2 == 0:
            ffn = lambda t: swiglu(t, ffn_gate[i], ffn_up[i], ffn_down[i])
        else:
            ffn = lambda t: moe_swiglu(t, moe_router[i], moe_gate[i], moe_up[i], moe_down[i])
        x = x + g_f * ffn(modulate(rms_norm(x, norm_ffn[l]), sh_f, sc_f))
        if need_ctx:
            xc = xc + cg_a * yc
            xc = xc + cg_f * ffn(modulate(rms_norm(xc, norm_ffn[l]), csh_f, csc_f))
    return rms_norm(x, norm_f)
```

```python
import math
from contextlib import ExitStack
import numpy as np
import concourse.bass as bass
import concourse.mybir as mybir
from concourse.bass_utils import run_bass_kernel_spmd

F32 = mybir.dt.float32
BF16 = mybir.dt.bfloat16
AF = mybir.ActivationFunctionType
ALU = mybir.AluOpType
AX = mybir.AxisListType

D = 1024
NSLOT = 8


class Prog:
    ENGS = ["pe", "act", "dve", "pool", "sp"]

    def __init__(self, nc, es):
        self.nc = nc
        self.es = es
        self.ncoll = 0
        self.stream = {e: [] for e in self.ENGS}
        self.cnt = {e: 0 for e in self.ENGS}
        self.known = {e: {} for e in self.ENGS}
        self.lastw = {}
        self.rds = {}
        self.dmacnt = {e: 0 for e in self.ENGS}
        self.sems = {}
        self.semmax = {}
        for e in ["pe", "act", "dve", "pool"]:
            self.sems[e] = es.enter_context(nc.semaphore("s_" + e))
        for q in ["sp", "act", "pool"]:
            for j in range(NSLOT):
                self.sems[(q, j)] = es.enter_context(nc.semaphore("d_%s_%d" % (q, j)))

    def _wait(self, eng, ev):
        if ev is None:
            return
        k, v = ev
        if k == eng and eng == "pe":
            return
        if self.known[eng].get(k, 0) >= v:
            return
        self.known[eng][k] = v
        self.stream[eng].append(("w", k, v))

    def _deps(self, eng, reads, writes):
        for r in reads:
            self._wait(eng, self.lastw.get(r))
        for w in writes:
            self._wait(eng, self.lastw.get(w))
            for k, v in self.rds.get(w, {}).items():
                self._wait(eng, (k, v))

    def _commit(self, ev, reads, writes):
        k, v = ev
        self.semmax[k] = max(self.semmax.get(k, 0), v)
        for r in reads:
            d = self.rds.setdefault(r, {})
            d[k] = max(d.get(k, 0), v)
        for w in writes:
            self.lastw[w] = ev
            self.rds[w] = {}

    def op(self, eng, fn, reads=(), writes=()):
        self._deps(eng, reads, writes)
        self.cnt[eng] += 1
        ev = (eng, self.cnt[eng])
        self.stream[eng].append(("c", fn))
        self._commit(ev, reads, writes)

    def dma(self, out, in_, reads=(), writes=(), q="sp", **kw):
        n = self.dmacnt[q]
        self.dmacnt[q] += 1
        slot = n % NSLOT
        val = 16 * (n // NSLOT + 1)
        if val > 16:
            self._wait(q, ((q, slot), val - 16))
        self._deps(q, reads, writes)
        self.stream[q].append(("d", out, in_, (q, slot), kw))
        self._commit(((q, slot), val), reads, writes)

    def coll(self, fn, reads=(), writes=()):
        key = ("cc", self.ncoll)
        self.ncoll += 1
        self.sems[key] = self.es.enter_context(self.nc.semaphore("cc_%d" % key[1]))
        self._deps("pool", reads, writes)
        self.stream["pool"].append(("x", fn, key))
        self._commit((key, 1), reads, writes)

    def barrier(self):
        for e in self.ENGS:
            for k, v in self.semmax.items():
                self._wait(e, (k, v))

    def mm(self, out, lhsT, rhs, start, stop, reads, writes, skip=False):
        if skip:
            self.op("pe", lambda e: e.matmul(out, lhsT=lhsT, rhs=rhs, start=start, stop=stop, skip_group_check=True),
                    reads, writes)
        else:
            self.op("pe", lambda e: e.matmul(out, lhsT=lhsT, rhs=rhs, start=start, stop=stop), reads, writes)

    def tr(self, out, in_, ident, reads, writes):
        self.op("pe", lambda e: e.transpose(out, in_, ident), reads, writes)

    def act(self, out, in_, func, reads, writes, bias=None, scale=None, accum_out=None):
        kw = {}
        if bias is not None:
            kw["bias"] = bias
        if scale is not None:
            kw["scale"] = scale
        if accum_out is not None:
            kw["accum_out"] = accum_out
        self.op("act", lambda e: e.activation(out, in_, func, **kw), reads, writes)

    def tt(self, eng, out, in0, in1, op, reads, writes):
        self.op(eng, lambda e: e.tensor_tensor(out, in0, in1, op), reads, writes)

    def ts(self, eng, out, in0, s1, s2, op0, op1, reads, writes, accum_out=None):
        if op1 is None:
            self.op(eng, lambda e: e.tensor_scalar(out, in0, s1, None, op0), reads, writes)
        elif accum_out is None:
            self.op(eng, lambda e: e.tensor_scalar(out, in0, s1, s2, op0, op1), reads, writes)
        else:
            self.op(eng, lambda e: e.tensor_scalar(out, in0, s1, s2, op0, op1, accum_out=accum_out), reads, writes)

    def stt(self, out, in0, scalar, in1, op0, op1, reads, writes):
        self.op("dve", lambda e: e.scalar_tensor_tensor(out, in0, scalar, in1, op0, op1), reads, writes)

    def cp(self, eng, out, in_, reads, writes):
        if eng == "act":
            self.op("act", lambda e: e.copy(out, in_), reads, writes)
        else:
            self.op(eng, lambda e: e.tensor_copy(out, in_), reads, writes)

    def memset(self, eng, ap, val, writes):
        self.op(eng, lambda e: e.memset(ap, val), (), writes)

    def emit(self):
        nc = self.nc
        self.barrier()
        sems = self.sems
        streams = self.stream

        def run(name, eng):
            for it in streams[name]:
                if it[0] == "w":
                    eng.wait_ge(sems[it[1]], it[2])
                elif it[0] == "c":
                    it[1](eng).then_inc(sems[name], 1)
                elif it[0] == "x":
                    it[1](eng).then_inc(sems[it[2]], 1)
                else:
                    eng.dma_start(out=it[1], in_=it[2], **it[4]).then_inc(sems[it[3]], 16)

        with nc.Block() as block:
            @block.tensor
            def _(e):
                run("pe", e)

            @block.scalar
            def _(e):
                run("act", e)

            @block.vector
            def _(e):
                run("dve", e)

            @block.gpsimd
            def _(e):
                run("pool", e)

            @block.sync
            def _(e):
                run("sp", e)


class Ctx:
    def __init__(self, nc, es, pfx=""):
        self.nc = nc
        self.es = es
        self.pfx = pfx

    def sb(self, name, shape, dt=F32):
        return self.es.enter_context(self.nc.sbuf_tensor(self.pfx + name, list(shape), dt))

    def ps(self, name, shape, dt=F32):
        return self.es.enter_context(self.nc.psum_tensor(name, list(shape), dt))

    def din(self, name, shape, dt=F32):
        return self.nc.dram_tensor(name, list(shape), dt, kind="ExternalInput").ap()

    def dout(self, name, shape, dt=F32):
        return self.nc.dram_tensor(name, list(shape), dt, kind="ExternalOutput").ap()


def emit_mod(P, C, banks, ccT_d, wmod_d, bmod_d, groups, rows_d, tag):
    nc = P.nc
    ng = len(groups)
    modB = C.sb(tag + "modB", [128, 2, ng, 1024])
    rowB = C.sb(tag + "rowB", [128, max(1, len(rows_d)), 1024])
    with ExitStack() as es2:
        C2 = Ctx(nc, es2, C.pfx)
        ccT = C2.sb(tag + "ccT", [128, 16])
        scT = C2.sb(tag + "scT", [128, 16])
        ones = C2.sb(tag + "ones", [128, 128])
        crep = C2.sb(tag + "crep", [128, 16, 128])
        wm = [C2.sb(tag + "wm%d" % i, [128, 8, 512]) for i in range(2)]
        rowt = C2.sb(tag + "rowt", [1, 1024])
        brow = C2.sb(tag + "brow", [1, 1024])
        P.dma(ccT[:], ccT_d, writes=[tag + "ccT"])
        P.act(scT[:], ccT[:], AF.Silu, [tag + "ccT"], [tag + "scT"])
        P.memset("pool", ones[:], 1.0, [tag + "ones"])
        for jc in range(16):
            P.ts("dve", crep[:, jc, :], ones[:], scT[:, jc:jc + 1], None, ALU.mult, None,
                 [tag + "ones", tag + "scT"], [tag + "crep"])
        bi = 0
        for ri, rd in enumerate(rows_d):
            P.dma(rowt[:], rd, writes=[tag + "rowt"])
            for hf in range(2):
                b = banks[bi % 2]
                bi += 1
                P.mm(b[0][:], ones[0:1, :], rowt[0:1, hf * 512:(hf + 1) * 512], True, True,
                     [tag + "ones", tag + "rowt"], [b[1]])
                P.cp("act", rowB[:, ri, hf * 512:(hf + 1) * 512], b[0][:], [b[1]], [tag + "rowB"])
        wv = wmod_d.rearrange("(c p) n -> p c n", p=128)
        li = 0
        for gi, g in enumerate(groups):
            P.dma(brow[:], bmod_d[0:1, g * 1024:(g + 1) * 1024], writes=[tag + "brow"])
            for hf in range(2):
                col0 = g * 1024 + hf * 512
                w = wm[li % 2]
                wk = tag + "wm%d" % (li % 2)
                li += 1
                P.dma(w[:], wv[:, :, col0:col0 + 512], writes=[wk])
                for j in range(2):
                    b = banks[bi % 2]
                    bi += 1
                    for c in range(8):
                        P.mm(b[0][:], crep[:, j * 8 + c, :], w[:, c, :], c == 0, False,
                             [tag + "crep", wk], [b[1]])
                    P.mm(b[0][:], ones[0:1, :], brow[0:1, hf * 512:(hf + 1) * 512], False, True,
                         [tag + "ones", tag + "brow"], [b[1]])
                    P.cp("act", modB[:, j, gi, hf * 512:(hf + 1) * 512], b[0][:], [b[1]], [tag + "modB"])
        P.barrier()
    return modB, rowB


def emit_rstd(P, ss, tag, n=D):
    P.ts("dve", ss[:, 1:2], ss[:, 0:1], 1.0 / n, 1e-6, ALU.mult, ALU.add, [tag + "ss"], [tag + "ss1"])
    P.act(ss[:, 3:4], ss[:, 1:2], AF.Sqrt, [tag + "ss1"], [tag + "ss3"])
    P.op("dve", lambda e: e.reciprocal(ss[:, 2:3], ss[:, 3:4]), [tag + "ss3"], [tag + "ss2"])


def emit_norm_mod(P, xt, xkey, G, S, gskeys, junk, ss, tmp, hf_out, hkey, tag):
    P.act(tmp[:], xt, AF.Square, [xkey], [tag + "tmp", tag + "ss"], accum_out=ss[:, 0:1])
    emit_rstd(P, ss, tag)
    P.stt(tmp[:], xt, ss[:, 2:3], G, ALU.mult, ALU.mult, [xkey, tag + "ss2"] + gskeys, [tag + "tmp"])
    P.tt("pool", hf_out, tmp[:], S, ALU.add, [tag + "tmp"] + gskeys, [hkey])


def emit_ffn(P, nc, C, banks, kind, tiles_spec, dd, final_norm):
    ntile = len(tiles_spec)
    tl = tiles_spec
    FF = 2816 if kind == "dense" else 3584
    NE = 1 if kind == "dense" else 8
    ccT_d, wmod_d, bmod_d, nffn_d, nf_d = dd["ccT"], dd["w_mod"], dd["b_mod"], dd["norm_ffn"], dd["norm_f"]
    wg_d, wu_d, wd_d = dd["wg"], dd["wu"], dd["wd"]
    wrT_d = dd.get("wrT")

    xall = C.sb("xall", [128, ntile, D])
    hT = C.sb("hT", [128, 8, ntile * 128], BF16)
    gateB = C.sb("gateB", [128, 2, D])
    nfB = C.sb("nfB", [128, D]) if final_norm else None
    comb = C.sb("comb", [128, ntile, 8])
    ss = C.sb("ss", [128, 4])
    with ExitStack() as e1:
        C1 = Ctx(nc, e1, C.pfx)
        modB, rowB = emit_mod(P, C1, banks, ccT_d, wmod_d, bmod_d, [3, 4, 5], [nffn_d, nf_d], "m_")
        for j in range(2):
            P.stt(modB[:, j, 1, :], modB[:, j, 1, :], 1.0, rowB[:, 0, :], ALU.add, ALU.mult,
                  ["m_modB", "m_rowB"], ["m_modB"])
            P.cp("pool", gateB[:, j, :], modB[:, j, 2, :], ["m_modB"], ["gateB"])
        if final_norm:
            P.cp("pool", nfB[:], rowB[:, 1, :], ["m_rowB"], ["nfB"])
        ident = C1.sb("ident", [128, 128])
        P.memset("pool", ident[:], 1.0, ["ident"])
        _asel(P, ident[:], "ident", [[-1, 128]], ALU.is_equal, 0, 1)
        tmp = C1.sb("tmp", [128, D])
        hf = C1.sb("hf", [128, D])
        if kind == "moe":
            junk = C1.sb("junk", [128, D])
            wrB = C1.sb("wrB", [128, 8, D])
            ones1 = C1.sb("ones1", [1, 128])
            P.memset("pool", ones1[:], 1.0, ["ones1"])
            for e_ in range(8):
                P.dma(junk[0:1, :], wrT_d[e_:e_ + 1, :], writes=["n_junk"])
                for h2 in range(2):
                    b = banks[h2]
                    P.mm(b[0][:], ones1[0:1, :], junk[0:1, h2 * 512:(h2 + 1) * 512], True, True,
                         ["ones1", "n_junk"], [b[1]])
                    P.cp("act", wrB[:, e_, h2 * 512:(h2 + 1) * 512], b[0][:], [b[1]], ["wrB"])
            logit = C1.sb("logit", [128, 8])
            top8 = C1.sb("top8", [128, 8])
            cb2 = C1.sb("cb2", [128, 8])
            rt = C1.sb("rt", [128, 8])
        for t in range(ntile):
            j = tl[t][4]
            xk = "xall%d" % t
            P.dma(xall[:, t, :], tl[t][0], reads=[tl[t][1]], writes=[xk])
            emit_norm_mod(P, xall[:, t, :], xk, modB[:, j, 1, :], modB[:, j, 0, :], ["m_modB"],
                          None, ss, tmp, hf[:], "hf", "n_")
            if kind == "moe":
                for e_ in range(8):
                    P.op("dve", (lambda e, e_=e_: e.scalar_tensor_tensor(
                        junk[:], hf[:], 1.0, wrB[:, e_, :], ALU.mult, ALU.mult, accum_out=logit[:, e_:e_ + 1])),
                        ["hf", "wrB"], ["n_junk", "logit"])
                P.op("dve", lambda e: e.max(top8[:], logit[:]), ["logit"], ["top8"])
                P.tt("dve", rt[:, 0:1], top8[:, 1:2], top8[:, 0:1], ALU.subtract, ["top8"], ["rt"])
                P.act(rt[:, 1:2], rt[:, 0:1], AF.Exp, ["rt"], ["rt"])
                P.ts("dve", rt[:, 2:3], rt[:, 1:2], 1.0, None, ALU.add, None, ["rt"], ["rt"])
                P.op("dve", lambda e: e.reciprocal(rt[:, 3:4], rt[:, 2:3]), ["rt"], ["rt"])
                P.tt("dve", rt[:, 4:5], rt[:, 1:2], rt[:, 3:4], ALU.mult, ["rt"], ["rt"])
                P.ts("dve", comb[:, t, :], logit[:], top8[:, 0:1], rt[:, 3:4], ALU.is_equal, ALU.mult,
                     ["logit", "top8", "rt"], ["comb"])
                P.ts("dve", cb2[:], logit[:], top8[:, 1:2], rt[:, 4:5], ALU.is_equal, ALU.mult,
                     ["logit", "top8", "rt"], ["cb2"])
                P.tt("dve", comb[:, t, :], comb[:, t, :], cb2[:], ALU.add, ["comb", "cb2"], ["comb"])
            for half in range(2):
                b = banks[half]
                for c4 in range(4):
                    c = half * 4 + c4
                    P.tr(b[0][:, c4 * 128:(c4 + 1) * 128], hf[:, c * 128:(c + 1) * 128], ident[:],
                         ["hf", "ident"], [b[1]])
                P.cp("act", hT[:, half * 4:(half + 1) * 4, t * 128:(t + 1) * 128],
                     b[0][:].rearrange("p (c t) -> p c t", c=4), [b[1]], ["hT"])
        P.barrier()
    with ExitStack() as e2:
        C2 = Ctx(nc, e2, C.pfx)
        stg = [C2.sb("stg%d" % i, [128, 2048]) for i in range(3)]
        wgb = [C2.sb("wgb%d" % i, [128, 8, 512], BF16) for i in range(2)]
        wub = [C2.sb("wub%d" % i, [128, 8, 512], BF16) for i in range(2)]
        dbf = [C2.sb("dbf%d" % i, [128, 4, D], BF16) for i in range(2)]
        actT = [C2.sb("actT%d" % i, [128, 4, 512], BF16) for i in range(2)]
        sg = [C2.sb("sg%d" % i, [128, 512], BF16) for i in range(2)]
        tmp2 = C2.sb("tmp2", [128, 2, 512])
        nst = [0]
        cnt = {"g": 0, "a": 0, "b": 0, "t": 0}

        def stage_cast(src3, dst3, dkey, eng):
            k = nst[0] % 3
            nst[0] += 1
            a, w = src3.shape[1], src3.shape[2]
            v = stg[k][:].rearrange("p (a w) -> p a w", a=a)
            P.dma(v, src3, writes=["stg%d" % k])
            P.cp(eng, dst3, v, ["stg%d" % k], [dkey])

        def prefetch(pi, grp=None):
            calls = []
            sc = lambda *a: calls.append(a)
            _prefetch_list(pi, sc)
            n = len(calls)
            sel_ = range(n) if grp is None else range((grp * n) // 3, ((grp + 1) * n) // 3)
            for i in sel_:
                stage_cast(*calls[i])

        def _prefetch_list(pi, stage_cast):
            e_, g0, gw = parts[pi]
            pb = pi % 2
            nch = gw // 128
            wgv = wg_d[e_].rearrange("(c p) n -> p c n", p=128)
            wuv = wu_d[e_].rearrange("(c p) n -> p c n", p=128)
            wdv = wd_d[e_].rearrange("(c p) n -> p c n", p=128)
            if gw == 512:
                for c0 in range(0, 8, 4):
                    stage_cast(wgv[:, c0:c0 + 4, g0:g0 + 512], wgb[pb][:, c0:c0 + 4, :], "wgb%d" % pb, "act")
                    stage_cast(wuv[:, c0:c0 + 4, g0:g0 + 512], wub[pb][:, c0:c0 + 4, :], "wub%d" % pb, "act")
            else:
                stage_cast(wgv[:, :, g0:g0 + 256], wgb[pb][:, :, 0:256], "wgb%d" % pb, "act")
                stage_cast(wuv[:, :, g0:g0 + 256], wub[pb][:, :, 0:256], "wub%d" % pb, "act")
            for o in range(0, nch, 2):
                c0 = g0 // 128 + o
                stage_cast(wdv[:, c0:c0 + 2, :], dbf[pb][:, o:o + 2, :], "dbf%d" % pb, "pool")

        parts = [(e_, g0, min(512, FF - g0)) for e_ in range(NE) for g0 in range(0, FF, 512)]
        blocks = [list(range(b0, min(ntile, b0 + 4))) for b0 in range(0, ntile, 4)]
        prefetch(0)
        for pi, (e_, g0, gw) in enumerate(parts):
            pb = pi % 2
            nch = gw // 128
            for bix, blk in enumerate(blocks):
                if pi + 1 < len(parts) and 1 <= bix <= 3:
                    prefetch(pi + 1, bix - 1)
                nbt = len(blk) * 128
                t0_ = blk[0] * 128
                ab = cnt["a"] % 2
                cnt["a"] += 1
                for jj in range(nch):
                    gi = cnt["g"] % 2
                    cnt["g"] += 1
                    bG = banks[2 * gi]
                    bU = banks[2 * gi + 1]
                    for c in range(8):
                        P.mm(bG[0][:, 0:nbt], wgb[pb][:, c, jj * 128:(jj + 1) * 128], hT[:, c, t0_:t0_ + nbt],
                             c == 0, c == 7, ["wgb%d" % pb, "hT"], [bG[1]])
                    for c in range(8):
                        P.mm(bU[0][:, 0:nbt], wub[pb][:, c, jj * 128:(jj + 1) * 128], hT[:, c, t0_:t0_ + nbt],
                             c == 0, c == 7, ["wub%d" % pb, "hT"], [bU[1]])
                    P.act(sg[gi][:, 0:nbt], bG[0][:, 0:nbt], AF.Silu, [bG[1]], ["sg%d" % gi])
                    P.tt("dve", actT[ab][:, jj, 0:nbt], sg[gi][:, 0:nbt], bU[0][:, 0:nbt], ALU.mult,
                         ["sg%d" % gi, bU[1]], ["actT%d" % ab])
                for ti, t in enumerate(blk):
                    j = tl[t][4]
                    for h2 in range(2):
                        bi = cnt["b"] % 4
                        cnt["b"] += 1
                        b = banks[4 + bi]
                        for jj in range(nch):
                            P.mm(b[0][:], actT[ab][:, jj, ti * 128:(ti + 1) * 128], dbf[pb][:, jj, h2 * 512:(h2 + 1) * 512],
                                 jj == 0, jj == nch - 1, ["actT%d" % ab, "dbf%d" % pb], [b[1]])
                        cs = slice(h2 * 512, (h2 + 1) * 512)
                        tk = cnt["t"] % 2
                        cnt["t"] += 1
                        if kind == "dense":
                            P.tt("dve", tmp2[:, tk, :], b[0][:], gateB[:, j, cs], ALU.mult, [b[1], "gateB"], ["tmp2_%d" % tk])
                        else:
                            P.stt(tmp2[:, tk, :], b[0][:], comb[:, t, e_:e_ + 1], gateB[:, j, cs], ALU.mult, ALU.mult,
                                  [b[1], "gateB", "comb"], ["tmp2_%d" % tk])
                        P.tt("pool", xall[:, t, cs], xall[:, t, cs], tmp2[:, tk, :], ALU.add,
                             ["tmp2_%d" % tk, "xall%d" % t], ["xall%d" % t])
        for t in range(ntile):
            xk = "xall%d" % t
            if final_norm:
                P.act(tmp2[:].rearrange("p a b -> p (a b)"), xall[:, t, :], AF.Square, [xk], ["tmp2_0", "tmp2_1", "n_ss"],
                      accum_out=ss[:, 0:1])
                emit_rstd(P, ss, "n_")
                P.stt(xall[:, t, :], xall[:, t, :], ss[:, 2:3], nfB[:], ALU.mult, ALU.mult,
                      [xk, "n_ss2", "nfB"], [xk])
            P.dma(tl[t][2], xall[:, t, :], reads=[xk], writes=[tl[t][3]])
        P.barrier()


NKT = 34


def _asel(P, t, key, pattern, op, base, cm):
    P.op("pool", lambda e: e.affine_select(out=t, in_=t, pattern=pattern, compare_op=op, fill=0.0,
                                            base=base, channel_multiplier=cm), [key], [key])


def emit_rope(P, eng, dst, src, cs, sn, H, hw, r1, r2, rkeys, skey, dkey):
    X = src.rearrange("p (h a b i) -> p h a b i", h=H, a=2, b=2, i=hw)
    Y = dst.rearrange("p (h a b i) -> p h a b i", h=H, a=2, b=2, i=hw)
    R1 = r1.rearrange("p (h a i) -> p h a i", h=H, a=2, i=hw)
    R2 = r2.rearrange("p (h a i) -> p h a i", h=H, a=2, i=hw)
    cb = cs.rearrange("p (a i) -> p a i", a=2).unsqueeze(1).to_broadcast([128, H, 2, hw])
    sb = sn.rearrange("p (a i) -> p a i", a=2).unsqueeze(1).to_broadcast([128, H, 2, hw])
    x1 = X[:, :, :, 0, :]
    x2 = X[:, :, :, 1, :]
    P.tt(eng, R1, x1, cb, ALU.mult, [skey] + rkeys, ["rp1"])
    P.tt(eng, R2, x2, sb, ALU.mult, [skey] + rkeys, ["rp2"])
    P.tt(eng, Y[:, :, :, 0, :], R1, R2, ALU.subtract, ["rp1", "rp2"], [dkey])
    P.tt(eng, R1, x1, sb, ALU.mult, [skey] + rkeys, ["rp1"])
    P.tt(eng, R2, x2, cb, ALU.mult, [skey] + rkeys, ["rp2"])
    P.tt(eng, Y[:, :, :, 1, :], R1, R2, ALU.add, ["rp1", "rp2"], [dkey])


def emit_mixer(P, nc, C, banks, need_ctx, lam_init, dd):
    debug = False
    ccT_d, wmod_d, bmod_d, nmix_d = dd["ccT"], dd["w_mod"], dd["b_mod"], dd["norm_mix"]
    win_d, wout_d = dd["w_in"], dd["w_out"]
    w4f_d, w4b_d, a2f_d, a2b_d, rows_d = dd["w4Tf"], dd["w4Tb"], dd["a2f"], dd["a2b"], dd["rows"]
    ropeO = dd["ropeO"]
    B = [b[0] for b in banks]
    BK = [b[1] for b in banks]

    modB, rowB0 = emit_mod(P, C, banks, ccT_d, wmod_d, bmod_d, [0, 1, 2], [nmix_d], "m_")
    for j in range(2):
        P.stt(modB[:, j, 1, :], modB[:, j, 1, :], 1.0, rowB0[:, 0, :], ALU.add, ALU.mult,
              ["m_modB", "m_rowB"], ["m_modB"])
    MK = ["m_modB"]

    ident = C.sb("ident", [128, 128])
    P.memset("pool", ident[:], 1.0, ["ident"])
    _asel(P, ident[:], "ident", [[-1, 128]], ALU.is_equal, 0, 1)
    msk = C.sb("msk", [128, 6, 128])
    for i in range(4):
        P.memset("pool", msk[:, i, :], -1.0 / 16.0, ["msk"])
    for i in range(4, 6):
        P.memset("pool", msk[:, i, :], 1.0, ["msk"])
    _asel(P, msk[:, 0, :], "msk", [[1, 128]], ALU.is_ge, 0, -1)
    _asel(P, msk[:, 1, :], "msk", [[-1, 128]], ALU.is_ge, 0, 1)
    _asel(P, msk[:, 2, :], "msk", [[-1, 128]], ALU.is_gt, 0, 1)
    _asel(P, msk[:, 3, :], "msk", [[1, 128]], ALU.is_gt, 0, -1)
    _asel(P, msk[:, 4, :], "msk", [[1, 128]], ALU.is_ge, 0, -1)
    _asel(P, msk[:, 5, :], "msk", [[-1, 128]], ALU.is_ge, 0, 1)
    bd = C.sb("bd", [128, 256])
    P.memset("pool", bd[:], 1.0, ["bd"])
    for h in range(4):
        _asel(P, bd[:, h * 64:(h + 1) * 64], "bd", [[0, 64]], ALU.is_ge, -32 * h, 1)
        _asel(P, bd[:, h * 64:(h + 1) * 64], "bd", [[0, 64]], ALU.is_ge, 32 * h + 31, -1)
    sel = C.sb("sel", [128, 4])
    P.dma(sel[:, 0:2], dd["sel"], writes=["sel"])
    negcol = C.sb("negcol", [128, 1])
    P.memset("pool", negcol[:], -1.0 / 16.0, ["negcol"])
    ones1 = C.sb("ones1", [1, 128])
    P.memset("pool", ones1[:], 1.0, ["ones1"])
    rB = C.sb("rB", [128, 1536])
    with ExitStack() as es0:
        rowS = Ctx(nc, es0, C.pfx).sb("rowS", [1, 2048])
        P.dma(rowS[:], rows_d, writes=["rowS"])
        for i in range(3):
            P.mm(B[i][:], ones1[0:1, :], rowS[0:1, i * 512:(i + 1) * 512], True, True, ["ones1", "rowS"], [BK[i]])
            P.cp("act", rB[:, i * 512:(i + 1) * 512], B[i][:], [BK[i]], ["rB"])
        P.barrier()
    abB = rB[:, 0:256]
    gnB = rB[:, 256:512]
    qnB = rB[:, 512:1024]
    knB = rB[:, 1024:1152]
    dnB = rB[:, 1152:1408]
    lmB = rB[:, 1408:1536]
    lam = C.sb("lam", [128, 8])
    junk = C.sb("junk", [128, 32])
    P.op("dve", lambda e: e.scalar_tensor_tensor(junk[:, 0:32], lmB[:, 0:32], 1.0, lmB[:, 32:64], ALU.mult, ALU.mult,
                                                  accum_out=lam[:, 0:1]), ["rB"], ["n_junk", "lam"])
    P.op("dve", lambda e: e.scalar_tensor_tensor(junk[:, 0:32], lmB[:, 64:96], 1.0, lmB[:, 96:128], ALU.mult, ALU.mult,
                                                  accum_out=lam[:, 1:2]), ["rB"], ["n_junk", "lam"])
    P.act(lam[:, 2:4], lam[:, 0:2], AF.Exp, ["lam"], ["lam2"])
    P.tt("dve", lam[:, 4:5], lam[:, 3:4], lam[:, 2:3], ALU.subtract, ["lam2"], ["lam3"])
    P.ts("dve", lam[:, 5:6], lam[:, 4:5], -lam_init, None, ALU.add, None, ["lam3"], ["nlam"])
    nlam = lam[:, 5:6]
    P.ts("dve", rB[:, 512:1024], rB[:, 512:1024], 0.125, None, ALU.mult, None, ["rB"], ["rB"])
    P.ts("dve", rB[:, 1152:1408], rB[:, 1152:1408], 1.0 - lam_init, None, ALU.mult, None, ["rB"], ["rB"])

    KT = C.sb("KT", [64, 2, NKT * 128], BF16)
    Vg = C.sb("Vg", [128, NKT, 2, 65], BF16)
    KTd = C.sb("KTd", [128, 2, NKT * 128], BF16)
    Vd = C.sb("Vd", [128, NKT, 4, 65], BF16)
    P.memset("pool", Vg[:, :, :, 64:65], 1.0, ["Vg"])
    P.memset("pool", Vd[:, :, :, 64:65], 1.0, ["Vd"])
    glaout = C.sb("glaout", [128, 18, 256], BF16)
    ofS = glaout
    xt = C.sb("xt", [128, D])
    tmp = C.sb("tmp", [128, D])
    hf = C.sb("hf", [128, D])
    ss = C.sb("ss", [128, 4])
    hT = C.sb("hT", [128, 8, 128], BF16)
    sq = C.sb("sq", [128, 512])
    s8 = C.sb("s8", [128, 32])
    fa = C.sb("fa", [128, 512])
    fb = C.sb("fb", [128, 512])
    r1 = C.sb("r1", [128, 256])
    r2 = C.sb("r2", [128, 256])
    cG = C.sb("cG", [128, 64])
    cD = C.sb("cD", [128, 32])
    wst = [None, None]

    def load_w(dst, dcol, src_d, scol, width):
        for o in range(0, width, 256):
            w = min(256, width - o)
            k = load_w.n % 2
            load_w.n += 1
            P.dma(wst[k][:, :, 0:w], src_d.rearrange("(c p) n -> p c n", p=128)[:, :, scol + o:scol + o + w],
                  writes=["wst%d" % k])
            P.cp("pool", dst[:, :, dcol + o:dcol + o + w], wst[k][:, :, 0:w], ["wst%d" % k], ["W"])
    load_w.n = 0

    def load_h(src, j, rope):
        src_ap, src_key = src
        P.dma(xt[:], src_ap, reads=[src_key], writes=["xt"])
        if rope is not None:
            tabs, rope_row0 = rope
            P.dma(cG[:, 0:32], tabs[0][rope_row0:rope_row0 + 128, :], writes=["cG"])
            P.dma(cG[:, 32:64], tabs[1][rope_row0:rope_row0 + 128, :], writes=["cG"])
            P.dma(cD[:, 0:16], tabs[2][rope_row0:rope_row0 + 128, :], writes=["cD"])
            P.dma(cD[:, 16:32], tabs[3][rope_row0:rope_row0 + 128, :], writes=["cD"])
        emit_norm_mod(P, xt[:], "xt", modB[:, j, 1, :], modB[:, j, 0, :], MK, junk, ss, tmp, hf[:], "hf", "n_")
        for half in range(2):
            b = banks[3 + half]
            for c4 in range(4):
                c = half * 4 + c4
                P.tr(b[0][:, c4 * 128:(c4 + 1) * 128], hf[:, c * 128:(c + 1) * 128], ident[:], ["hf", "ident"], [b[1]])
            P.cp("act", hT[:, half * 4:(half + 1) * 4, :], b[0][:].rearrange("p (c t) -> p c t", c=4), [b[1]], ["hT"])

    def head_rstd(src_ps, src_key, H, dh, out_s8):
        P.act(sq[:, 0:H * dh], src_ps, AF.Square, [src_key], ["sq"])
        P.op("dve", lambda e: e.tensor_reduce(out=s8[:, 8:8 + H], in_=sq[:, 0:H * dh].rearrange("p (h d) -> p h d", h=H),
                                               axis=AX.X, op=ALU.add), ["sq"], ["s8a"])
        P.ts("dve", s8[:, 16:16 + H], s8[:, 8:8 + H], 1.0 / dh, 1e-6, ALU.mult, ALU.add, ["s8a"], ["s8b"])
        P.act(s8[:, 24:24 + H], s8[:, 16:16 + H], AF.Sqrt, ["s8b"], ["s8c"])
        P.op("dve", lambda e: e.reciprocal(out_s8, s8[:, 24:24 + H]), ["s8c"], ["s8"])

    with ExitStack() as es1:
        C1 = Ctx(nc, es1, C.pfx)
        wi1 = C1.sb("wi1", [128, 8, 1536], BF16)
        esw = ExitStack()
        Cw = Ctx(nc, esw, C.pfx)
        wst[0] = Cw.sb("wst0", [128, 8, 256])
        wst[1] = Cw.sb("wst1", [128, 8, 256])
        w4 = Cw.sb("w4", [16, 2, D])
        a2 = Cw.sb("a2", [16, 2, 128])
        P.dma(w4[:, 0, :], w4f_d, writes=["w4"])
        P.dma(w4[:, 1, :], w4b_d, writes=["w4"])
        P.dma(a2[:, 0, :], a2f_d, writes=["a2"])
        P.dma(a2[:, 1, :], a2b_d, writes=["a2"])
        for dr in range(2):
            for c in range(8):
                b = banks[c % 2]
                P.mm(b[0][:, 0:128], w4[:, dr, c * 128:(c + 1) * 128], a2[:, dr, :], True, True, ["w4", "a2"], [b[1]])
                P.cp("act", wi1[:, c, 256 + dr * 128:256 + (dr + 1) * 128], b[0][:, 0:128], [b[1]], ["W"])
        load_w(wi1, 0, win_d, 0, 256)
        load_w(wi1, 512, win_d, 256, 256)
        load_w(wi1, 768, win_d, 1312, 256)
        load_w(wi1, 1024, win_d, 2080, 256)
        load_w(wi1, 1280, win_d, 1824, 256)
        P.barrier()
        esw.close()
        NST = 18
        qkB = C1.sb("qkB", [128, NST, 2, 128], BF16)
        qkF = C1.sb("qkF", [128, 2, 128], BF16)
        kdB = C1.sb("kdB", [128, NST, 128], BF16)
        kdF = C1.sb("kdF", [128, 128], BF16)
        vS = C1.sb("vS", [128, NST, 256], BF16)
        dec = C1.sb("dec", [128, NST, 2])
        zt = C1.sb("zt", [128, 256])
        sp = C1.sb("sp", [128, 256])
        Eq = C1.sb("Eq", [128, 256])
        Ek = C1.sb("Ek", [128, 256])
        Ed = C1.sb("Ed", [128, 256])
        qk = C1.sb("qk", [128, 256])
        qdki = C1.sb("qdki", [128, 4, 128])
        kim = C1.sb("kim", [128, 4, 128], BF16)
        AT = C1.sb("AT", [128, 4, 128], BF16)
        KVm = C1.sb("KVm", [128, 256])
        S = [C1.sb("S%d" % i, [128, 256]) for i in range(2)]
        Sb = [C1.sb("Sb%d" % i, [128, 256], BF16) for i in range(2)]
        og = C1.sb("og", [128, 256])
        for i in range(2):
            P.memset("pool", S[i][:], 0.0, ["S%d" % i])
            P.memset("pool", Sb[i][:], 0.0, ["Sb%d" % i])

        def proj1():
            for g in range(3):
                for c in range(8):
                    P.mm(B[g][:], hT[:, c, :], wi1[:, c, g * 512:(g + 1) * 512], c == 0, c == 7, ["hT", "W"], [BK[g]])

        def gla_prep(st, full):
            P.tt("dve", zt[:], B[0][:, 256:512], abB, ALU.add, [BK[0], "rB"], ["zt"])
            P.act(Eq[:], zt[:], AF.Exp, ["zt"], ["Eq"], scale=-1.0)
            P.act(sp[:], Eq[:], AF.Ln, ["Eq"], ["sp"], bias=1.0)
            b3 = B[5]
            P.mm(b3[:, 0:128], msk[:, 0, :], sp[:, 0:128], True, True, ["msk", "sp"], [BK[5]])
            P.mm(b3[:, 128:256], msk[:, 1, :], sp[:, 128:256], True, True, ["msk", "sp"], [BK[5]])
            P.mm(b3[:, 256:384], msk[:, 2, :], sp[:, 0:128], True, True, ["msk", "sp"], [BK[5]])
            P.mm(b3[:, 384:512], msk[:, 3 if full else 2, :], sp[:, 128:256], True, True, ["msk", "sp"], [BK[5]])
            P.mm(B[6][:, 0:1], sp[:, 0:128], negcol[:], True, True, ["sp", "negcol"], [BK[6]])
            P.mm(B[6][:, 1:2], sp[:, 128:256], negcol[:], True, True, ["sp", "negcol"], [BK[6]])
            P.act(dec[:, st, :], B[6][:, 0:2], AF.Exp, [BK[6]], ["dec"])
            P.act(Ed[:], b3[:, 256:512], AF.Exp, [BK[5]], ["Ed"])
            P.cp("act", qk[:], B[0][:, 0:256], [BK[0]], ["qk"])
            P.tt("dve", kdB[:, st, :], qk[:, 128:256], Ed[:, 128:256], ALU.mult, ["qk", "Ed"], ["kdB"])
            if full:
                P.tt("dve", kdF[:], qk[:, 128:256], Ed[:, 0:128], ALU.mult, ["qk", "Ed"], ["kdF"])
            P.cp("act", vS[:, st, :], B[1][:, 0:256], [BK[1]], ["vS"])
            if not full:
                return
            P.act(Eq[:], b3[:, 0:256], AF.Exp, [BK[5]], ["Eq"])
            P.act(Ek[:], b3[:, 0:256], AF.Exp, [BK[5]], ["Ek"], scale=-1.0)
            for dr in range(2):
                P.stt(qdki[:, 2 * dr, :], qk[:, 0:128], 32.0 ** -0.5, Eq[:, dr * 128:(dr + 1) * 128], ALU.mult, ALU.mult,
                      ["qk", "Eq"], ["qdki"])
                P.tt("dve", qdki[:, 2 * dr + 1, :], qk[:, 128:256], Ek[:, dr * 128:(dr + 1) * 128], ALU.mult,
                     ["qk", "Ek"], ["qdki"])
            for i in range(4):
                P.tr(B[7][:, i * 128:(i + 1) * 128], qdki[:, i, :], ident[:], ["qdki", "ident"], [BK[7]])
            P.cp("act", qkF[:], B[7][:, 0:256].rearrange("p (i t) -> p i t", i=2), [BK[7]], ["qkF"])
            P.cp("act", qkB[:, st, :, :], B[7][:, 256:512].rearrange("p (i t) -> p i t", i=2), [BK[7]], ["qkB"])

        def kv_prep(kidx, rope):
            head_rstd(B[1][:, 256:384], BK[1], 2, 64, s8[:, 0:2])
            P.tt("dve", fa[:, 0:128].rearrange("p (h d) -> p h d", h=2), B[1][:, 256:384].rearrange("p (h d) -> p h d", h=2),
                 s8[:, 0:2].unsqueeze(2).to_broadcast([128, 2, 64]), ALU.mult, [BK[1], "s8"], ["fa"])
            P.tt("pool", fb[:, 0:128], fa[:, 0:128], knB, ALU.mult, ["fa", "rB"], ["fb"])
            src = fb
            if rope:
                emit_rope(P, "pool", fa[:, 0:128], fb[:, 0:128], cG[:, 0:32], cG[:, 32:64], 2, 16,
                          r1[:, 0:64], r2[:, 0:64], ["cG"], "fb", "fa")
                src = fa
            skey = "fa" if rope else "fb"
            for h in range(2):
                P.tr(B[7][0:64, h * 128:(h + 1) * 128], src[:, h * 64:(h + 1) * 64], ident[:], [skey, "ident"], [BK[7]])
            P.cp("act", KT[:, :, kidx * 128:(kidx + 1) * 128], B[7][0:64, 0:256].rearrange("p (h t) -> p h t", h=2),
                 [BK[7]], ["KT"])
            P.cp("act", Vg[:, kidx, :, 0:64], B[1][:, 384:512].rearrange("p (h d) -> p h d", h=2), [BK[1]], ["Vg"])
            P.cp("act", Vd[:, kidx, :, 0:64], B[2][:, 0:256].rearrange("p (h d) -> p h d", h=4), [BK[2]], ["Vd"])
            P.cp("act", fb[:, 256:512], B[2][:, 256:512], [BK[2]], ["fb2"])
            src2, k2 = fb[:, 256:512], "fb2"
            if rope:
                emit_rope(P, "pool", fa[:, 256:512], fb[:, 256:512], cD[:, 0:16], cD[:, 16:32], 8, 8,
                          r1[:, 0:128], r2[:, 0:128], ["cD"], "fb2", "fa2")
                src2, k2 = fa[:, 256:512], "fa2"
            for g in range(2):
                P.tr(B[7][:, 256 + g * 128:256 + (g + 1) * 128], src2[:, g * 128:(g + 1) * 128], ident[:],
                     [k2, "ident"], [BK[7]])
            P.cp("act", KTd[:, :, kidx * 128:(kidx + 1) * 128], B[7][:, 256:512].rearrange("p (g t) -> p g t", g=2),
                 [BK[7]], ["KTd"])

        def scan_step(dr, st, with_out, final_to=None, slot=None):
            Sd, Sbd = S[dr], Sb[dr]
            sk, sbk = "S%d" % dr, "Sb%d" % dr
            if dr == 0:
                qd_, ki_, kd_, qkk, kdk = qkF[:, 0, :], qkF[:, 1, :], kdF[:], "qkF", "kdF"
            else:
                qd_, ki_, kd_, qkk, kdk = qkB[:, st, 0, :], qkB[:, st, 1, :], kdB[:, st, :], "qkB", "kdB"
            if with_out:
                for h in range(4):
                    P.ts("pool", kim[:, h, :], ki_, bd[:, 64 * h:64 * h + 1], None, ALU.mult, None,
                         [qkk, "bd"], ["kim"])
                for h in range(4):
                    P.mm(B[5][:, h * 128:(h + 1) * 128], kim[:, h, :], qd_, True, True, ["kim", qkk], [BK[5]])
                P.tt("dve", AT[:], B[5][:].rearrange("p (h c) -> p h c", h=4),
                     msk[:, 4 + dr, :].unsqueeze(1).to_broadcast([128, 4, 128]), ALU.mult, [BK[5], "msk"], ["AT"])
                for h in range(4):
                    P.mm(B[7][:, h * 64:(h + 1) * 64], AT[:, h, :], vS[:, st, h * 64:(h + 1) * 64], True, False,
                         ["AT", "vS"], [BK[7]])
                    P.mm(B[7][:, h * 64:(h + 1) * 64], qd_, Sbd[:, h * 64:(h + 1) * 64], False, True,
                         [qkk, sbk], [BK[7]])
                if final_to is None:
                    P.cp("act", ofS[:, st, :], B[7][:, 0:256], [BK[7]], ["ofS"])
                else:
                    P.tt("dve", og[:], B[7][:, 0:256], ofS[:, st, :], ALU.add, [BK[7], "ofS"], ["og"])
                    P.act(sq[:, 0:256], og[:], AF.Square, ["og"], ["sq"])
                    P.op("dve", lambda e: e.tensor_reduce(out=s8[:, 8:12], in_=sq[:, 0:256].rearrange("p (h d) -> p h d", h=4),
                                                           axis=AX.X, op=ALU.add), ["sq"], ["s8a"])
                    P.ts("dve", s8[:, 16:20], s8[:, 8:12], 1.0 / 64, 1e-6, ALU.mult, ALU.add, ["s8a"], ["s8b"])
                    P.act(s8[:, 24:28], s8[:, 16:20], AF.Sqrt, ["s8b"], ["s8c"])
                    P.op("dve", lambda e: e.reciprocal(s8[:, 0:4], s8[:, 24:28]), ["s8c"], ["s8"])
                    P.tt("dve", og[:].rearrange("p (h d) -> p h d", h=4), og[:].rearrange("p (h d) -> p h d", h=4),
                         s8[:, 0:4].unsqueeze(2).to_broadcast([128, 4, 64]), ALU.mult, ["og", "s8"], ["og"])
                    P.tt("pool", glaout[:, final_to, :], og[:], gnB, ALU.mult, ["og", "rB"], ["glaout"])
            P.mm(B[6][:, 0:256], kd_, vS[:, st, :], True, True, [kdk, "vS"], [BK[6]])
            if slot is None:
                P.tt("dve", KVm[:], B[6][:, 0:256], bd[:], ALU.mult, [BK[6], "bd"], ["KVm"])
                P.stt(Sd[:], Sd[:], dec[:, st, dr:dr + 1], KVm[:], ALU.mult, ALU.add, [sk, "dec", "KVm"], [sk])
            else:
                sg_ = sel[:, slot:slot + 1]
                P.stt(KVm[:], B[6][:, 0:256], sg_, bd[:], ALU.mult, ALU.mult, [BK[6], "bd", "sel"], ["KVm"])
                P.ts("dve", sel[:, 2:3], dec[:, st, dr:dr + 1], -1.0, sg_, ALU.add, ALU.mult, ["dec", "sel"], ["sel2"])
                P.ts("dve", sel[:, 3:4], sel[:, 2:3], 1.0, None, ALU.add, None, ["sel2"], ["sel3"])
                P.stt(Sd[:], Sd[:], sel[:, 3:4], KVm[:], ALU.mult, ALU.add, [sk, "sel3", "KVm"], [sk])
            P.cp("act", Sbd[:], Sd[:], [sk], [sbk])

        for i in range(2):
            load_h(dd["ctx"][i], 1, None)
            proj1()
            gla_prep(16 + i, True)
            kv_prep(32 + i, False)
            scan_step(0, 16 + i, need_ctx, None)
        for i in (1, 0):
            scan_step(1, 16 + i, need_ctx, (16 + i) if need_ctx else None)
        for t in range(16):
            load_h(dd["xown"][t], 0, (ropeO, t * 128))
            proj1()
            gla_prep(t, True)
            kv_prep(t, True)
            scan_step(0, t, True, None)
        groups = [[0, 1], [2, 3], [4, 5], [6, 7]]
        X = dd["xch"]

        def allgather(src, dst, key):
            P.coll(lambda e: e.collective_compute("AllGather", ALU.bypass, replica_groups=groups,
                                                   ins=[src], outs=[dst]), [key + "_i"], [key + "_o"])
        P.dma(X["sx_i"], S[0][:], reads=["S0"], writes=["sx_i"])
        allgather(X["sx_i"], X["sx_o"], "sx")
        Sx = C1.sb("Sx", [128, 2, 256])
        P.dma(Sx[:], X["sx_o"].rearrange("(s p) n -> p s n", p=128), reads=["sx_o"], writes=["Sx"])
        P.ts("dve", KVm[:], Sx[:, 0, :], sel[:, 0:1], None, ALU.mult, None, ["Sx", "sel"], ["KVm"])
        P.stt(S[1][:], Sx[:, 1, :], sel[:, 1:2], KVm[:], ALU.mult, ALU.add, ["Sx", "sel", "KVm"], ["S1"])
        P.cp("act", Sb[1][:], S[1][:], ["S1"], ["Sb1"])
        P.dma(X["kt_i"].rearrange("p (k t) -> p k t", k=2), KT[:, :, 0:2048], reads=["KT"], writes=["kt_i"])
        P.dma(X["ktd_i"].rearrange("p (k t) -> p k t", k=2), KTd[:, :, 0:2048], reads=["KTd"], writes=["ktd_i"])
        P.dma(X["vg_i"].rearrange("p (t k d) -> p t k d", t=16, k=2), Vg[:, 0:16, :, :], reads=["Vg"], writes=["vg_i"])
        for hh in range(2):
            P.dma(X["vd_i%d" % hh].rearrange("p (t k d) -> p t k d", t=8, k=4), Vd[:, hh * 8:(hh + 1) * 8, :, :],
                  reads=["Vd"], writes=["vd%d_i" % hh])
        allgather(X["kt_i"], X["kt_o"], "kt")
        allgather(X["ktd_i"], X["ktd_o"], "ktd")
        allgather(X["vg_i"], X["vg_o"], "vg")
        for hh in range(2):
            allgather(X["vd_i%d" % hh], X["vd_o%d" % hh], "vd%d" % hh)
        for sl in range(2):
            P.dma(KT[:, :, sl * 2048:(sl + 1) * 2048], X["kt_o"][sl * 64:(sl + 1) * 64, :].rearrange("p (k t) -> p k t", k=2),
                  reads=["kt_o"], writes=["KT"])
            P.dma(KTd[:, :, sl * 2048:(sl + 1) * 2048], X["ktd_o"][sl * 128:(sl + 1) * 128, :].rearrange("p (k t) -> p k t", k=2),
                  reads=["ktd_o"], writes=["KTd"])
            P.dma(Vg[:, sl * 16:(sl + 1) * 16, :, :], X["vg_o"][sl * 128:(sl + 1) * 128, :].rearrange("p (t k d) -> p t k d", t=16, k=2),
                  reads=["vg_o"], writes=["Vg"])
            for hh in range(2):
                P.dma(Vd[:, sl * 16 + hh * 8:sl * 16 + (hh + 1) * 8, :, :],
                      X["vd_o%d" % hh][sl * 128:(sl + 1) * 128, :].rearrange("p (t k d) -> p t k d", t=8, k=4),
                      reads=["vd%d_o" % hh], writes=["Vd"])
        for t in range(15, -1, -1):
            scan_step(1, t, True, t)
        P.barrier()

    with ExitStack() as es2:
        C2 = Ctx(nc, es2, C.pfx)
        wi2 = C2.sb("wi2", [128, 8, 1024], BF16)
        wo = C2.sb("wo", [128, 8, 1024], BF16)
        with ExitStack() as esw2:
            Cw2 = Ctx(nc, esw2, C.pfx)
            wst[0] = Cw2.sb("wst0b", [128, 8, 256])
            wst[1] = Cw2.sb("wst1b", [128, 8, 256])
            load_w(wi2, 0, win_d, 800, 512)
            load_w(wi2, 512, win_d, 1568, 256)
            load_w(wi2, 768, win_d, 512, 256)
            load_w(wo, 0, wout_d, 0, 1024)
            P.barrier()
        QT = C2.sb("QT", [64, 8, 128], BF16)
        QTd = C2.sb("QTd", [128, 2, 128], BF16)
        QTm = C2.sb("QTm", [128, 8, 128], BF16)
        PT = [C2.sb("PT%d" % i, [128, 512], BF16) for i in range(3)]
        cat = C2.sb("cat", [128, D])
        catT = C2.sb("catT", [128, 8, 128], BF16)
        rs = C2.sb("rs", [128, 256])
        rc = C2.sb("rc", [128, 16])
        od = C2.sb("od", [128, 256])
        t0 = C2.sb("t0", [128, 64])
        xo = C2.sb("xo", [128, D])
        OTs = [C2.sb("OTs%d" % i, [65, 512]) for i in range(2)]

        def pass2_tile(src, j, rope_row0, gidx, keys, dst):
            load_h(src, j, None if rope_row0 is None else (ropeO, rope_row0))
            for g in range(2):
                for c in range(8):
                    P.mm(B[g][:], hT[:, c, :], wi2[:, c, g * 512:(g + 1) * 512], c == 0, c == 7, ["hT", "W"], [BK[g]])
            head_rstd(B[0][:], BK[0], 8, 64, s8[:, 0:8])
            P.tt("dve", fa[:].rearrange("p (h d) -> p h d", h=8), B[0][:].rearrange("p (h d) -> p h d", h=8),
                 s8[:, 0:8].unsqueeze(2).to_broadcast([128, 8, 64]), ALU.mult, [BK[0], "s8"], ["fa"])
            P.tt("pool", fb[:], fa[:], qnB, ALU.mult, ["fa", "rB"], ["fb"])
            src, skey = fb, "fb"
            if rope_row0 is not None:
                emit_rope(P, "pool", fa[:], fb[:], cG[:, 0:32], cG[:, 32:64], 8, 16, r1[:], r2[:], ["cG"], "fb", "fa")
                src, skey = fa, "fa"
            for hh in range(2):
                b = banks[2 + hh]
                for h4 in range(4):
                    h = hh * 4 + h4
                    P.tr(b[0][0:64, h4 * 128:(h4 + 1) * 128], src[:, h * 64:(h + 1) * 64], ident[:], [skey, "ident"], [b[1]])
                P.cp("act", QT[:, hh * 4:(hh + 1) * 4, :], b[0][0:64, :].rearrange("p (h t) -> p h t", h=4), [b[1]], ["QT"])
            P.act(sq[:, 0:256], B[1][:, 0:256], AF.Copy, [BK[1]], ["sq"], scale=32.0 ** -0.5)
            src2, k2 = sq[:, 0:256], "sq"
            if rope_row0 is not None:
                emit_rope(P, "pool", sq[:, 256:512], sq[:, 0:256], cD[:, 0:16], cD[:, 16:32], 8, 8,
                          r1[:, 0:128], r2[:, 0:128], ["cD"], "sq", "sqr")
                src2, k2 = sq[:, 256:512], "sqr"
            for g in range(2):
                P.tr(B[4][:, g * 128:(g + 1) * 128], src2[:, g * 128:(g + 1) * 128], ident[:], [k2, "ident"], [BK[4]])
            P.cp("act", QTd[:], B[4][:, 0:256].rearrange("p (g t) -> p g t", g=2), [BK[4]], ["QTd"])
            for m in range(8):
                P.ts("pool", QTm[:, m, :], QTd[:, m // 4, :], bd[:, 64 * (m % 4):64 * (m % 4) + 1], None, ALU.mult, None,
                     ["QTd", "bd"], ["QTm"])
            P.act(rs[:], B[1][:, 256:512], AF.Silu, [BK[1]], ["rs"])
            P.tt("dve", cat[:, 0:256], glaout[:, gidx, :], rs[:], ALU.mult, ["glaout", "rs"], ["cat"])
            items = []

            def untranspose(grp):
                ob = banks[6 + grp]
                tb = banks[grp]
                P.cp("dve", OTs[grp][:], ob[0][0:65, :], [ob[1]], ["OTs%d" % grp])
                for g in range(4):
                    P.tr(tb[0][:, g * 65:(g + 1) * 65], OTs[grp][0:65, g * 128:(g + 1) * 128], ident[0:65, 0:65],
                         ["OTs%d" % grp, "ident"], [tb[1]])
                return tb

            def gqa_post(kv):
                ob = untranspose(kv)
                for g in range(4):
                    h = kv * 4 + g
                    P.op("dve", lambda e, g=g, ob=ob, h=h: e.reciprocal(rc[:, h:h + 1], ob[0][:, g * 65 + 64:g * 65 + 65]),
                         [ob[1]], ["rc"])
                    P.ts("dve", cat[:, 256 + h * 64:256 + (h + 1) * 64], ob[0][:, g * 65:g * 65 + 64], rc[:, h:h + 1], None,
                         ALU.mult, None, [ob[1], "rc"], ["cat"])

            def diff_post(gg):
                ob = untranspose(gg)
                for hh in range(2):
                    h = gg * 2 + hh
                    m0, m1 = 2 * hh, 2 * hh + 1
                    P.op("dve", lambda e, ob=ob, m0=m0: e.reciprocal(rc[:, 8:9], ob[0][:, m0 * 65 + 64:m0 * 65 + 65]),
                         [ob[1]], ["rc"])
                    P.op("dve", lambda e, ob=ob, m1=m1: e.reciprocal(rc[:, 9:10], ob[0][:, m1 * 65 + 64:m1 * 65 + 65]),
                         [ob[1]], ["rc"])
                    P.tt("dve", rc[:, 10:11], rc[:, 9:10], nlam, ALU.mult, ["rc", "nlam"], ["rc"])
                    P.ts("dve", t0[:], ob[0][:, m0 * 65:m0 * 65 + 64], rc[:, 8:9], None, ALU.mult, None, [ob[1], "rc"], ["t0"])
                    P.stt(od[:, h * 64:(h + 1) * 64], ob[0][:, m1 * 65:m1 * 65 + 64], rc[:, 10:11], t0[:], ALU.mult, ALU.add,
                          [ob[1], "rc", "t0"], ["od"])

            nk = len(keys)
            for typ in range(2):
                for grp in range(2):
                    for si, s in enumerate(keys):
                        items.append((typ, grp, si, s))
            NB = 3

            def emit_score(ix):
                typ, grp, si, s = items[ix]
                sbk = banks[3 + ix % NB]
                if typ == 0:
                    P.mm(sbk[0][:], KT[:, grp, s * 128:(s + 1) * 128], QT[:, grp * 4:(grp + 1) * 4, :], True, True,
                         ["KT", "QT"], [sbk[1]])
                else:
                    P.mm(sbk[0][:], KTd[:, grp, s * 128:(s + 1) * 128], QTm[:, grp * 4:(grp + 1) * 4, :], True, True,
                         ["KTd", "QTm"], [sbk[1]])

            def emit_rest(ix):
                typ, grp, si, s = items[ix]
                sbk = banks[3 + ix % NB]
                pt = PT[ix % NB]
                ptk = "PT%d" % (ix % NB)
                ob = banks[6 + grp]
                P.act(pt[:], sbk[0][:], AF.Exp, [sbk[1]], [ptk])
                if typ == 0:
                    P.mm(ob[0][0:65, :], Vg[:, s, grp, :], pt[:], si == 0, si == nk - 1, [ptk, "Vg"], [ob[1]], skip=True)
                else:
                    for hh in range(2):
                        P.mm(ob[0][0:65, hh * 256:(hh + 1) * 256], Vd[:, s, grp * 2 + hh, :], pt[:, hh * 256:(hh + 1) * 256],
                             si == 0 and hh == 0, si == nk - 1, [ptk, "Vd"], [ob[1]], skip=True)
                if si == nk - 1:
                    pending.append((ix + 3, gqa_post if typ == 0 else diff_post, grp))

            LOOK = 2
            pending = []
            for ix in range(min(LOOK, len(items))):
                emit_score(ix)
            for ix in range(len(items)):
                if ix + LOOK < len(items):
                    emit_score(ix + LOOK)
                emit_rest(ix)
                while pending and pending[0][0] <= ix:
                    _, fn_, g_ = pending.pop(0)
                    fn_(g_)
            while pending:
                _, fn_, g_ = pending.pop(0)
                fn_(g_)
            head_rstd(od[:], "od", 4, 64, s8[:, 0:4])
            P.tt("dve", od[:].rearrange("p (h d) -> p h d", h=4), od[:].rearrange("p (h d) -> p h d", h=4),
                 s8[:, 0:4].unsqueeze(2).to_broadcast([128, 4, 64]), ALU.mult, ["od", "s8"], ["od"])
            P.tt("pool", cat[:, 768:1024], od[:], dnB, ALU.mult, ["od", "rB"], ["cat"])
            if debug and gidx >= 16:
                P.dma(dbg_d[(gidx - 16) * 128:(gidx - 15) * 128, :], cat[:], reads=["cat"])
            for half in range(2):
                b = banks[2 + half]
                for c4 in range(4):
                    c = half * 4 + c4
                    P.tr(b[0][:, c4 * 128:(c4 + 1) * 128], cat[:, c * 128:(c + 1) * 128], ident[:], ["cat", "ident"], [b[1]])
                P.cp("act", catT[:, half * 4:(half + 1) * 4, :], b[0][:].rearrange("p (c t) -> p c t", c=4), [b[1]], ["catT"])
            for h2 in range(2):
                b = banks[h2]
                for c in range(8):
                    P.mm(b[0][:], catT[:, c, :], wo[:, c, h2 * 512:(h2 + 1) * 512], c == 0, c == 7, ["catT", "W"], [b[1]])
                cs = slice(h2 * 512, (h2 + 1) * 512)
                P.tt("dve", tmp[:, cs], b[0][:], modB[:, j, 2, cs], ALU.mult, [b[1]] + MK, ["n_tmp"])
                P.tt("pool", xo[:, cs], xt[:, cs], tmp[:, cs], ALU.add, ["n_tmp", "xt"], ["xo"])
            P.dma(dst[0], xo[:], reads=["xo"], writes=[dst[1]])

        for t in range(16):
            pass2_tile(dd["xown"][t], 0, t * 128, t, list(range(NKT)), dd["yo"][t])
        if need_ctx:
            for i in range(2):
                pass2_tile(dd["ctx"][i], 1, None, 16 + i, [32, 33], dd["yc"][i])
        P.barrier()


LAM_INIT = [0.8 - 0.6 * math.exp(-0.3 * l) for l in range(2)]


def build_fused(stages=("m0", "f0", "cc", "m1", "f1")):
    nc = bass.Bass("TRN2", target_bir_lowering=False)
    es = ExitStack()
    C = Ctx(nc, es)
    P = Prog(nc, es)
    di = {}

    def din(name, shape):
        di[name] = C.din(name, shape)
        return di[name]

    din("xown", [2048, D]); din("ctx", [256, D]); din("ccT", [128, 16]); din("sel", [128, 2])
    ropeO = [din("cosGo", [2048, 32]), din("sinGo", [2048, 32]), din("cosDo", [2048, 16]), din("sinDo", [2048, 16])]
    for l in range(2):
        din("w_mod%d" % l, [D, 6 * D]); din("b_mod%d" % l, [1, 6 * D])
        din("norm_mix%d" % l, [1, D]); din("norm_ffn%d" % l, [1, D])
        din("w_in%d" % l, [D, 2336]); din("w_out%d" % l, [D, D])
        din("w4Tf%d" % l, [16, D]); din("w4Tb%d" % l, [16, D]); din("a2f%d" % l, [16, 128]); din("a2b%d" % l, [16, 128])
        din("rows%d" % l, [1, 2048])
    din("norm_f", [1, D])
    din("wg0", [1, D, 2816]); din("wu0", [1, D, 2816]); din("wd0", [1, 2816, D])
    din("wg1", [8, D, 3584]); din("wu1", [8, D, 3584]); din("wd1", [8, 3584, D]); din("wrT", [8, D])
    y_d = C.dout("y", [2048, D])
    xmid0 = nc.dram_tensor("xmid0", [2304, D], F32, kind="Internal").ap()
    x1own = nc.dram_tensor("x1own", [2048, D], F32, kind="Internal").ap()

    def xch(l):
        def t(name, shape, dt):
            return nc.dram_tensor("x%d_%s" % (l, name), shape, dt, kind="Internal").ap()
        return {"sx_i": t("sx_i", [128, 256], F32), "sx_o": t("sx_o", [256, 256], F32),
                "kt_i": t("kt_i", [64, 4096], BF16), "kt_o": t("kt_o", [128, 4096], BF16),
                "ktd_i": t("ktd_i", [128, 4096], BF16), "ktd_o": t("ktd_o", [256, 4096], BF16),
                "vg_i": t("vg_i", [128, 2080], BF16), "vg_o": t("vg_o", [256, 2080], BF16),
                "vd_i0": t("vd_i0", [128, 2080], BF16), "vd_o0": t("vd_o0", [256, 2080], BF16),
                "vd_i1": t("vd_i1", [128, 2080], BF16), "vd_o1": t("vd_o1", [256, 2080], BF16)}
    xc1 = nc.dram_tensor("xc1", [256, D], F32, kind="Internal").ap()
    xmid1 = nc.dram_tensor("xmid1", [2048, D], F32, kind="Internal").ap()
    banks = [(C.ps("bank%d" % i, [128, 512]), "bank%d" % i) for i in range(8)]

    def tl(ap, key, n, off=0):
        return [(ap[off + t * 128:off + (t + 1) * 128, :], key) for t in range(n)]

    def mixer(l, xown_t, ctx_t, yo_t, yc_t):
        with ExitStack() as st:
            Cs = Ctx(nc, st, "M%d_" % l)
            dd = {"ccT": di["ccT"], "sel": di["sel"], "ropeO": ropeO, "xch": xch(l),
                  "xown": xown_t, "ctx": ctx_t, "yo": yo_t, "yc": yc_t}
            for k in ("w_mod", "b_mod", "norm_mix", "w_in", "w_out", "w4Tf", "w4Tb", "a2f", "a2b", "rows"):
                dd[k] = di["%s%d" % (k, l)]
            emit_mixer(P, nc, Cs, banks, l == 0, LAM_INIT[l], dd)

    def ffn(l, tiles):
        with ExitStack() as st:
            Cs = Ctx(nc, st, "F%d_" % l)
            dd = {"ccT": di["ccT"], "w_mod": di["w_mod%d" % l], "b_mod": di["b_mod%d" % l],
                  "norm_ffn": di["norm_ffn%d" % l], "norm_f": di["norm_f"],
                  "wg": di["wg%d" % l], "wu": di["wu%d" % l], "wd": di["wd%d" % l], "wrT": di["wrT"]}
            emit_ffn(P, nc, Cs, banks, "dense" if l == 0 else "moe", tiles, dd, l == 1)

    if "m0" in stages:
      mixer(0, tl(di["xown"], "in", 16), tl(di["ctx"], "in", 2),
          tl(y_d if stages == ("m0",) else xmid0, "xmid0", 16), tl(xmid0, "xmid0", 2, 2048))
    t0 = [(xmid0[t * 128:(t + 1) * 128, :], "xmid0", x1own[t * 128:(t + 1) * 128, :], "x1own", 0) for t in range(16)]
    t0 += [(xmid0[2048 + i * 128:2048 + (i + 1) * 128, :], "xmid0", xc1[i * 128:(i + 1) * 128, :], "xc1", 1) for i in range(2)]
    if "f0" in stages:
        ffn(0, t0)
    if "m1" in stages:
      mixer(1, tl(x1own, "x1own", 16), tl(xc1, "xc1", 2), tl(xmid1, "xmid1", 16), [])
    t1 = [(xmid1[t * 128:(t + 1) * 128, :], "xmid1", y_d[t * 128:(t + 1) * 128, :], "yout", 0) for t in range(16)]
    if "f1" in stages:
        ffn(1, t1)
    P.emit()
    es.close()
    return nc


def _ccT(cb, cc):
    return np.ascontiguousarray(np.concatenate([cb.reshape(8, 128).T, cc.reshape(8, 128).T], axis=1), dtype=np.float32)


def _rope_tables():
    t = np.arange(4096)
    rows = (t // 64).astype(np.float64)
    cols = (t % 64).astype(np.float64)

    def tab(half):
        fr = 10000.0 ** (-np.arange(half, dtype=np.float64) / half)
        ang = np.concatenate([rows[:, None] * fr[None, :], cols[:, None] * fr[None, :]], axis=1)
        return np.cos(ang).astype(np.float32), np.sin(ang).astype(np.float32)
    cG, sG = tab(16)
    cD, sD = tab(8)
    return cG, sG, cD, sD


def core_inputs(inp, b, half, ropes):
    f = lambda a: np.ascontiguousarray(a, dtype=np.float32)
    cG, sG, cD, sD = ropes
    xb = inp["x"][b]
    if half == 0:
        oorder = np.arange(2048)
        ctxl = inp["ctx"][b]
        sel = np.array([0.0, 1.0], np.float32)
    else:
        oorder = np.arange(4095, 2047, -1)
        ctxl = inp["ctx"][b][::-1]
        sel = np.array([1.0, 0.0], np.float32)
    m = {"xown": f(xb[oorder]), "ctx": f(ctxl), "ccT": _ccT(inp["c"][b], inp["c_ctx"]),
         "sel": f(np.tile(sel[None, :], (128, 1))),
         "cosGo": f(cG[oorder]), "sinGo": f(sG[oorder]), "cosDo": f(cD[oorder]), "sinDo": f(sD[oorder]),
         "norm_f": f(inp["norm_f"][None, :]),
         "wg0": inp["ffn_gate"], "wu0": inp["ffn_up"], "wd0": inp["ffn_down"],
         "wg1": inp["moe_gate"][0], "wu1": inp["moe_up"][0], "wd1": inp["moe_down"][0],
         "wrT": f(inp["moe_router"][0].T)}
    for l in range(2):
        w_in = inp["w_in"][l]
        if half == 0:
            w4f, w4b = w_in[:, 768:784], w_in[:, 784:800]
            a2f, a2b = inp["gla_a2_f"][l], inp["gla_a2_b"][l]
            abf, abb = inp["gla_ab_f"][l], inp["gla_ab_b"][l]
        else:
            w4f, w4b = w_in[:, 784:800], w_in[:, 768:784]
            a2f, a2b = inp["gla_a2_b"][l], inp["gla_a2_f"][l]
            abf, abb = inp["gla_ab_b"][l], inp["gla_ab_f"][l]
        rows = np.concatenate([abf, abb, np.tile(inp["gla_norm"][l], 4), np.tile(inp["gqa_q_norm"][l], 8),
                               np.tile(inp["gqa_k_norm"][l], 2), np.tile(inp["diff_norm"][l], 4),
                               inp["diff_lam_q1"][l], inp["diff_lam_k1"][l], inp["diff_lam_q2"][l], inp["diff_lam_k2"][l],
                               np.zeros(512, np.float32)]).astype(np.float32)[None, :]
        m.update({"w_mod%d" % l: inp["w_mod"][l], "b_mod%d" % l: f(inp["b_mod"][l][None, :]),
                  "norm_mix%d" % l: f(inp["norm_mix"][l][None, :]), "norm_ffn%d" % l: f(inp["norm_ffn"][l][None, :]),
                  "w_in%d" % l: w_in, "w_out%d" % l: inp["w_out"][l], "w4Tf%d" % l: f(w4f.T), "w4Tb%d" % l: f(w4b.T),
                  "a2f%d" % l: f(a2f), "a2b%d" % l: f(a2b), "rows%d" % l: f(rows)})
    return m


def kernel(**inp):
    inp = {k: np.asarray(v) for k, v in inp.items()}
    ropes = _rope_tables()
    nc = build_fused()
    maps = [core_inputs(inp, c // 2, c % 2, ropes) for c in range(8)]
    res = run_bass_kernel_spmd(nc, maps, core_ids=list(range(8))).results
    Bn = inp["x"].shape[0]
    out = np.empty((Bn, 4096, D), np.float32)
    for c in range(8):
        b, half = c // 2, c % 2
        y = res[c]["y"]
        if half == 0:
            out[b, 0:2048] = y
        else:
            out[b, 2048:4096] = y[::-1]
    return out
```

```python
import math
from contextlib import ExitStack
import numpy as np
import concourse.bass as bass
import concourse.mybir as mybir
from concourse.bass_utils import run_bass_kernel_spmd

F32 = mybir.dt.float32
BF16 = mybir.dt.bfloat16
AF = mybir.ActivationFunctionType
ALU = mybir.AluOpType
AX = mybir.AxisListType

D = 1024
NSLOT = 8


class Prog:
    ENGS = ["pe", "act", "dve", "pool", "sp"]

    def __init__(self, nc, es):
        self.nc = nc
        self.es = es
        self.ncoll = 0
        self.stream = {e: [] for e in self.ENGS}
        self.cnt = {e: 0 for e in self.ENGS}
        self.known = {e: {} for e in self.ENGS}
        self.lastw = {}
        self.rds = {}
        self.dmacnt = {e: 0 for e in self.ENGS}
        self.sems = {}
        self.semmax = {}
        for e in ["pe", "act", "dve", "pool"]:
            self.sems[e] = es.enter_context(nc.semaphore("s_" + e))
        for q in ["sp", "act", "pool"]:
            for j in range(NSLOT):
                self.sems[(q, j)] = es.enter_context(nc.semaphore("d_%s_%d" % (q, j)))

    def _wait(self, eng, ev):
        if ev is None:
            return
        k, v = ev
        if k == eng and eng == "pe":
            return
        if self.known[eng].get(k, 0) >= v:
            return
        self.known[eng][k] = v
        self.stream[eng].append(("w", k, v))

    def _deps(self, eng, reads, writes):
        for r in reads:
            self._wait(eng, self.lastw.get(r))
        for w in writes:
            self._wait(eng, self.lastw.get(w))
            for k, v in self.rds.get(w, {}).items():
                self._wait(eng, (k, v))

    def _commit(self, ev, reads, writes):
        k, v = ev
        self.semmax[k] = max(self.semmax.get(k, 0), v)
        for r in reads:
            d = self.rds.setdefault(r, {})
            d[k] = max(d.get(k, 0), v)
        for w in writes:
            self.lastw[w] = ev
            self.rds[w] = {}

    def op(self, eng, fn, reads=(), writes=()):
        self._deps(eng, reads, writes)
        self.cnt[eng] += 1
        ev = (eng, self.cnt[eng])
        self.stream[eng].append(("c", fn))
        self._commit(ev, reads, writes)

    def dma(self, out, in_, reads=(), writes=(), q="sp", **kw):
        n = self.dmacnt[q]
        self.dmacnt[q] += 1
        slot = n % NSLOT
        val = 16 * (n // NSLOT + 1)
        if val > 16:
            self._wait(q, ((q, slot), val - 16))
        self._deps(q, reads, writes)
        self.stream[q].append(("d", out, in_, (q, slot), kw))
        self._commit(((q, slot), val), reads, writes)

    def coll(self, fn, reads=(), writes=()):
        key = ("cc", self.ncoll)
        self.ncoll += 1
        self.sems[key] = self.es.enter_context(self.nc.semaphore("cc_%d" % key[1]))
        self._deps("pool", reads, writes)
        self.stream["pool"].append(("x", fn, key))
        self._commit((key, 1), reads, writes)

    def barrier(self):
        for e in self.ENGS:
            for k, v in self.semmax.items():
                self._wait(e, (k, v))

    def mm(self, out, lhsT, rhs, start, stop, reads, writes, skip=False):
        if skip:
            self.op("pe", lambda e: e.matmul(out, lhsT=lhsT, rhs=rhs, start=start, stop=stop, skip_group_check=True),
                    reads, writes)
        else:
            self.op("pe", lambda e: e.matmul(out, lhsT=lhsT, rhs=rhs, start=start, stop=stop), reads, writes)

    def tr(self, out, in_, ident, reads, writes):
        self.op("pe", lambda e: e.transpose(out, in_, ident), reads, writes)

    def act(self, out, in_, func, reads, writes, bias=None, scale=None, accum_out=None):
        kw = {}
        if bias is not None:
            kw["bias"] = bias
        if scale is not None:
            kw["scale"] = scale
        if accum_out is not None:
            kw["accum_out"] = accum_out
        self.op("act", lambda e: e.activation(out, in_, func, **kw), reads, writes)

    def tt(self, eng, out, in0, in1, op, reads, writes):
        self.op(eng, lambda e: e.tensor_tensor(out, in0, in1, op), reads, writes)

    def ts(self, eng, out, in0, s1, s2, op0, op1, reads, writes, accum_out=None):
        if op1 is None:
            self.op(eng, lambda e: e.tensor_scalar(out, in0, s1, None, op0), reads, writes)
        elif accum_out is None:
            self.op(eng, lambda e: e.tensor_scalar(out, in0, s1, s2, op0, op1), reads, writes)
        else:
            self.op(eng, lambda e: e.tensor_scalar(out, in0, s1, s2, op0, op1, accum_out=accum_out), reads, writes)

    def stt(self, out, in0, scalar, in1, op0, op1, reads, writes):
        self.op("dve", lambda e: e.scalar_tensor_tensor(out, in0, scalar, in1, op0, op1), reads, writes)

    def cp(self, eng, out, in_, reads, writes):
        if eng == "act":
            self.op("act", lambda e: e.copy(out, in_), reads, writes)
        else:
            self.op(eng, lambda e: e.tensor_copy(out, in_), reads, writes)

    def memset(self, eng, ap, val, writes):
        self.op(eng, lambda e: e.memset(ap, val), (), writes)

    def emit(self):
        nc = self.nc
        self.barrier()
        sems = self.sems
        streams = self.stream

        def run(name, eng):
            for it in streams[name]:
                if it[0] == "w":
                    eng.wait_ge(sems[it[1]], it[2])
                elif it[0] == "c":
                    it[1](eng).then_inc(sems[name], 1)
                elif it[0] == "x":
                    it[1](eng).then_inc(sems[it[2]], 1)
                else:
                    eng.dma_start(out=it[1], in_=it[2], **it[4]).then_inc(sems[it[3]], 16)

        with nc.Block() as block:
            @block.tensor
            def _(e):
                run("pe", e)

            @block.scalar
            def _(e):
                run("act", e)

            @block.vector
            def _(e):
                run("dve", e)

            @block.gpsimd
            def _(e):
                run("pool", e)

            @block.sync
            def _(e):
                run("sp", e)


class Ctx:
    def __init__(self, nc, es, pfx=""):
        self.nc = nc
        self.es = es
        self.pfx = pfx

    def sb(self, name, shape, dt=F32):
        return self.es.enter_context(self.nc.sbuf_tensor(self.pfx + name, list(shape), dt))

    def ps(self, name, shape, dt=F32):
        return self.es.enter_context(self.nc.psum_tensor(name, list(shape), dt))

    def din(self, name, shape, dt=F32):
        return self.nc.dram_tensor(name, list(shape), dt, kind="ExternalInput").ap()

    def dout(self, name, shape, dt=F32):
        return self.nc.dram_tensor(name, list(shape), dt, kind="ExternalOutput").ap()


def emit_mod(P, C, banks, ccT_d, wmod_d, bmod_d, groups, rows_d, tag):
    nc = P.nc
    ng = len(groups)
    modB = C.sb(tag + "modB", [128, 2, ng, 1024])
    rowB = C.sb(tag + "rowB", [128, max(1, len(rows_d)), 1024])
    with ExitStack() as es2:
        C2 = Ctx(nc, es2, C.pfx)
        ccT = C2.sb(tag + "ccT", [128, 16])
        scT = C2.sb(tag + "scT", [128, 16])
        ones = C2.sb(tag + "ones", [128, 128])
        crep = C2.sb(tag + "crep", [128, 16, 128])
        wm = [C2.sb(tag + "wm%d" % i, [128, 8, 512]) for i in range(2)]
        rowt = C2.sb(tag + "rowt", [1, 1024])
        brow = C2.sb(tag + "brow", [1, 1024])
        P.dma(ccT[:], ccT_d, writes=[tag + "ccT"])
        P.act(scT[:], ccT[:], AF.Silu, [tag + "ccT"], [tag + "scT"])
        P.memset("pool", ones[:], 1.0, [tag + "ones"])
        for jc in range(16):
            P.ts("dve", crep[:, jc, :], ones[:], scT[:, jc:jc + 1], None, ALU.mult, None,
                 [tag + "ones", tag + "scT"], [tag + "crep"])
        bi = 0
        for ri, rd in enumerate(rows_d):
            P.dma(rowt[:], rd, writes=[tag + "rowt"])
            for hf in range(2):
                b = banks[bi % 2]
                bi += 1
                P.mm(b[0][:], ones[0:1, :], rowt[0:1, hf * 512:(hf + 1) * 512], True, True,
                     [tag + "ones", tag + "rowt"], [b[1]])
                P.cp("act", rowB[:, ri, hf * 512:(hf + 1) * 512], b[0][:], [b[1]], [tag + "rowB"])
        wv = wmod_d.rearrange("(c p) n -> p c n", p=128)
        li = 0
        for gi, g in enumerate(groups):
            P.dma(brow[:], bmod_d[0:1, g * 1024:(g + 1) * 1024], writes=[tag + "brow"])
            for hf in range(2):
                col0 = g * 1024 + hf * 512
                w = wm[li % 2]
                wk = tag + "wm%d" % (li % 2)
                li += 1
                P.dma(w[:], wv[:, :, col0:col0 + 512], writes=[wk])
                for j in range(2):
                    b = banks[bi % 2]
                    bi += 1
                    for c in range(8):
                        P.mm(b[0][:], crep[:, j * 8 + c, :], w[:, c, :], c == 0, False,
                             [tag + "crep", wk], [b[1]])
                    P.mm(b[0][:], ones[0:1, :], brow[0:1, hf * 512:(hf + 1) * 512], False, True,
                         [tag + "ones", tag + "brow"], [b[1]])
                    P.cp("act", modB[:, j, gi, hf * 512:(hf + 1) * 512], b[0][:], [b[1]], [tag + "modB"])
        P.barrier()
    return modB, rowB


def emit_rstd(P, ss, tag, n=D):
    P.ts("dve", ss[:, 1:2], ss[:, 0:1], 1.0 / n, 1e-6, ALU.mult, ALU.add, [tag + "ss"], [tag + "ss1"])
    P.act(ss[:, 3:4], ss[:, 1:2], AF.Ln, [tag + "ss1"], [tag + "ss3"])
    P.act(ss[:, 2:3], ss[:, 3:4], AF.Exp, [tag + "ss3"], [tag + "ss2"], scale=-0.5)


def emit_norm_mod(P, xt, xkey, G, S, gskeys, junk, ss, tmp, hf_out, hkey, tag):
    P.act(tmp[:], xt, AF.Square, [xkey], [tag + "tmp", tag + "ss"], accum_out=ss[:, 0:1])
    emit_rstd(P, ss, tag)
    P.stt(tmp[:], xt, ss[:, 2:3], G, ALU.mult, ALU.mult, [xkey, tag + "ss2"] + gskeys, [tag + "tmp"])
    P.tt("pool", hf_out, tmp[:], S, ALU.add, [tag + "tmp"] + gskeys, [hkey])


def emit_ffn(P, nc, C, banks, kind, tiles_spec, dd, final_norm):
    ntile = len(tiles_spec)
    tl = tiles_spec
    FF = 2816 if kind == "dense" else 3584
    NE = 1 if kind == "dense" else 8
    ccT_d, wmod_d, bmod_d, nffn_d, nf_d = dd["ccT"], dd["w_mod"], dd["b_mod"], dd["norm_ffn"], dd["norm_f"]
    wg_d, wu_d, wd_d = dd["wg"], dd["wu"], dd["wd"]
    wrT_d = dd.get("wrT")

    xall = C.sb("xall", [128, ntile, D])
    hT = C.sb("hT", [128, 8, ntile * 128], BF16)
    gateB = C.sb("gateB", [128, 2, D])
    nfB = C.sb("nfB", [128, D]) if final_norm else None
    comb = C.sb("comb", [128, ntile, 8])
    ss = C.sb("ss", [128, 4])
    with ExitStack() as e1:
        C1 = Ctx(nc, e1, C.pfx)
        modB, rowB = emit_mod(P, C1, banks, ccT_d, wmod_d, bmod_d, [3, 4, 5], [nffn_d, nf_d], "m_")
        for j in range(2):
            P.stt(modB[:, j, 1, :], modB[:, j, 1, :], 1.0, rowB[:, 0, :], ALU.add, ALU.mult,
                  ["m_modB", "m_rowB"], ["m_modB"])
            P.cp("pool", gateB[:, j, :], modB[:, j, 2, :], ["m_modB"], ["gateB"])
        if final_norm:
            P.cp("pool", nfB[:], rowB[:, 1, :], ["m_rowB"], ["nfB"])
        ident = C1.sb("ident", [128, 128])
        P.memset("pool", ident[:], 1.0, ["ident"])
        _asel(P, ident[:], "ident", [[-1, 128]], ALU.is_equal, 0, 1)
        tmp = C1.sb("tmp", [128, D])
        hf = C1.sb("hf", [128, D])
        if kind == "moe":
            junk = C1.sb("junk", [128, D])
            wrB = C1.sb("wrB", [128, 8, D])
            ones1 = C1.sb("ones1", [1, 128])
            P.memset("pool", ones1[:], 1.0, ["ones1"])
            for e_ in range(8):
                P.dma(junk[0:1, :], wrT_d[e_:e_ + 1, :], writes=["n_junk"])
                for h2 in range(2):
                    b = banks[h2]
                    P.mm(b[0][:], ones1[0:1, :], junk[0:1, h2 * 512:(h2 + 1) * 512], True, True,
                         ["ones1", "n_junk"], [b[1]])
                    P.cp("act", wrB[:, e_, h2 * 512:(h2 + 1) * 512], b[0][:], [b[1]], ["wrB"])
            logit = C1.sb("logit", [128, 8])
            top8 = C1.sb("top8", [128, 8])
            cb2 = C1.sb("cb2", [128, 8])
            rt = C1.sb("rt", [128, 8])
        for t in range(ntile):
            j = tl[t][4]
            xk = "xall%d" % t
            P.dma(xall[:, t, :], tl[t][0], reads=[tl[t][1]], writes=[xk])
            emit_norm_mod(P, xall[:, t, :], xk, modB[:, j, 1, :], modB[:, j, 0, :], ["m_modB"],
                          None, ss, tmp, hf[:], "hf", "n_")
            if kind == "moe":
                for e_ in range(8):
                    P.op("dve", (lambda e, e_=e_: e.scalar_tensor_tensor(
                        junk[:], hf[:], 1.0, wrB[:, e_, :], ALU.mult, ALU.mult, accum_out=logit[:, e_:e_ + 1])),
                        ["hf", "wrB"], ["n_junk", "logit"])
                P.op("dve", lambda e: e.max(top8[:], logit[:]), ["logit"], ["top8"])
                P.tt("dve", rt[:, 0:1], top8[:, 1:2], top8[:, 0:1], ALU.subtract, ["top8"], ["rt"])
                P.act(rt[:, 1:2], rt[:, 0:1], AF.Exp, ["rt"], ["rt"])
                P.ts("dve", rt[:, 2:3], rt[:, 1:2], 1.0, None, ALU.add, None, ["rt"], ["rt"])
                P.op("dve", lambda e: e.reciprocal(rt[:, 3:4], rt[:, 2:3]), ["rt"], ["rt"])
                P.tt("dve", rt[:, 4:5], rt[:, 1:2], rt[:, 3:4], ALU.mult, ["rt"], ["rt"])
                P.ts("dve", comb[:, t, :], logit[:], top8[:, 0:1], rt[:, 3:4], ALU.is_equal, ALU.mult,
                     ["logit", "top8", "rt"], ["comb"])
                P.ts("dve", cb2[:], logit[:], top8[:, 1:2], rt[:, 4:5], ALU.is_equal, ALU.mult,
                     ["logit", "top8", "rt"], ["cb2"])
                P.tt("dve", comb[:, t, :], comb[:, t, :], cb2[:], ALU.add, ["comb", "cb2"], ["comb"])
            for half in range(2):
                b = banks[half]
                for c4 in range(4):
                    c = half * 4 + c4
                    P.tr(b[0][:, c4 * 128:(c4 + 1) * 128], hf[:, c * 128:(c + 1) * 128], ident[:],
                         ["hf", "ident"], [b[1]])
                P.cp("act", hT[:, half * 4:(half + 1) * 4, t * 128:(t + 1) * 128],
                     b[0][:].rearrange("p (c t) -> p c t", c=4), [b[1]], ["hT"])
        P.barrier()
    with ExitStack() as e2:
        C2 = Ctx(nc, e2, C.pfx)
        stg = [C2.sb("stg%d" % i, [128, 2048]) for i in range(3)]
        wgb = [C2.sb("wgb%d" % i, [128, 8, 512], BF16) for i in range(2)]
        wub = [C2.sb("wub%d" % i, [128, 8, 512], BF16) for i in range(2)]
        dbf = [C2.sb("dbf%d" % i, [128, 4, D], BF16) for i in range(2)]
        actT = [C2.sb("actT%d" % i, [128, 4, 512], BF16) for i in range(2)]
        sg = [C2.sb("sg%d" % i, [128, 512], BF16) for i in range(2)]
        tmp2 = C2.sb("tmp2", [128, 2, 512])
        nst = [0]
        cnt = {"g": 0, "a": 0, "b": 0, "t": 0}

        def stage_cast(src3, dst3, dkey, eng):
            k = nst[0] % 3
            nst[0] += 1
            a, w = src3.shape[1], src3.shape[2]
            v = stg[k][:].rearrange("p (a w) -> p a w", a=a)
            P.dma(v, src3, writes=["stg%d" % k])
            P.cp(eng, dst3, v, ["stg%d" % k], [dkey])

        def prefetch(pi, grp=None):
            calls = []
            sc = lambda *a: calls.append(a)
            _prefetch_list(pi, sc)
            n = len(calls)
            sel_ = range(n) if grp is None else range((grp * n) // 3, ((grp + 1) * n) // 3)
            for i in sel_:
                stage_cast(*calls[i])

        def _prefetch_list(pi, stage_cast):
            e_, g0, gw = parts[pi]
            pb = pi % 2
            nch = gw // 128
            wgv = wg_d[e_].rearrange("(c p) n -> p c n", p=128)
            wuv = wu_d[e_].rearrange("(c p) n -> p c n", p=128)
            wdv = wd_d[e_].rearrange("(c p) n -> p c n", p=128)
            if gw == 512:
                for c0 in range(0, 8, 4):
                    stage_cast(wgv[:, c0:c0 + 4, g0:g0 + 512], wgb[pb][:, c0:c0 + 4, :], "wgb%d" % pb, "act")
                    stage_cast(wuv[:, c0:c0 + 4, g0:g0 + 512], wub[pb][:, c0:c0 + 4, :], "wub%d" % pb, "act")
            else:
                stage_cast(wgv[:, :, g0:g0 + 256], wgb[pb][:, :, 0:256], "wgb%d" % pb, "act")
                stage_cast(wuv[:, :, g0:g0 + 256], wub[pb][:, :, 0:256], "wub%d" % pb, "act")
            for o in range(0, nch, 2):
                c0 = g0 // 128 + o
                stage_cast(wdv[:, c0:c0 + 2, :], dbf[pb][:, o:o + 2, :], "dbf%d" % pb, "pool")

        parts = [(e_, g0, min(512, FF - g0)) for e_ in range(NE) for g0 in range(0, FF, 512)]
        blocks = [list(range(b0, min(ntile, b0 + 4))) for b0 in range(0, ntile, 4)]
        prefetch(0)
        for pi, (e_, g0, gw) in enumerate(parts):
            pb = pi % 2
            nch = gw // 128
            for bix, blk in enumerate(blocks):
                if pi + 1 < len(parts) and 1 <= bix <= 3:
                    prefetch(pi + 1, bix - 1)
                nbt = len(blk) * 128
                t0_ = blk[0] * 128
                ab = cnt["a"] % 2
                cnt["a"] += 1
                for jj in range(nch):
                    gi = cnt["g"] % 2
                    cnt["g"] += 1
                    bG = banks[2 * gi]
                    bU = banks[2 * gi + 1]
                    for c in range(8):
                        P.mm(bG[0][:, 0:nbt], wgb[pb][:, c, jj * 128:(jj + 1) * 128], hT[:, c, t0_:t0_ + nbt],
                             c == 0, c == 7, ["wgb%d" % pb, "hT"], [bG[1]])
                    for c in range(8):
                        P.mm(bU[0][:, 0:nbt], wub[pb][:, c, jj * 128:(jj + 1) * 128], hT[:, c, t0_:t0_ + nbt],
                             c == 0, c == 7, ["wub%d" % pb, "hT"], [bU[1]])
                    P.act(sg[gi][:, 0:nbt], bG[0][:, 0:nbt], AF.Silu, [bG[1]], ["sg%d" % gi])
                    P.tt("dve", actT[ab][:, jj, 0:nbt], sg[gi][:, 0:nbt], bU[0][:, 0:nbt], ALU.mult,
                         ["sg%d" % gi, bU[1]], ["actT%d" % ab])
                for ti, t in enumerate(blk):
                    j = tl[t][4]
                    for h2 in range(2):
                        bi = cnt["b"] % 4
                        cnt["b"] += 1
                        b = banks[4 + bi]
                        for jj in range(nch):
                            P.mm(b[0][:], actT[ab][:, jj, ti * 128:(ti + 1) * 128], dbf[pb][:, jj, h2 * 512:(h2 + 1) * 512],
                                 jj == 0, jj == nch - 1, ["actT%d" % ab, "dbf%d" % pb], [b[1]])
                        cs = slice(h2 * 512, (h2 + 1) * 512)
                        tk = cnt["t"] % 2
                        cnt["t"] += 1
                        if kind == "dense":
                            P.tt("dve", tmp2[:, tk, :], b[0][:], gateB[:, j, cs], ALU.mult, [b[1], "gateB"], ["tmp2_%d" % tk])
                        else:
                            P.stt(tmp2[:, tk, :], b[0][:], comb[:, t, e_:e_ + 1], gateB[:, j, cs], ALU.mult, ALU.mult,
                                  [b[1], "gateB", "comb"], ["tmp2_%d" % tk])
                        P.tt("pool", xall[:, t, cs], xall[:, t, cs], tmp2[:, tk, :], ALU.add,
                             ["tmp2_%d" % tk, "xall%d" % t], ["xall%d" % t])
        for t in range(ntile):
            xk = "xall%d" % t
            if final_norm:
                P.act(tmp2[:].rearrange("p a b -> p (a b)"), xall[:, t, :], AF.Square, [xk], ["tmp2_0", "tmp2_1", "n_ss"],
                      accum_out=ss[:, 0:1])
                emit_rstd(P, ss, "n_")
                P.stt(xall[:, t, :], xall[:, t, :], ss[:, 2:3], nfB[:], ALU.mult, ALU.mult,
                      [xk, "n_ss2", "nfB"], [xk])
            P.dma(tl[t][2], xall[:, t, :], reads=[xk], writes=[tl[t][3]])
        P.barrier()


NKT = 34


def _asel(P, t, key, pattern, op, base, cm):
    P.op("pool", lambda e: e.affine_select(out=t, in_=t, pattern=pattern, compare_op=op, fill=0.0,
                                            base=base, channel_multiplier=cm), [key], [key])


def emit_rope(P, eng, dst, src, cs, sn, H, hw, r1, r2, rkeys, skey, dkey):
    X = src.rearrange("p (h a b i) -> p h a b i", h=H, a=2, b=2, i=hw)
    Y = dst.rearrange("p (h a b i) -> p h a b i", h=H, a=2, b=2, i=hw)
    R1 = r1.rearrange("p (h a i) -> p h a i", h=H, a=2, i=hw)
    R2 = r2.rearrange("p (h a i) -> p h a i", h=H, a=2, i=hw)
    cb = cs.rearrange("p (a i) -> p a i", a=2).unsqueeze(1).to_broadcast([128, H, 2, hw])
    sb = sn.rearrange("p (a i) -> p a i", a=2).unsqueeze(1).to_broadcast([128, H, 2, hw])
    x1 = X[:, :, :, 0, :]
    x2 = X[:, :, :, 1, :]
    P.tt(eng, R1, x1, cb, ALU.mult, [skey] + rkeys, ["rp1"])
    P.tt(eng, R2, x2, sb, ALU.mult, [skey] + rkeys, ["rp2"])
    P.tt(eng, Y[:, :, :, 0, :], R1, R2, ALU.subtract, ["rp1", "rp2"], [dkey])
    P.tt(eng, R1, x1, sb, ALU.mult, [skey] + rkeys, ["rp1"])
    P.tt(eng, R2, x2, cb, ALU.mult, [skey] + rkeys, ["rp2"])
    P.tt(eng, Y[:, :, :, 1, :], R1, R2, ALU.add, ["rp1", "rp2"], [dkey])


def emit_mixer(P, nc, C, banks, need_ctx, lam_init, dd):
    debug = False
    ccT_d, wmod_d, bmod_d, nmix_d = dd["ccT"], dd["w_mod"], dd["b_mod"], dd["norm_mix"]
    win_d, wout_d = dd["w_in"], dd["w_out"]
    w4f_d, w4b_d, a2f_d, a2b_d, rows_d = dd["w4Tf"], dd["w4Tb"], dd["a2f"], dd["a2b"], dd["rows"]
    ropeO = dd["ropeO"]
    PS2 = banks[0][2]
    B = [b[0] for b in banks]
    BK = [b[1] for b in banks]

    modB, rowB0 = emit_mod(P, C, banks, ccT_d, wmod_d, bmod_d, [0, 1, 2], [nmix_d], "m_")
    for j in range(2):
        P.stt(modB[:, j, 1, :], modB[:, j, 1, :], 1.0, rowB0[:, 0, :], ALU.add, ALU.mult,
              ["m_modB", "m_rowB"], ["m_modB"])
    MK = ["m_modB"]

    ident = C.sb("ident", [128, 128])
    P.memset("pool", ident[:], 1.0, ["ident"])
    _asel(P, ident[:], "ident", [[-1, 128]], ALU.is_equal, 0, 1)
    msk = C.sb("msk", [128, 6, 128])
    for i in range(4):
        P.memset("pool", msk[:, i, :], -1.0 / 16.0, ["msk"])
    for i in range(4, 6):
        P.memset("pool", msk[:, i, :], 1.0, ["msk"])
    _asel(P, msk[:, 0, :], "msk", [[1, 128]], ALU.is_ge, 0, -1)
    _asel(P, msk[:, 1, :], "msk", [[-1, 128]], ALU.is_ge, 0, 1)
    _asel(P, msk[:, 2, :], "msk", [[-1, 128]], ALU.is_gt, 0, 1)
    _asel(P, msk[:, 3, :], "msk", [[1, 128]], ALU.is_gt, 0, -1)
    _asel(P, msk[:, 4, :], "msk", [[1, 128]], ALU.is_ge, 0, -1)
    _asel(P, msk[:, 5, :], "msk", [[-1, 128]], ALU.is_ge, 0, 1)
    bd = C.sb("bd", [128, 256])
    P.memset("pool", bd[:], 1.0, ["bd"])
    for h in range(4):
        _asel(P, bd[:, h * 64:(h + 1) * 64], "bd", [[0, 64]], ALU.is_ge, -32 * h, 1)
        _asel(P, bd[:, h * 64:(h + 1) * 64], "bd", [[0, 64]], ALU.is_ge, 32 * h + 31, -1)
    sel = C.sb("sel", [128, 4])
    P.dma(sel[:, 0:2], dd["sel"], writes=["sel"])
    negcol = C.sb("negcol", [128, 1])
    P.memset("pool", negcol[:], -1.0 / 16.0, ["negcol"])
    ones1 = C.sb("ones1", [1, 128])
    P.memset("pool", ones1[:], 1.0, ["ones1"])
    rB = C.sb("rB", [128, 1536])
    with ExitStack() as es0:
        rowS = Ctx(nc, es0, C.pfx).sb("rowS", [1, 2048])
        P.dma(rowS[:], rows_d, writes=["rowS"])
        for i in range(3):
            P.mm(B[i][:], ones1[0:1, :], rowS[0:1, i * 512:(i + 1) * 512], True, True, ["ones1", "rowS"], [BK[i]])
            P.cp("act", rB[:, i * 512:(i + 1) * 512], B[i][:], [BK[i]], ["rB"])
        P.barrier()
    abB = rB[:, 0:256]
    gnB = rB[:, 256:512]
    qnB = rB[:, 512:1024]
    knB = rB[:, 1024:1152]
    dnB = rB[:, 1152:1408]
    lmB = rB[:, 1408:1536]
    lam = C.sb("lam", [128, 8])
    junk = C.sb("junk", [128, 32])
    P.op("dve", lambda e: e.scalar_tensor_tensor(junk[:, 0:32], lmB[:, 0:32], 1.0, lmB[:, 32:64], ALU.mult, ALU.mult,
                                                  accum_out=lam[:, 0:1]), ["rB"], ["n_junk", "lam"])
    P.op("dve", lambda e: e.scalar_tensor_tensor(junk[:, 0:32], lmB[:, 64:96], 1.0, lmB[:, 96:128], ALU.mult, ALU.mult,
                                                  accum_out=lam[:, 1:2]), ["rB"], ["n_junk", "lam"])
    P.act(lam[:, 2:4], lam[:, 0:2], AF.Exp, ["lam"], ["lam2"])
    P.tt("dve", lam[:, 4:5], lam[:, 3:4], lam[:, 2:3], ALU.subtract, ["lam2"], ["lam3"])
    P.ts("dve", lam[:, 5:6], lam[:, 4:5], -lam_init, None, ALU.add, None, ["lam3"], ["nlam"])
    nlam = lam[:, 5:6]
    P.ts("dve", rB[:, 512:1024], rB[:, 512:1024], 0.125, None, ALU.mult, None, ["rB"], ["rB"])
    P.ts("dve", rB[:, 1152:1408], rB[:, 1152:1408], 1.0 - lam_init, None, ALU.mult, None, ["rB"], ["rB"])

    KT = C.sb("KT", [64, 2, NKT * 128], BF16)
    Vg = C.sb("Vg", [128, NKT, 2, 65], BF16)
    KTd = C.sb("KTd", [128, 2, NKT * 128], BF16)
    Vd = C.sb("Vd", [128, NKT, 4, 65], BF16)
    P.memset("pool", Vg[:, :, :, 64:65], 1.0, ["Vg"])
    P.memset("pool", Vd[:, :, :, 64:65], 1.0, ["Vd"])
    glaout = C.sb("glaout", [128, 18, 256], BF16)
    ofS = glaout
    xt = C.sb("xt", [128, D])
    tmp = C.sb("tmp", [128, D])
    hf = C.sb("hf", [128, D])
    ss = C.sb("ss", [128, 4])
    hT = C.sb("hT", [128, 8, 128], BF16)
    sq = C.sb("sq", [128, 512])
    s8 = C.sb("s8", [128, 32])
    fa = C.sb("fa", [128, 512])
    fb = C.sb("fb", [128, 512])
    r1 = C.sb("r1", [128, 256])
    r2 = C.sb("r2", [128, 256])
    cG = C.sb("cG", [128, 64])
    cD = C.sb("cD", [128, 32])
    wst = [None, None]

    def load_w(dst, dcol, src_d, scol, width):
        for o in range(0, width, 256):
            w = min(256, width - o)
            k = load_w.n % 2
            load_w.n += 1
            P.dma(wst[k][:, :, 0:w], src_d.rearrange("(c p) n -> p c n", p=128)[:, :, scol + o:scol + o + w],
                  writes=["wst%d" % k])
            P.cp("pool", dst[:, :, dcol + o:dcol + o + w], wst[k][:, :, 0:w], ["wst%d" % k], ["W"])
    load_w.n = 0

    def load_h(src, j, rope):
        src_ap, src_key = src
        P.dma(xt[:], src_ap, reads=[src_key], writes=["xt"])
        if rope is not None:
            tabs, rope_row0 = rope
            P.dma(cG[:, 0:32], tabs[0][rope_row0:rope_row0 + 128, :], writes=["cG"])
            P.dma(cG[:, 32:64], tabs[1][rope_row0:rope_row0 + 128, :], writes=["cG"])
            P.dma(cD[:, 0:16], tabs[2][rope_row0:rope_row0 + 128, :], writes=["cD"])
            P.dma(cD[:, 16:32], tabs[3][rope_row0:rope_row0 + 128, :], writes=["cD"])
        emit_norm_mod(P, xt[:], "xt", modB[:, j, 1, :], modB[:, j, 0, :], MK, junk, ss, tmp, hf[:], "hf", "n_")
        for half in range(2):
            b = banks[3 + half]
            for c4 in range(4):
                c = half * 4 + c4
                P.tr(b[0][:, c4 * 128:(c4 + 1) * 128], hf[:, c * 128:(c + 1) * 128], ident[:], ["hf", "ident"], [b[1]])
            P.cp("act", hT[:, half * 4:(half + 1) * 4, :], b[0][:].rearrange("p (c t) -> p c t", c=4), [b[1]], ["hT"])

    def head_rstd(src_ps, src_key, H, dh, out_s8):
        P.act(sq[:, 0:H * dh], src_ps, AF.Square, [src_key], ["sq"])
        P.op("dve", lambda e: e.tensor_reduce(out=s8[:, 8:8 + H], in_=sq[:, 0:H * dh].rearrange("p (h d) -> p h d", h=H),
                                               axis=AX.X, op=ALU.add), ["sq"], ["s8a"])
        P.ts("dve", s8[:, 16:16 + H], s8[:, 8:8 + H], 1.0 / dh, 1e-6, ALU.mult, ALU.add, ["s8a"], ["s8b"])
        P.act(s8[:, 24:24 + H], s8[:, 16:16 + H], AF.Ln, ["s8b"], ["s8c"])
        P.act(out_s8, s8[:, 24:24 + H], AF.Exp, ["s8c"], ["s8"], scale=-0.5)

    with ExitStack() as es1:
        C1 = Ctx(nc, es1, C.pfx)
        wi1 = C1.sb("wi1", [128, 8, 1536], BF16)
        esw = ExitStack()
        Cw = Ctx(nc, esw, C.pfx)
        wst[0] = Cw.sb("wst0", [128, 8, 256])
        wst[1] = Cw.sb("wst1", [128, 8, 256])
        w4 = Cw.sb("w4", [16, 2, D])
        a2 = Cw.sb("a2", [16, 2, 128])
        P.dma(w4[:, 0, :], w4f_d, writes=["w4"])
        P.dma(w4[:, 1, :], w4b_d, writes=["w4"])
        P.dma(a2[:, 0, :], a2f_d, writes=["a2"])
        P.dma(a2[:, 1, :], a2b_d, writes=["a2"])
        for dr in range(2):
            for c in range(8):
                b = banks[c % 2]
                P.mm(b[0][:, 0:128], w4[:, dr, c * 128:(c + 1) * 128], a2[:, dr, :], True, True, ["w4", "a2"], [b[1]])
                P.cp("act", wi1[:, c, 256 + dr * 128:256 + (dr + 1) * 128], b[0][:, 0:128], [b[1]], ["W"])
        load_w(wi1, 0, win_d, 0, 256)
        load_w(wi1, 512, win_d, 256, 256)
        load_w(wi1, 768, win_d, 1312, 256)
        load_w(wi1, 1024, win_d, 2080, 256)
        load_w(wi1, 1280, win_d, 1824, 256)
        P.barrier()
        esw.close()
        NST = 18
        qkB = C1.sb("qkB", [128, NST, 2, 128], BF16)
        qkF = C1.sb("qkF", [128, 2, 128], BF16)
        kdB = C1.sb("kdB", [128, NST, 128], BF16)
        kdF = C1.sb("kdF", [128, 128], BF16)
        vS = C1.sb("vS", [128, NST, 256], BF16)
        dec = C1.sb("dec", [128, NST, 2])
        zt = C1.sb("zt", [128, 256])
        sp = C1.sb("sp", [128, 256])
        Eq = C1.sb("Eq", [128, 256])
        Ek = C1.sb("Ek", [128, 256])
        Ed = C1.sb("Ed", [128, 256])
        qk = C1.sb("qk", [128, 256])
        qdki = C1.sb("qdki", [128, 4, 128])
        kim = C1.sb("kim", [128, 4, 128], BF16)
        AT = C1.sb("AT", [128, 4, 128], BF16)
        KVm = C1.sb("KVm", [128, 256])
        S = [C1.sb("S%d" % i, [128, 256]) for i in range(2)]
        Sb = [C1.sb("Sb%d" % i, [128, 256], BF16) for i in range(2)]
        og = C1.sb("og", [128, 256])
        for i in range(2):
            P.memset("pool", S[i][:], 0.0, ["S%d" % i])
            P.memset("pool", Sb[i][:], 0.0, ["Sb%d" % i])

        def proj1():
            for g in range(3):
                for c in range(8):
                    P.mm(B[g][:], hT[:, c, :], wi1[:, c, g * 512:(g + 1) * 512], c == 0, c == 7, ["hT", "W"], [BK[g]])

        def gla_prep(st, full):
            P.tt("dve", zt[:], B[0][:, 256:512], abB, ALU.add, [BK[0], "rB"], ["zt"])
            P.act(Eq[:], zt[:], AF.Exp, ["zt"], ["Eq"], scale=-1.0)
            P.act(sp[:], Eq[:], AF.Ln, ["Eq"], ["sp"], bias=1.0)
            b3 = B[5]
            P.mm(b3[:, 0:128], msk[:, 0, :], sp[:, 0:128], True, True, ["msk", "sp"], [BK[5]])
            P.mm(b3[:, 128:256], msk[:, 1, :], sp[:, 128:256], True, True, ["msk", "sp"], [BK[5]])
            P.mm(b3[:, 256:384], msk[:, 2, :], sp[:, 0:128], True, True, ["msk", "sp"], [BK[5]])
            P.mm(b3[:, 384:512], msk[:, 3 if full else 2, :], sp[:, 128:256], True, True, ["msk", "sp"], [BK[5]])
            P.mm(B[6][:, 0:1], sp[:, 0:128], negcol[:], True, True, ["sp", "negcol"], [BK[6]])
            P.mm(B[6][:, 1:2], sp[:, 128:256], negcol[:], True, True, ["sp", "negcol"], [BK[6]])
            P.act(dec[:, st, :], B[6][:, 0:2], AF.Exp, [BK[6]], ["dec"])
            P.act(Ed[:], b3[:, 256:512], AF.Exp, [BK[5]], ["Ed"])
            P.cp("act", qk[:], B[0][:, 0:256], [BK[0]], ["qk"])
            P.tt("dve", kdB[:, st, :], qk[:, 128:256], Ed[:, 128:256], ALU.mult, ["qk", "Ed"], ["kdB"])
            if full:
                P.tt("dve", kdF[:], qk[:, 128:256], Ed[:, 0:128], ALU.mult, ["qk", "Ed"], ["kdF"])
            P.cp("act", vS[:, st, :], B[1][:, 0:256], [BK[1]], ["vS"])
            if not full:
                return
            P.act(Eq[:], b3[:, 0:256], AF.Exp, [BK[5]], ["Eq"])
            P.act(Ek[:], b3[:, 0:256], AF.Exp, [BK[5]], ["Ek"], scale=-1.0)
            for dr in range(2):
                P.stt(qdki[:, 2 * dr, :], qk[:, 0:128], 32.0 ** -0.5, Eq[:, dr * 128:(dr + 1) * 128], ALU.mult, ALU.mult,
                      ["qk", "Eq"], ["qdki"])
                P.tt("dve", qdki[:, 2 * dr + 1, :], qk[:, 128:256], Ek[:, dr * 128:(dr + 1) * 128], ALU.mult,
                     ["qk", "Ek"], ["qdki"])
            for i in range(4):
                P.tr(B[7][:, i * 128:(i + 1) * 128], qdki[:, i, :], ident[:], ["qdki", "ident"], [BK[7]])
            P.cp("act", qkF[:], B[7][:, 0:256].rearrange("p (i t) -> p i t", i=2), [BK[7]], ["qkF"])
            P.cp("act", qkB[:, st, :, :], B[7][:, 256:512].rearrange("p (i t) -> p i t", i=2), [BK[7]], ["qkB"])

        def kv_prep(kidx, rope):
            head_rstd(B[1][:, 256:384], BK[1], 2, 64, s8[:, 0:2])
            P.tt("dve", fa[:, 0:128].rearrange("p (h d) -> p h d", h=2), B[1][:, 256:384].rearrange("p (h d) -> p h d", h=2),
                 s8[:, 0:2].unsqueeze(2).to_broadcast([128, 2, 64]), ALU.mult, [BK[1], "s8"], ["fa"])
            P.tt("pool", fb[:, 0:128], fa[:, 0:128], knB, ALU.mult, ["fa", "rB"], ["fb"])
            src = fb
            if rope:
                emit_rope(P, "pool", fa[:, 0:128], fb[:, 0:128], cG[:, 0:32], cG[:, 32:64], 2, 16,
                          r1[:, 0:64], r2[:, 0:64], ["cG"], "fb", "fa")
                src = fa
            skey = "fa" if rope else "fb"
            for h in range(2):
                P.tr(B[7][0:64, h * 128:(h + 1) * 128], src[:, h * 64:(h + 1) * 64], ident[:], [skey, "ident"], [BK[7]])
            P.cp("act", KT[:, :, kidx * 128:(kidx + 1) * 128], B[7][0:64, 0:256].rearrange("p (h t) -> p h t", h=2),
                 [BK[7]], ["KT"])
            P.cp("act", Vg[:, kidx, :, 0:64], B[1][:, 384:512].rearrange("p (h d) -> p h d", h=2), [BK[1]], ["Vg"])
            P.cp("act", Vd[:, kidx, :, 0:64], B[2][:, 0:256].rearrange("p (h d) -> p h d", h=4), [BK[2]], ["Vd"])
            P.cp("act", fb[:, 256:512], B[2][:, 256:512], [BK[2]], ["fb2"])
            src2, k2 = fb[:, 256:512], "fb2"
            if rope:
                emit_rope(P, "pool", fa[:, 256:512], fb[:, 256:512], cD[:, 0:16], cD[:, 16:32], 8, 8,
                          r1[:, 0:128], r2[:, 0:128], ["cD"], "fb2", "fa2")
                src2, k2 = fa[:, 256:512], "fa2"
            for g in range(2):
                P.tr(B[7][:, 256 + g * 128:256 + (g + 1) * 128], src2[:, g * 128:(g + 1) * 128], ident[:],
                     [k2, "ident"], [BK[7]])
            P.cp("act", KTd[:, :, kidx * 128:(kidx + 1) * 128], B[7][:, 256:512].rearrange("p (g t) -> p g t", g=2),
                 [BK[7]], ["KTd"])

        def scan_step(dr, st, with_out, final_to=None, slot=None):
            Sd, Sbd = S[dr], Sb[dr]
            sk, sbk = "S%d" % dr, "Sb%d" % dr
            if dr == 0:
                qd_, ki_, kd_, qkk, kdk = qkF[:, 0, :], qkF[:, 1, :], kdF[:], "qkF", "kdF"
            else:
                qd_, ki_, kd_, qkk, kdk = qkB[:, st, 0, :], qkB[:, st, 1, :], kdB[:, st, :], "qkB", "kdB"
            if with_out:
                for h in range(4):
                    P.ts("pool", kim[:, h, :], ki_, bd[:, 64 * h:64 * h + 1], None, ALU.mult, None,
                         [qkk, "bd"], ["kim"])
                for h in range(4):
                    P.mm(B[5][:, h * 128:(h + 1) * 128], kim[:, h, :], qd_, True, True, ["kim", qkk], [BK[5]])
                P.tt("dve", AT[:], B[5][:].rearrange("p (h c) -> p h c", h=4),
                     msk[:, 4 + dr, :].unsqueeze(1).to_broadcast([128, 4, 128]), ALU.mult, [BK[5], "msk"], ["AT"])
                for h in range(4):
                    P.mm(B[7][:, h * 64:(h + 1) * 64], AT[:, h, :], vS[:, st, h * 64:(h + 1) * 64], True, False,
                         ["AT", "vS"], [BK[7]])
                    P.mm(B[7][:, h * 64:(h + 1) * 64], qd_, Sbd[:, h * 64:(h + 1) * 64], False, True,
                         [qkk, sbk], [BK[7]])
                if final_to is None:
                    P.cp("act", ofS[:, st, :], B[7][:, 0:256], [BK[7]], ["ofS"])
                else:
                    P.tt("dve", og[:], B[7][:, 0:256], ofS[:, st, :], ALU.add, [BK[7], "ofS"], ["og"])
                    P.act(sq[:, 0:256], og[:], AF.Square, ["og"], ["sq"])
                    P.op("dve", lambda e: e.tensor_reduce(out=s8[:, 8:12], in_=sq[:, 0:256].rearrange("p (h d) -> p h d", h=4),
                                                           axis=AX.X, op=ALU.add), ["sq"], ["s8a"])
                    P.ts("dve", s8[:, 16:20], s8[:, 8:12], 1.0 / 64, 1e-6, ALU.mult, ALU.add, ["s8a"], ["s8b"])
                    P.act(s8[:, 24:28], s8[:, 16:20], AF.Ln, ["s8b"], ["s8c"])
                    P.act(s8[:, 0:4], s8[:, 24:28], AF.Exp, ["s8c"], ["s8"], scale=-0.5)
                    P.tt("dve", og[:].rearrange("p (h d) -> p h d", h=4), og[:].rearrange("p (h d) -> p h d", h=4),
                         s8[:, 0:4].unsqueeze(2).to_broadcast([128, 4, 64]), ALU.mult, ["og", "s8"], ["og"])
                    P.tt("pool", glaout[:, final_to, :], og[:], gnB, ALU.mult, ["og", "rB"], ["glaout"])
            P.mm(B[6][:, 0:256], kd_, vS[:, st, :], True, True, [kdk, "vS"], [BK[6]])
            if slot is None:
                P.tt("dve", KVm[:], B[6][:, 0:256], bd[:], ALU.mult, [BK[6], "bd"], ["KVm"])
                P.stt(Sd[:], Sd[:], dec[:, st, dr:dr + 1], KVm[:], ALU.mult, ALU.add, [sk, "dec", "KVm"], [sk])
            else:
                sg_ = sel[:, slot:slot + 1]
                P.stt(KVm[:], B[6][:, 0:256], sg_, bd[:], ALU.mult, ALU.mult, [BK[6], "bd", "sel"], ["KVm"])
                P.ts("dve", sel[:, 2:3], dec[:, st, dr:dr + 1], -1.0, sg_, ALU.add, ALU.mult, ["dec", "sel"], ["sel2"])
                P.ts("dve", sel[:, 3:4], sel[:, 2:3], 1.0, None, ALU.add, None, ["sel2"], ["sel3"])
                P.stt(Sd[:], Sd[:], sel[:, 3:4], KVm[:], ALU.mult, ALU.add, [sk, "sel3", "KVm"], [sk])
            P.cp("act", Sbd[:], Sd[:], [sk], [sbk])

        for i in range(2):
            load_h(dd["ctx"][i], 1, None)
            proj1()
            gla_prep(16 + i, True)
            kv_prep(32 + i, False)
            scan_step(0, 16 + i, need_ctx, None)
        for i in (1, 0):
            scan_step(1, 16 + i, need_ctx, (16 + i) if need_ctx else None)
        for t in range(16):
            load_h(dd["xown"][t], 0, (ropeO, t * 128))
            proj1()
            gla_prep(t, True)
            kv_prep(t, True)
            scan_step(0, t, True, None)
        groups = [[0, 1], [2, 3], [4, 5], [6, 7]]
        X = dd["xch"]

        def allgather(src, dst, key):
            P.coll(lambda e: e.collective_compute("AllGather", ALU.bypass, replica_groups=groups,
                                                   ins=[src], outs=[dst]), [key + "_i"], [key + "_o"])
        P.dma(X["sx_i"], S[0][:], reads=["S0"], writes=["sx_i"])
        allgather(X["sx_i"], X["sx_o"], "sx")
        Sx = C1.sb("Sx", [128, 2, 256])
        P.dma(Sx[:], X["sx_o"].rearrange("(s p) n -> p s n", p=128), reads=["sx_o"], writes=["Sx"])
        P.ts("dve", KVm[:], Sx[:, 0, :], sel[:, 0:1], None, ALU.mult, None, ["Sx", "sel"], ["KVm"])
        P.stt(S[1][:], Sx[:, 1, :], sel[:, 1:2], KVm[:], ALU.mult, ALU.add, ["Sx", "sel", "KVm"], ["S1"])
        P.cp("act", Sb[1][:], S[1][:], ["S1"], ["Sb1"])
        P.dma(X["kt_i"].rearrange("p (k t) -> p k t", k=2), KT[:, :, 0:2048], reads=["KT"], writes=["kt_i"])
        P.dma(X["ktd_i"].rearrange("p (k t) -> p k t", k=2), KTd[:, :, 0:2048], reads=["KTd"], writes=["ktd_i"])
        P.dma(X["vg_i"].rearrange("p (t k d) -> p t k d", t=16, k=2), Vg[:, 0:16, :, :], reads=["Vg"], writes=["vg_i"])
        for hh in range(2):
            P.dma(X["vd_i%d" % hh].rearrange("p (t k d) -> p t k d", t=8, k=4), Vd[:, hh * 8:(hh + 1) * 8, :, :],
                  reads=["Vd"], writes=["vd%d_i" % hh])
        allgather(X["kt_i"], X["kt_o"], "kt")
        allgather(X["ktd_i"], X["ktd_o"], "ktd")
        allgather(X["vg_i"], X["vg_o"], "vg")
        for hh in range(2):
            allgather(X["vd_i%d" % hh], X["vd_o%d" % hh], "vd%d" % hh)
        for sl in range(2):
            P.dma(KT[:, :, sl * 2048:(sl + 1) * 2048], X["kt_o"][sl * 64:(sl + 1) * 64, :].rearrange("p (k t) -> p k t", k=2),
                  reads=["kt_o"], writes=["KT"])
            P.dma(KTd[:, :, sl * 2048:(sl + 1) * 2048], X["ktd_o"][sl * 128:(sl + 1) * 128, :].rearrange("p (k t) -> p k t", k=2),
                  reads=["ktd_o"], writes=["KTd"])
            P.dma(Vg[:, sl * 16:(sl + 1) * 16, :, :], X["vg_o"][sl * 128:(sl + 1) * 128, :].rearrange("p (t k d) -> p t k d", t=16, k=2),
                  reads=["vg_o"], writes=["Vg"])
            for hh in range(2):
                P.dma(Vd[:, sl * 16 + hh * 8:sl * 16 + (hh + 1) * 8, :, :],
                      X["vd_o%d" % hh][sl * 128:(sl + 1) * 128, :].rearrange("p (t k d) -> p t k d", t=8, k=4),
                      reads=["vd%d_o" % hh], writes=["Vd"])
        for t in range(15, -1, -1):
            scan_step(1, t, True, t)
        P.barrier()

    with ExitStack() as es2:
        C2 = Ctx(nc, es2, C.pfx)
        wi2 = C2.sb("wi2", [128, 8, 1024], BF16)
        wo = C2.sb("wo", [128, 8, 1024], BF16)
        with ExitStack() as esw2:
            Cw2 = Ctx(nc, esw2, C.pfx)
            wst[0] = Cw2.sb("wst0b", [128, 8, 256])
            wst[1] = Cw2.sb("wst1b", [128, 8, 256])
            load_w(wi2, 0, win_d, 800, 512)
            load_w(wi2, 512, win_d, 1568, 256)
            load_w(wi2, 768, win_d, 512, 256)
            load_w(wo, 0, wout_d, 0, 1024)
            P.barrier()
        QT2 = [C2.sb("QT%d" % i, [64, 8, 128], BF16) for i in range(2)]
        QTm2 = [C2.sb("QTm%d" % i, [128, 8, 128], BF16) for i in range(2)]
        cat2 = [C2.sb("cat%d" % i, [128, D]) for i in range(2)]
        xt2 = [xt, C2.sb("xtb", [128, D])]
        hT2 = [hT, C2.sb("hTb", [128, 8, 128], BF16)]
        QTd = C2.sb("QTd", [128, 2, 128], BF16)
        PT = [C2.sb("PT%d" % i, [128, 1024], BF16) for i in range(2)]
        catT = C2.sb("catT", [128, 8, 128], BF16)
        rs = C2.sb("rs", [128, 256])
        rc = C2.sb("rc", [128, 16])
        od = C2.sb("od", [128, 256])
        t0 = C2.sb("t0", [128, 64])
        xo = C2.sb("xo", [128, D])

        def prep_chunks(tile, p):
            (src_ap, src_key), j, rope_row0, gidx, keys, dst = tile
            xt_, hT_, QT_, QTm_, cat_ = xt2[p], hT2[p], QT2[p], QTm2[p], cat2[p]
            kx, kh, kq, kqm, kc = "xt%d" % p, "hT%d" % p, "QT%d" % p, "QTm%d" % p, "cat%d" % p
            rope = rope_row0 is not None

            def c0():
                P.dma(xt_[:], src_ap, reads=[src_key], writes=[kx])
                if rope:
                    P.dma(cG[:, 0:32], ropeO[0][rope_row0:rope_row0 + 128, :], writes=["cG"])
                    P.dma(cG[:, 32:64], ropeO[1][rope_row0:rope_row0 + 128, :], writes=["cG"])
                    P.dma(cD[:, 0:16], ropeO[2][rope_row0:rope_row0 + 128, :], writes=["cD"])
                    P.dma(cD[:, 16:32], ropeO[3][rope_row0:rope_row0 + 128, :], writes=["cD"])

            def c1():
                emit_norm_mod(P, xt_[:], kx, modB[:, j, 1, :], modB[:, j, 0, :], MK, None, ss, tmp, hf[:], "hf", "n_")

            def c2():
                for half in range(2):
                    b = banks[half]
                    for c4 in range(4):
                        c = half * 4 + c4
                        P.tr(b[0][:, c4 * 128:(c4 + 1) * 128], hf[:, c * 128:(c + 1) * 128], ident[:], ["hf", "ident"], [b[1]])
                    P.cp("act", hT_[:, half * 4:(half + 1) * 4, :], b[0][:].rearrange("p (c t) -> p c t", c=4), [b[1]], [kh])

            def c3():
                for g in range(2):
                    for c in range(8):
                        P.mm(B[g][:], hT_[:, c, :], wi2[:, c, g * 512:(g + 1) * 512], c == 0, c == 7, [kh, "W"], [BK[g]])

            def c4():
                head_rstd(B[0][:], BK[0], 8, 64, s8[:, 0:8])
                P.tt("dve", fa[:].rearrange("p (h d) -> p h d", h=8), B[0][:].rearrange("p (h d) -> p h d", h=8),
                     s8[:, 0:8].unsqueeze(2).to_broadcast([128, 8, 64]), ALU.mult, [BK[0], "s8"], ["fa"])
                P.tt("pool", fb[:], fa[:], qnB, ALU.mult, ["fa", "rB"], ["fb"])
                if rope:
                    emit_rope(P, "pool", fa[:], fb[:], cG[:, 0:32], cG[:, 32:64], 8, 16, r1[:], r2[:], ["cG"], "fb", "fa")
                P.act(sq[:, 0:256], B[1][:, 0:256], AF.Copy, [BK[1]], ["sq"], scale=32.0 ** -0.5)
                if rope:
                    emit_rope(P, "pool", sq[:, 256:512], sq[:, 0:256], cD[:, 0:16], cD[:, 16:32], 8, 8,
                              r1[:, 0:128], r2[:, 0:128], ["cD"], "sq", "sqr")
                P.act(rs[:], B[1][:, 256:512], AF.Silu, [BK[1]], ["rs"])
                P.tt("dve", cat_[:, 0:256], glaout[:, gidx, :], rs[:], ALU.mult, ["glaout", "rs"], [kc])

            def c5():
                src, skey = (fa, "fa") if rope else (fb, "fb")
                for hh in range(2):
                    b = banks[hh]
                    for h4 in range(4):
                        h = hh * 4 + h4
                        P.tr(b[0][0:64, h4 * 128:(h4 + 1) * 128], src[:, h * 64:(h + 1) * 64], ident[:], [skey, "ident"], [b[1]])
                    P.cp("act", QT_[:, hh * 4:(hh + 1) * 4, :], b[0][0:64, :].rearrange("p (h t) -> p h t", h=4), [b[1]], [kq])
                src2, k2 = (sq[:, 256:512], "sqr") if rope else (sq[:, 0:256], "sq")
                for g in range(2):
                    P.tr(B[0][:, g * 128:(g + 1) * 128], src2[:, g * 128:(g + 1) * 128], ident[:], [k2, "ident"], [BK[0]])
                P.cp("act", QTd[:], B[0][:, 0:256].rearrange("p (g t) -> p g t", g=2), [BK[0]], ["QTd"])
                for m in range(8):
                    P.ts("pool", QTm_[:, m, :], QTd[:, m // 4, :], bd[:, 64 * (m % 4):64 * (m % 4) + 1], None, ALU.mult, None,
                         ["QTd", "bd"], [kqm])
            return [c0, c1, c2, c3, c4, c5]

        SCHED = {1: 0, 6: 1, 15: 2, 21: 3, 23: 4, 46: 5}

        def attention(tile, p, nxt):
            (src_ap, src_key), j, rope_row0, gidx, keys, dst = tile
            QT_, QTm_, cat_ = QT2[p], QTm2[p], cat2[p]
            kq, kqm, kc = "QT%d" % p, "QTm%d" % p, "cat%d" % p
            done = [0]

            def gqa_post(kv):
                ob = banks[6 + kv]
                for g in range(4):
                    h = kv * 4 + g
                    P.op("dve", lambda e, g=g, ob=ob, h=h: e.reciprocal(rc[:, h:h + 1], ob[0][:, g * 65 + 64:g * 65 + 65]),
                         [ob[1]], ["rc"])
                    P.ts("dve", cat_[:, 256 + h * 64:256 + (h + 1) * 64], ob[0][:, g * 65:g * 65 + 64], rc[:, h:h + 1], None,
                         ALU.mult, None, [ob[1], "rc"], [kc])

            def diff_post(gg):
                ob = banks[6 + gg]
                for hh in range(2):
                    h = gg * 2 + hh
                    m0, m1 = 2 * hh, 2 * hh + 1
                    P.op("dve", lambda e, ob=ob, m0=m0: e.reciprocal(rc[:, 8:9], ob[0][:, m0 * 65 + 64:m0 * 65 + 65]),
                         [ob[1]], ["rc"])
                    P.op("dve", lambda e, ob=ob, m1=m1: e.reciprocal(rc[:, 9:10], ob[0][:, m1 * 65 + 64:m1 * 65 + 65]),
                         [ob[1]], ["rc"])
                    P.tt("dve", rc[:, 10:11], rc[:, 9:10], nlam, ALU.mult, ["rc", "nlam"], ["rc"])
                    P.ts("dve", t0[:], ob[0][:, m0 * 65:m0 * 65 + 64], rc[:, 8:9], None, ALU.mult, None, [ob[1], "rc"], ["t0"])
                    P.stt(od[:, h * 64:(h + 1) * 64], ob[0][:, m1 * 65:m1 * 65 + 64], rc[:, 10:11], t0[:], ALU.mult, ALU.add,
                          [ob[1], "rc", "t0"], ["od"])

            npair = len(keys) // 2
            items = [(typ, grp, pi_, (keys[2 * pi_], keys[2 * pi_ + 1]))
                     for typ in range(2) for grp in range(2) for pi_ in range(npair)]

            def emit_score(ix):
                typ, grp, pi_, pr = items[ix]
                for hx in range(2):
                    sbk = banks[2 + 2 * (ix % 2) + hx]
                    s = pr[hx]
                    if typ == 0:
                        P.mm(sbk[0][:], KT[:, grp, s * 128:(s + 1) * 128], QT_[:, grp * 4:(grp + 1) * 4, :], True, True,
                             ["KT", kq], [sbk[1]])
                    else:
                        P.mm(sbk[0][:], KTd[:, grp, s * 128:(s + 1) * 128], QTm_[:, grp * 4:(grp + 1) * 4, :], True, True,
                             ["KTd", kqm], [sbk[1]])

            def emit_rest(ix):
                typ, grp, pi_, pr = items[ix]
                k0, k1 = "bank%d" % (2 + 2 * (ix % 2)), "bank%d" % (3 + 2 * (ix % 2))
                pt = PT[ix % 2]
                ptk = "PT%d" % (ix % 2)
                ob = banks[6 + grp]
                P.act(pt[:], PS2[1 + ix % 2][:], AF.Exp, [k0, k1], [ptk])
                for hx in range(2):
                    s = pr[hx]
                    for g in range(4):
                        if typ == 0:
                            rhs, vk = Vg[:, s, grp, :], "Vg"
                        else:
                            rhs, vk = Vd[:, s, (grp * 4 + g) // 2, :], "Vd"
                        P.mm(ob[0][:, g * 65:(g + 1) * 65], pt[:, hx * 512 + g * 128:hx * 512 + (g + 1) * 128], rhs,
                             pi_ == 0 and hx == 0 and g == 0, pi_ == npair - 1 and hx == 1, [ptk, vk], [ob[1]], skip=True)
                if pi_ == npair - 1:
                    if typ == 0:
                        gqa_post(grp)
                    else:
                        diff_post(grp)

            emit_score(0)
            for ix in range(len(items)):
                if ix + 1 < len(items):
                    emit_score(ix + 1)
                emit_rest(ix)
                if ix in SCHED and SCHED[ix] == done[0] and done[0] < len(nxt):
                    nxt[done[0]]()
                    done[0] += 1
            while done[0] < len(nxt):
                nxt[done[0]]()
                done[0] += 1

        def post(tile, p):
            (src_ap, src_key), j, rope_row0, gidx, keys, dst = tile
            xt_, cat_ = xt2[p], cat2[p]
            kx, kc = "xt%d" % p, "cat%d" % p
            head_rstd(od[:], "od", 4, 64, s8[:, 0:4])
            P.tt("dve", od[:].rearrange("p (h d) -> p h d", h=4), od[:].rearrange("p (h d) -> p h d", h=4),
                 s8[:, 0:4].unsqueeze(2).to_broadcast([128, 4, 64]), ALU.mult, ["od", "s8"], ["od"])
            P.tt("pool", cat_[:, 768:1024], od[:], dnB, ALU.mult, ["od", "rB"], [kc])
            for half in range(2):
                b = banks[half]
                for c4 in range(4):
                    c = half * 4 + c4
                    P.tr(b[0][:, c4 * 128:(c4 + 1) * 128], cat_[:, c * 128:(c + 1) * 128], ident[:], [kc, "ident"], [b[1]])
                P.cp("act", catT[:, half * 4:(half + 1) * 4, :], b[0][:].rearrange("p (c t) -> p c t", c=4), [b[1]], ["catT"])
            for h2 in range(2):
                b = banks[h2]
                for c in range(8):
                    P.mm(b[0][:], catT[:, c, :], wo[:, c, h2 * 512:(h2 + 1) * 512], c == 0, c == 7, ["catT", "W"], [b[1]])
                cs = slice(h2 * 512, (h2 + 1) * 512)
                P.tt("dve", tmp[:, cs], b[0][:], modB[:, j, 2, cs], ALU.mult, [b[1]] + MK, ["n_tmp"])
                P.tt("pool", xo[:, cs], xt_[:, cs], tmp[:, cs], ALU.add, ["n_tmp", kx], ["xo"])
            P.dma(dst[0], xo[:], reads=["xo"], writes=[dst[1]])

        tiles = [(dd["xown"][t], 0, t * 128, t, list(range(NKT)), dd["yo"][t]) for t in range(16)]
        if need_ctx:
            tiles += [(dd["ctx"][i], 1, None, 16 + i, [32, 33], dd["yc"][i]) for i in range(2)]
        for ch in prep_chunks(tiles[0], 0):
            ch()
        for i, tile in enumerate(tiles):
            nxt = prep_chunks(tiles[i + 1], (i + 1) % 2) if i + 1 < len(tiles) else []
            attention(tile, i % 2, nxt)
            post(tile, i % 2)
        P.barrier()


LAM_INIT = [0.8 - 0.6 * math.exp(-0.3 * l) for l in range(2)]


def build_fused(stages=("m0", "f0", "cc", "m1", "f1")):
    nc = bass.Bass("TRN2", target_bir_lowering=False)
    es = ExitStack()
    C = Ctx(nc, es)
    P = Prog(nc, es)
    di = {}

    def din(name, shape):
        di[name] = C.din(name, shape)
        return di[name]

    din("xown", [2048, D]); din("ctx", [256, D]); din("ccT", [128, 16]); din("sel", [128, 2])
    ropeO = [din("cosGo", [2048, 32]), din("sinGo", [2048, 32]), din("cosDo", [2048, 16]), din("sinDo", [2048, 16])]
    for l in range(2):
        din("w_mod%d" % l, [D, 6 * D]); din("b_mod%d" % l, [1, 6 * D])
        din("norm_mix%d" % l, [1, D]); din("norm_ffn%d" % l, [1, D])
        din("w_in%d" % l, [D, 2336]); din("w_out%d" % l, [D, D])
        din("w4Tf%d" % l, [16, D]); din("w4Tb%d" % l, [16, D]); din("a2f%d" % l, [16, 128]); din("a2b%d" % l, [16, 128])
        din("rows%d" % l, [1, 2048])
    din("norm_f", [1, D])
    din("wg0", [1, D, 2816]); din("wu0", [1, D, 2816]); din("wd0", [1, 2816, D])
    din("wg1", [8, D, 3584]); din("wu1", [8, D, 3584]); din("wd1", [8, 3584, D]); din("wrT", [8, D])
    y_d = C.dout("y", [2048, D])
    xmid0 = nc.dram_tensor("xmid0", [2304, D], F32, kind="Internal").ap()
    x1own = nc.dram_tensor("x1own", [2048, D], F32, kind="Internal").ap()

    def xch(l):
        def t(name, shape, dt):
            return nc.dram_tensor("x%d_%s" % (l, name), shape, dt, kind="Internal").ap()
        return {"sx_i": t("sx_i", [128, 256], F32), "sx_o": t("sx_o", [256, 256], F32),
                "kt_i": t("kt_i", [64, 4096], BF16), "kt_o": t("kt_o", [128, 4096], BF16),
                "ktd_i": t("ktd_i", [128, 4096], BF16), "ktd_o": t("ktd_o", [256, 4096], BF16),
                "vg_i": t("vg_i", [128, 2080], BF16), "vg_o": t("vg_o", [256, 2080], BF16),
                "vd_i0": t("vd_i0", [128, 2080], BF16), "vd_o0": t("vd_o0", [256, 2080], BF16),
                "vd_i1": t("vd_i1", [128, 2080], BF16), "vd_o1": t("vd_o1", [256, 2080], BF16)}
    xc1 = nc.dram_tensor("xc1", [256, D], F32, kind="Internal").ap()
    xmid1 = nc.dram_tensor("xmid1", [2048, D], F32, kind="Internal").ap()
    PS2 = [C.ps("psd%d" % i, [128, 1024]) for i in range(4)]
    banks = [(PS2[i // 2][:, (i % 2) * 512:(i % 2 + 1) * 512], "bank%d" % i) for i in range(8)]
    banks[0] = banks[0] + (PS2,)

    def tl(ap, key, n, off=0):
        return [(ap[off + t * 128:off + (t + 1) * 128, :], key) for t in range(n)]

    def mixer(l, xown_t, ctx_t, yo_t, yc_t):
        with ExitStack() as st:
            Cs = Ctx(nc, st, "M%d_" % l)
            dd = {"ccT": di["ccT"], "sel": di["sel"], "ropeO": ropeO, "xch": xch(l),
                  "xown": xown_t, "ctx": ctx_t, "yo": yo_t, "yc": yc_t}
            for k in ("w_mod", "b_mod", "norm_mix", "w_in", "w_out", "w4Tf", "w4Tb", "a2f", "a2b", "rows"):
                dd[k] = di["%s%d" % (k, l)]
            emit_mixer(P, nc, Cs, banks, l == 0, LAM_INIT[l], dd)

    def ffn(l, tiles):
        with ExitStack() as st:
            Cs = Ctx(nc, st, "F%d_" % l)
            dd = {"ccT": di["ccT"], "w_mod": di["w_mod%d" % l], "b_mod": di["b_mod%d" % l],
                  "norm_ffn": di["norm_ffn%d" % l], "norm_f": di["norm_f"],
                  "wg": di["wg%d" % l], "wu": di["wu%d" % l], "wd": di["wd%d" % l], "wrT": di["wrT"]}
            emit_ffn(P, nc, Cs, banks, "dense" if l == 0 else "moe", tiles, dd, l == 1)

    if "m0" in stages:
      mixer(0, tl(di["xown"], "in", 16), tl(di["ctx"], "in", 2),
          tl(y_d if stages == ("m0",) else xmid0, "xmid0", 16), tl(xmid0, "xmid0", 2, 2048))
    t0 = [(xmid0[t * 128:(t + 1) * 128, :], "xmid0", x1own[t * 128:(t + 1) * 128, :], "x1own", 0) for t in range(16)]
    t0 += [(xmid0[2048 + i * 128:2048 + (i + 1) * 128, :], "xmid0", xc1[i * 128:(i + 1) * 128, :], "xc1", 1) for i in range(2)]
    if "f0" in stages:
        ffn(0, t0)
    if "m1" in stages:
      mixer(1, tl(x1own, "x1own", 16), tl(xc1, "xc1", 2), tl(xmid1, "xmid1", 16), [])
    t1 = [(xmid1[t * 128:(t + 1) * 128, :], "xmid1", y_d[t * 128:(t + 1) * 128, :], "yout", 0) for t in range(16)]
    if "f1" in stages:
        ffn(1, t1)
    P.emit()
    es.close()
    return nc


def _ccT(cb, cc):
    return np.ascontiguousarray(np.concatenate([cb.reshape(8, 128).T, cc.reshape(8, 128).T], axis=1), dtype=np.float32)


def _rope_tables():
    t = np.arange(4096)
    rows = (t // 64).astype(np.float64)
    cols = (t % 64).astype(np.float64)

    def tab(half):
        fr = 10000.0 ** (-np.arange(half, dtype=np.float64) / half)
        ang = np.concatenate([rows[:, None] * fr[None, :], cols[:, None] * fr[None, :]], axis=1)
        return np.cos(ang).astype(np.float32), np.sin(ang).astype(np.float32)
    cG, sG = tab(16)
    cD, sD = tab(8)
    return cG, sG, cD, sD


def core_inputs(inp, b, half, ropes):
    f = lambda a: np.ascontiguousarray(a, dtype=np.float32)
    cG, sG, cD, sD = ropes
    xb = inp["x"][b]
    if half == 0:
        oorder = np.arange(2048)
        ctxl = inp["ctx"][b]
        sel = np.array([0.0, 1.0], np.float32)
    else:
        oorder = np.arange(4095, 2047, -1)
        ctxl = inp["ctx"][b][::-1]
        sel = np.array([1.0, 0.0], np.float32)
    m = {"xown": f(xb[oorder]), "ctx": f(ctxl), "ccT": _ccT(inp["c"][b], inp["c_ctx"]),
         "sel": f(np.tile(sel[None, :], (128, 1))),
         "cosGo": f(cG[oorder]), "sinGo": f(sG[oorder]), "cosDo": f(cD[oorder]), "sinDo": f(sD[oorder]),
         "norm_f": f(inp["norm_f"][None, :]),
         "wg0": inp["ffn_gate"], "wu0": inp["ffn_up"], "wd0": inp["ffn_down"],
         "wg1": inp["moe_gate"][0], "wu1": inp["moe_up"][0], "wd1": inp["moe_down"][0],
         "wrT": f(inp["moe_router"][0].T)}
    for l in range(2):
        w_in = inp["w_in"][l]
        if half == 0:
            w4f, w4b = w_in[:, 768:784], w_in[:, 784:800]
            a2f, a2b = inp["gla_a2_f"][l], inp["gla_a2_b"][l]
            abf, abb = inp["gla_ab_f"][l], inp["gla_ab_b"][l]
        else:
            w4f, w4b = w_in[:, 784:800], w_in[:, 768:784]
            a2f, a2b = inp["gla_a2_b"][l], inp["gla_a2_f"][l]
            abf, abb = inp["gla_ab_b"][l], inp["gla_ab_f"][l]
        rows = np.concatenate([abf, abb, np.tile(inp["gla_norm"][l], 4), np.tile(inp["gqa_q_norm"][l], 8),
                               np.tile(inp["gqa_k_norm"][l], 2), np.tile(inp["diff_norm"][l], 4),
                               inp["diff_lam_q1"][l], inp["diff_lam_k1"][l], inp["diff_lam_q2"][l], inp["diff_lam_k2"][l],
                               np.zeros(512, np.float32)]).astype(np.float32)[None, :]
        m.update({"w_mod%d" % l: inp["w_mod"][l], "b_mod%d" % l: f(inp["b_mod"][l][None, :]),
                  "norm_mix%d" % l: f(inp["norm_mix"][l][None, :]), "norm_ffn%d" % l: f(inp["norm_ffn"][l][None, :]),
                  "w_in%d" % l: w_in, "w_out%d" % l: inp["w_out"][l], "w4Tf%d" % l: f(w4f.T), "w4Tb%d" % l: f(w4b.T),
                  "a2f%d" % l: f(a2f), "a2b%d" % l: f(a2b), "rows%d" % l: f(rows)})
    return m


def kernel(**inp):
    inp = {k: np.asarray(v) for k, v in inp.items()}
    ropes = _rope_tables()
    nc = build_fused()
    maps = [core_inputs(inp, c // 2, c % 2, ropes) for c in range(8)]
    res = run_bass_kernel_spmd(nc, maps, core_ids=list(range(8))).results
    Bn = inp["x"].shape[0]
    out = np.empty((Bn, 4096, D), np.float32)
    for c in range(8):
        b, half = c // 2, c % 2
        y = res[c]["y"]
        if half == 0:
            out[b, 0:2048] = y
        else:
            out[b, 2048:4096] = y[::-1]
    return out
```

```python
import math
from contextlib import ExitStack
import numpy as np
import concourse.bass as bass
import concourse.mybir as mybir
from concourse.bass_utils import run_bass_kernel_spmd

F32 = mybir.dt.float32
BF16 = mybir.dt.bfloat16
AF = mybir.ActivationFunctionType
ALU = mybir.AluOpType
AX = mybir.AxisListType

D = 1024
NSLOT = 8


class Prog:
    ENGS = ["pe", "act", "dve", "pool", "sp"]

    def __init__(self, nc, es):
        self.nc = nc
        self.es = es
        self.ncoll = 0
        self.stream = {e: [] for e in self.ENGS}
        self.cnt = {e: 0 for e in self.ENGS}
        self.known = {e: {} for e in self.ENGS}
        self.lastw = {}
        self.rds = {}
        self.dmacnt = {e: 0 for e in self.ENGS}
        self.sems = {}
        self.semmax = {}
        for e in ["pe", "act", "dve", "pool"]:
            self.sems[e] = es.enter_context(nc.semaphore("s_" + e))
        for q in ["sp", "act", "pool"]:
            for j in range(NSLOT):
                self.sems[(q, j)] = es.enter_context(nc.semaphore("d_%s_%d" % (q, j)))

    def _wait(self, eng, ev):
        if ev is None:
            return
        k, v = ev
        if k == eng and eng == "pe":
            return
        if self.known[eng].get(k, 0) >= v:
            return
        self.known[eng][k] = v
        self.stream[eng].append(("w", k, v))

    def _deps(self, eng, reads, writes):
        for r in reads:
            self._wait(eng, self.lastw.get(r))
        for w in writes:
            self._wait(eng, self.lastw.get(w))
            for k, v in self.rds.get(w, {}).items():
                self._wait(eng, (k, v))

    def _commit(self, ev, reads, writes):
        k, v = ev
        self.semmax[k] = max(self.semmax.get(k, 0), v)
        for r in reads:
            d = self.rds.setdefault(r, {})
            d[k] = max(d.get(k, 0), v)
        for w in writes:
            self.lastw[w] = ev
            self.rds[w] = {}

    def op(self, eng, fn, reads=(), writes=()):
        self._deps(eng, reads, writes)
        self.cnt[eng] += 1
        ev = (eng, self.cnt[eng])
        self.stream[eng].append(("c", fn))
        self._commit(ev, reads, writes)

    def dma(self, out, in_, reads=(), writes=(), q="sp", **kw):
        n = self.dmacnt[q]
        self.dmacnt[q] += 1
        slot = n % NSLOT
        val = 16 * (n // NSLOT + 1)
        if val > 16:
            self._wait(q, ((q, slot), val - 16))
        self._deps(q, reads, writes)
        self.stream[q].append(("d", out, in_, (q, slot), kw))
        self._commit(((q, slot), val), reads, writes)

    def coll(self, fn, reads=(), writes=()):
        key = ("cc", self.ncoll)
        self.ncoll += 1
        self.sems[key] = self.es.enter_context(self.nc.semaphore("cc_%d" % key[1]))
        self._deps("pool", reads, writes)
        self.stream["pool"].append(("x", fn, key))
        self._commit((key, 1), reads, writes)

    def barrier(self):
        for e in self.ENGS:
            for k, v in self.semmax.items():
                self._wait(e, (k, v))

    def mm(self, out, lhsT, rhs, start, stop, reads, writes, skip=False):
        if skip:
            self.op("pe", lambda e: e.matmul(out, lhsT=lhsT, rhs=rhs, start=start, stop=stop, skip_group_check=True),
                    reads, writes)
        else:
            self.op("pe", lambda e: e.matmul(out, lhsT=lhsT, rhs=rhs, start=start, stop=stop), reads, writes)

    def tr(self, out, in_, ident, reads, writes):
        self.op("pe", lambda e: e.transpose(out, in_, ident), reads, writes)

    def act(self, out, in_, func, reads, writes, bias=None, scale=None, accum_out=None):
        kw = {}
        if bias is not None:
            kw["bias"] = bias
        if scale is not None:
            kw["scale"] = scale
        if accum_out is not None:
            kw["accum_out"] = accum_out
        self.op("act", lambda e: e.activation(out, in_, func, **kw), reads, writes)

    def tt(self, eng, out, in0, in1, op, reads, writes):
        self.op(eng, lambda e: e.tensor_tensor(out, in0, in1, op), reads, writes)

    def ts(self, eng, out, in0, s1, s2, op0, op1, reads, writes, accum_out=None):
        if op1 is None:
            self.op(eng, lambda e: e.tensor_scalar(out, in0, s1, None, op0), reads, writes)
        elif accum_out is None:
            self.op(eng, lambda e: e.tensor_scalar(out, in0, s1, s2, op0, op1), reads, writes)
        else:
            self.op(eng, lambda e: e.tensor_scalar(out, in0, s1, s2, op0, op1, accum_out=accum_out), reads, writes)

    def stt(self, out, in0, scalar, in1, op0, op1, reads, writes):
        self.op("dve", lambda e: e.scalar_tensor_tensor(out, in0, scalar, in1, op0, op1), reads, writes)

    def cp(self, eng, out, in_, reads, writes):
        if eng == "act":
            self.op("act", lambda e: e.copy(out, in_), reads, writes)
        else:
            self.op(eng, lambda e: e.tensor_copy(out, in_), reads, writes)

    def memset(self, eng, ap, val, writes):
        self.op(eng, lambda e: e.memset(ap, val), (), writes)

    def prune(self):
        comp = ("pe", "act", "dve", "pool")
        need = {e: set() for e in comp}
        for e in self.ENGS:
            for it in self.stream[e]:
                if it[0] == "w" and it[1] in need:
                    need[it[1]].add(it[2])
        rank = {e: {n: i + 1 for i, n in enumerate(sorted(need[e]))} for e in comp}
        out = {}
        for e in self.ENGS:
            lst = []
            idx = 0
            for it in self.stream[e]:
                if it[0] == "w" and it[1] in rank:
                    lst.append(("w", it[1], rank[it[1]][it[2]]))
                elif it[0] == "c":
                    idx += 1
                    lst.append(("c", it[1], idx in need[e]))
                else:
                    lst.append(it)
            out[e] = lst
        self.pruned = out
        return out

    def emit(self):
        nc = self.nc
        self.barrier()
        sems = self.sems
        streams = self.prune()

        def run(name, eng):
            for it in streams[name]:
                if it[0] == "w":
                    eng.wait_ge(sems[it[1]], it[2])
                elif it[0] == "c":
                    ins = it[1](eng)
                    if it[2]:
                        ins.then_inc(sems[name], 1)
                elif it[0] == "x":
                    it[1](eng).then_inc(sems[it[2]], 1)
                else:
                    eng.dma_start(out=it[1], in_=it[2], **it[4]).then_inc(sems[it[3]], 16)

        with nc.Block() as block:
            @block.tensor
            def _(e):
                run("pe", e)

            @block.scalar
            def _(e):
                run("act", e)

            @block.vector
            def _(e):
                run("dve", e)

            @block.gpsimd
            def _(e):
                run("pool", e)

            @block.sync
            def _(e):
                run("sp", e)


class Ctx:
    def __init__(self, nc, es, pfx=""):
        self.nc = nc
        self.es = es
        self.pfx = pfx

    def sb(self, name, shape, dt=F32):
        return self.es.enter_context(self.nc.sbuf_tensor(self.pfx + name, list(shape), dt))

    def ps(self, name, shape, dt=F32):
        return self.es.enter_context(self.nc.psum_tensor(name, list(shape), dt))

    def din(self, name, shape, dt=F32):
        return self.nc.dram_tensor(name, list(shape), dt, kind="ExternalInput").ap()

    def dout(self, name, shape, dt=F32):
        return self.nc.dram_tensor(name, list(shape), dt, kind="ExternalOutput").ap()


def emit_mod(P, C, banks, ccT_d, wmod_d, bmod_d, groups, rows_d, tag):
    nc = P.nc
    ng = len(groups)
    modB = C.sb(tag + "modB", [128, 2, ng, 1024])
    rowB = C.sb(tag + "rowB", [128, max(1, len(rows_d)), 1024])
    with ExitStack() as es2:
        C2 = Ctx(nc, es2, C.pfx)
        ccT = C2.sb(tag + "ccT", [128, 16])
        scT = C2.sb(tag + "scT", [128, 16])
        ones = C2.sb(tag + "ones", [128, 128])
        crep = C2.sb(tag + "crep", [128, 16, 128])
        wm = [C2.sb(tag + "wm%d" % i, [128, 8, 512]) for i in range(2)]
        rowt = C2.sb(tag + "rowt", [1, 1024])
        brow = C2.sb(tag + "brow", [1, 1024])
        P.dma(ccT[:], ccT_d, writes=[tag + "ccT"])
        P.act(scT[:], ccT[:], AF.Silu, [tag + "ccT"], [tag + "scT"])
        P.memset("pool", ones[:], 1.0, [tag + "ones"])
        for jc in range(16):
            P.ts("dve", crep[:, jc, :], ones[:], scT[:, jc:jc + 1], None, ALU.mult, None,
                 [tag + "ones", tag + "scT"], [tag + "crep"])
        bi = 0
        for ri, rd in enumerate(rows_d):
            P.dma(rowt[:], rd, writes=[tag + "rowt"])
            for hf in range(2):
                b = banks[bi % 2]
                bi += 1
                P.mm(b[0][:], ones[0:1, :], rowt[0:1, hf * 512:(hf + 1) * 512], True, True,
                     [tag + "ones", tag + "rowt"], [b[1]])
                P.cp("act", rowB[:, ri, hf * 512:(hf + 1) * 512], b[0][:], [b[1]], [tag + "rowB"])
        wv = wmod_d.rearrange("(c p) n -> p c n", p=128)
        li = 0
        for gi, g in enumerate(groups):
            P.dma(brow[:], bmod_d[0:1, g * 1024:(g + 1) * 1024], writes=[tag + "brow"])
            for hf in range(2):
                col0 = g * 1024 + hf * 512
                w = wm[li % 2]
                wk = tag + "wm%d" % (li % 2)
                li += 1
                P.dma(w[:], wv[:, :, col0:col0 + 512], writes=[wk])
                for j in range(2):
                    b = banks[bi % 2]
                    bi += 1
                    for c in range(8):
                        P.mm(b[0][:], crep[:, j * 8 + c, :], w[:, c, :], c == 0, False,
                             [tag + "crep", wk], [b[1]])
                    P.mm(b[0][:], ones[0:1, :], brow[0:1, hf * 512:(hf + 1) * 512], False, True,
                         [tag + "ones", tag + "brow"], [b[1]])
                    P.cp("act", modB[:, j, gi, hf * 512:(hf + 1) * 512], b[0][:], [b[1]], [tag + "modB"])
        P.barrier()
    return modB, rowB


def emit_rstd(P, ss, tag, n=D):
    P.ts("dve", ss[:, 1:2], ss[:, 0:1], 1.0 / n, 1e-6, ALU.mult, ALU.add, [tag + "ss"], [tag + "ss1"])
    P.act(ss[:, 3:4], ss[:, 1:2], AF.Ln, [tag + "ss1"], [tag + "ss3"])
    P.act(ss[:, 2:3], ss[:, 3:4], AF.Exp, [tag + "ss3"], [tag + "ss2"], scale=-0.5)


def emit_norm_mod(P, xt, xkey, G, S, gskeys, junk, ss, tmp, hf_out, hkey, tag):
    P.act(tmp[:], xt, AF.Square, [xkey], [tag + "tmp", tag + "ss"], accum_out=ss[:, 0:1])
    emit_rstd(P, ss, tag)
    P.stt(tmp[:], xt, ss[:, 2:3], G, ALU.mult, ALU.mult, [xkey, tag + "ss2"] + gskeys, [tag + "tmp"])
    P.tt("pool", hf_out, tmp[:], S, ALU.add, [tag + "tmp"] + gskeys, [hkey])


def emit_ffn(P, nc, C, banks, kind, tiles_spec, dd, final_norm):
    ntile = len(tiles_spec)
    tl = tiles_spec
    FF = 2816 if kind == "dense" else 3584
    NE = 1 if kind == "dense" else 8
    ccT_d, wmod_d, bmod_d, nffn_d, nf_d = dd["ccT"], dd["w_mod"], dd["b_mod"], dd["norm_ffn"], dd["norm_f"]
    wg_d, wu_d, wd_d = dd["wg"], dd["wu"], dd["wd"]
    wrT_d = dd.get("wrT")

    xall = C.sb("xall", [128, ntile, D])
    hT = C.sb("hT", [128, 8, ntile * 128], BF16)
    gateB = C.sb("gateB", [128, 2, D])
    nfB = C.sb("nfB", [128, D]) if final_norm else None
    comb = C.sb("comb", [128, ntile, 8])
    ss = C.sb("ss", [128, 4])
    with ExitStack() as e1:
        C1 = Ctx(nc, e1, C.pfx)
        modB, rowB = emit_mod(P, C1, banks, ccT_d, wmod_d, bmod_d, [3, 4, 5], [nffn_d, nf_d], "m_")
        for j in range(2):
            P.stt(modB[:, j, 1, :], modB[:, j, 1, :], 1.0, rowB[:, 0, :], ALU.add, ALU.mult,
                  ["m_modB", "m_rowB"], ["m_modB"])
            P.cp("pool", gateB[:, j, :], modB[:, j, 2, :], ["m_modB"], ["gateB"])
        if final_norm:
            P.cp("pool", nfB[:], rowB[:, 1, :], ["m_rowB"], ["nfB"])
        ident = C1.sb("ident", [128, 128])
        P.memset("pool", ident[:], 1.0, ["ident"])
        _asel(P, ident[:], "ident", [[-1, 128]], ALU.is_equal, 0, 1)
        tmp = C1.sb("tmp", [128, D])
        hf = C1.sb("hf", [128, D])
        if kind == "moe":
            junk = C1.sb("junk", [128, D])
            wrB = C1.sb("wrB", [128, 8, D])
            ones1 = C1.sb("ones1", [1, 128])
            P.memset("pool", ones1[:], 1.0, ["ones1"])
            for e_ in range(8):
                P.dma(junk[0:1, :], wrT_d[e_:e_ + 1, :], writes=["n_junk"])
                for h2 in range(2):
                    b = banks[h2]
                    P.mm(b[0][:], ones1[0:1, :], junk[0:1, h2 * 512:(h2 + 1) * 512], True, True,
                         ["ones1", "n_junk"], [b[1]])
                    P.cp("act", wrB[:, e_, h2 * 512:(h2 + 1) * 512], b[0][:], [b[1]], ["wrB"])
            logit = C1.sb("logit", [128, 8])
            top8 = C1.sb("top8", [128, 8])
            cb2 = C1.sb("cb2", [128, 8])
            rt = C1.sb("rt", [128, 8])
        for t in range(ntile):
            j = tl[t][4]
            xk = "xall%d" % t
            P.dma(xall[:, t, :], tl[t][0], reads=[tl[t][1]], writes=[xk])
            emit_norm_mod(P, xall[:, t, :], xk, modB[:, j, 1, :], modB[:, j, 0, :], ["m_modB"],
                          None, ss, tmp, hf[:], "hf", "n_")
            if kind == "moe":
                for e_ in range(8):
                    P.op("dve", (lambda e, e_=e_: e.scalar_tensor_tensor(
                        junk[:], hf[:], 1.0, wrB[:, e_, :], ALU.mult, ALU.mult, accum_out=logit[:, e_:e_ + 1])),
                        ["hf", "wrB"], ["n_junk", "logit"])
                P.op("dve", lambda e: e.max(top8[:], logit[:]), ["logit"], ["top8"])
                P.tt("dve", rt[:, 0:1], top8[:, 1:2], top8[:, 0:1], ALU.subtract, ["top8"], ["rt"])
                P.act(rt[:, 1:2], rt[:, 0:1], AF.Exp, ["rt"], ["rt"])
                P.ts("dve", rt[:, 2:3], rt[:, 1:2], 1.0, None, ALU.add, None, ["rt"], ["rt"])
                P.op("dve", lambda e: e.reciprocal(rt[:, 3:4], rt[:, 2:3]), ["rt"], ["rt"])
                P.tt("dve", rt[:, 4:5], rt[:, 1:2], rt[:, 3:4], ALU.mult, ["rt"], ["rt"])
                P.ts("dve", comb[:, t, :], logit[:], top8[:, 0:1], rt[:, 3:4], ALU.is_equal, ALU.mult,
                     ["logit", "top8", "rt"], ["comb"])
                P.ts("dve", cb2[:], logit[:], top8[:, 1:2], rt[:, 4:5], ALU.is_equal, ALU.mult,
                     ["logit", "top8", "rt"], ["cb2"])
                P.tt("dve", comb[:, t, :], comb[:, t, :], cb2[:], ALU.add, ["comb", "cb2"], ["comb"])
            for half in range(2):
                b = banks[half]
                for c4 in range(4):
                    c = half * 4 + c4
                    P.tr(b[0][:, c4 * 128:(c4 + 1) * 128], hf[:, c * 128:(c + 1) * 128], ident[:],
                         ["hf", "ident"], [b[1]])
                P.cp("act", hT[:, half * 4:(half + 1) * 4, t * 128:(t + 1) * 128],
                     b[0][:].rearrange("p (c t) -> p c t", c=4), [b[1]], ["hT"])
        P.barrier()
    with ExitStack() as e2:
        C2 = Ctx(nc, e2, C.pfx)
        stg = [C2.sb("stg%d" % i, [128, 2048]) for i in range(3)]
        wgb = [C2.sb("wgb%d" % i, [128, 8, 512], BF16) for i in range(2)]
        wub = [C2.sb("wub%d" % i, [128, 8, 512], BF16) for i in range(2)]
        dbf = [C2.sb("dbf%d" % i, [128, 4, D], BF16) for i in range(2)]
        actT = [C2.sb("actT%d" % i, [128, 4, 512], BF16) for i in range(2)]
        sg = [C2.sb("sg%d" % i, [128, 512], BF16) for i in range(2)]
        tmp2 = C2.sb("tmp2", [128, 2, 512])
        nst = [0]
        cnt = {"g": 0, "a": 0, "b": 0, "t": 0}

        def stage_cast(src3, dst3, dkey, eng):
            k = nst[0] % 3
            nst[0] += 1
            a, w = src3.shape[1], src3.shape[2]
            v = stg[k][:].rearrange("p (a w) -> p a w", a=a)
            P.dma(v, src3, writes=["stg%d" % k])
            P.cp(eng, dst3, v, ["stg%d" % k], [dkey])

        def prefetch(pi, grp=None):
            calls = []
            sc = lambda *a: calls.append(a)
            _prefetch_list(pi, sc)
            n = len(calls)
            sel_ = range(n) if grp is None else range((grp * n) // 3, ((grp + 1) * n) // 3)
            for i in sel_:
                stage_cast(*calls[i])

        def _prefetch_list(pi, stage_cast):
            e_, g0, gw = parts[pi]
            pb = pi % 2
            nch = gw // 128
            wgv = wg_d[e_].rearrange("(c p) n -> p c n", p=128)
            wuv = wu_d[e_].rearrange("(c p) n -> p c n", p=128)
            wdv = wd_d[e_].rearrange("(c p) n -> p c n", p=128)
            if gw == 512:
                for c0 in range(0, 8, 4):
                    stage_cast(wgv[:, c0:c0 + 4, g0:g0 + 512], wgb[pb][:, c0:c0 + 4, :], "wgb%d" % pb, "act")
                    stage_cast(wuv[:, c0:c0 + 4, g0:g0 + 512], wub[pb][:, c0:c0 + 4, :], "wub%d" % pb, "act")
            else:
                stage_cast(wgv[:, :, g0:g0 + 256], wgb[pb][:, :, 0:256], "wgb%d" % pb, "act")
                stage_cast(wuv[:, :, g0:g0 + 256], wub[pb][:, :, 0:256], "wub%d" % pb, "act")
            for o in range(0, nch, 2):
                c0 = g0 // 128 + o
                stage_cast(wdv[:, c0:c0 + 2, :], dbf[pb][:, o:o + 2, :], "dbf%d" % pb, "pool")

        parts = [(e_, g0, min(512, FF - g0)) for e_ in range(NE) for g0 in range(0, FF, 512)]
        blocks = [list(range(b0, min(ntile, b0 + 4))) for b0 in range(0, ntile, 4)]
        prefetch(0)
        for pi, (e_, g0, gw) in enumerate(parts):
            pb = pi % 2
            nch = gw // 128
            for bix, blk in enumerate(blocks):
                if pi + 1 < len(parts) and 1 <= bix <= 3:
                    prefetch(pi + 1, bix - 1)
                nbt = len(blk) * 128
                t0_ = blk[0] * 128
                ab = cnt["a"] % 2
                cnt["a"] += 1
                for jj in range(nch):
                    gi = cnt["g"] % 2
                    cnt["g"] += 1
                    bG = banks[2 * gi]
                    bU = banks[2 * gi + 1]
                    for c in range(8):
                        P.mm(bG[0][:, 0:nbt], wgb[pb][:, c, jj * 128:(jj + 1) * 128], hT[:, c, t0_:t0_ + nbt],
                             c == 0, c == 7, ["wgb%d" % pb, "hT"], [bG[1]])
                    for c in range(8):
                        P.mm(bU[0][:, 0:nbt], wub[pb][:, c, jj * 128:(jj + 1) * 128], hT[:, c, t0_:t0_ + nbt],
                             c == 0, c == 7, ["wub%d" % pb, "hT"], [bU[1]])
                    P.act(sg[gi][:, 0:nbt], bG[0][:, 0:nbt], AF.Silu, [bG[1]], ["sg%d" % gi])
                    P.tt("dve", actT[ab][:, jj, 0:nbt], sg[gi][:, 0:nbt], bU[0][:, 0:nbt], ALU.mult,
                         ["sg%d" % gi, bU[1]], ["actT%d" % ab])
                for ti, t in enumerate(blk):
                    j = tl[t][4]
                    for h2 in range(2):
                        bi = cnt["b"] % 4
                        cnt["b"] += 1
                        b = banks[4 + bi]
                        for jj in range(nch):
                            P.mm(b[0][:], actT[ab][:, jj, ti * 128:(ti + 1) * 128], dbf[pb][:, jj, h2 * 512:(h2 + 1) * 512],
                                 jj == 0, jj == nch - 1, ["actT%d" % ab, "dbf%d" % pb], [b[1]])
                        cs = slice(h2 * 512, (h2 + 1) * 512)
                        tk = cnt["t"] % 2
                        cnt["t"] += 1
                        if kind == "dense":
                            P.tt("dve", tmp2[:, tk, :], b[0][:], gateB[:, j, cs], ALU.mult, [b[1], "gateB"], ["tmp2_%d" % tk])
                        else:
                            P.stt(tmp2[:, tk, :], b[0][:], comb[:, t, e_:e_ + 1], gateB[:, j, cs], ALU.mult, ALU.mult,
                                  [b[1], "gateB", "comb"], ["tmp2_%d" % tk])
                        P.tt("pool", xall[:, t, cs], xall[:, t, cs], tmp2[:, tk, :], ALU.add,
                             ["tmp2_%d" % tk, "xall%d" % t], ["xall%d" % t])
        for t in range(ntile):
            xk = "xall%d" % t
            if final_norm:
                P.act(tmp2[:].rearrange("p a b -> p (a b)"), xall[:, t, :], AF.Square, [xk], ["tmp2_0", "tmp2_1", "n_ss"],
                      accum_out=ss[:, 0:1])
                emit_rstd(P, ss, "n_")
                P.stt(xall[:, t, :], xall[:, t, :], ss[:, 2:3], nfB[:], ALU.mult, ALU.mult,
                      [xk, "n_ss2", "nfB"], [xk])
            P.dma(tl[t][2], xall[:, t, :], reads=[xk], writes=[tl[t][3]])
        P.barrier()


NKT = 34


def _asel(P, t, key, pattern, op, base, cm):
    P.op("pool", lambda e: e.affine_select(out=t, in_=t, pattern=pattern, compare_op=op, fill=0.0,
                                            base=base, channel_multiplier=cm), [key], [key])


def emit_rope(P, eng, dst, src, cs, sn, H, hw, r1, r2, rkeys, skey, dkey):
    X = src.rearrange("p (h a b i) -> p h a b i", h=H, a=2, b=2, i=hw)
    Y = dst.rearrange("p (h a b i) -> p h a b i", h=H, a=2, b=2, i=hw)
    R1 = r1.rearrange("p (h a i) -> p h a i", h=H, a=2, i=hw)
    R2 = r2.rearrange("p (h a i) -> p h a i", h=H, a=2, i=hw)
    cb = cs.rearrange("p (a i) -> p a i", a=2).unsqueeze(1).to_broadcast([128, H, 2, hw])
    sb = sn.rearrange("p (a i) -> p a i", a=2).unsqueeze(1).to_broadcast([128, H, 2, hw])
    x1 = X[:, :, :, 0, :]
    x2 = X[:, :, :, 1, :]
    P.tt(eng, R1, x1, cb, ALU.mult, [skey] + rkeys, ["rp1"])
    P.tt(eng, R2, x2, sb, ALU.mult, [skey] + rkeys, ["rp2"])
    P.tt(eng, Y[:, :, :, 0, :], R1, R2, ALU.subtract, ["rp1", "rp2"], [dkey])
    P.tt(eng, R1, x1, sb, ALU.mult, [skey] + rkeys, ["rp1"])
    P.tt(eng, R2, x2, cb, ALU.mult, [skey] + rkeys, ["rp2"])
    P.tt(eng, Y[:, :, :, 1, :], R1, R2, ALU.add, ["rp1", "rp2"], [dkey])


def emit_mixer(P, nc, C, banks, need_ctx, lam_init, dd):
    debug = False
    ccT_d, wmod_d, bmod_d, nmix_d = dd["ccT"], dd["w_mod"], dd["b_mod"], dd["norm_mix"]
    win_d, wout_d = dd["w_in"], dd["w_out"]
    w4f_d, w4b_d, a2f_d, a2b_d, rows_d = dd["w4Tf"], dd["w4Tb"], dd["a2f"], dd["a2b"], dd["rows"]
    ropeO = dd["ropeO"]
    PS2 = banks[0][2]
    B = [b[0] for b in banks]
    BK = [b[1] for b in banks]

    modB, rowB0 = emit_mod(P, C, banks, ccT_d, wmod_d, bmod_d, [0, 1, 2], [nmix_d], "m_")
    for j in range(2):
        P.stt(modB[:, j, 1, :], modB[:, j, 1, :], 1.0, rowB0[:, 0, :], ALU.add, ALU.mult,
              ["m_modB", "m_rowB"], ["m_modB"])
    MK = ["m_modB"]

    ident = C.sb("ident", [128, 128])
    P.memset("pool", ident[:], 1.0, ["ident"])
    _asel(P, ident[:], "ident", [[-1, 128]], ALU.is_equal, 0, 1)
    msk = C.sb("msk", [128, 6, 128])
    for i in range(4):
        P.memset("pool", msk[:, i, :], -1.0 / 16.0, ["msk"])
    for i in range(4, 6):
        P.memset("pool", msk[:, i, :], 1.0, ["msk"])
    _asel(P, msk[:, 0, :], "msk", [[1, 128]], ALU.is_ge, 0, -1)
    _asel(P, msk[:, 1, :], "msk", [[-1, 128]], ALU.is_ge, 0, 1)
    _asel(P, msk[:, 2, :], "msk", [[-1, 128]], ALU.is_gt, 0, 1)
    _asel(P, msk[:, 3, :], "msk", [[1, 128]], ALU.is_gt, 0, -1)
    _asel(P, msk[:, 4, :], "msk", [[1, 128]], ALU.is_ge, 0, -1)
    _asel(P, msk[:, 5, :], "msk", [[-1, 128]], ALU.is_ge, 0, 1)
    bd = C.sb("bd", [128, 256])
    P.memset("pool", bd[:], 1.0, ["bd"])
    for h in range(4):
        _asel(P, bd[:, h * 64:(h + 1) * 64], "bd", [[0, 64]], ALU.is_ge, -32 * h, 1)
        _asel(P, bd[:, h * 64:(h + 1) * 64], "bd", [[0, 64]], ALU.is_ge, 32 * h + 31, -1)
    sel = C.sb("sel", [128, 4])
    P.dma(sel[:, 0:2], dd["sel"], writes=["sel"])
    negcol = C.sb("negcol", [128, 1])
    P.memset("pool", negcol[:], -1.0 / 16.0, ["negcol"])
    ones1 = C.sb("ones1", [1, 128])
    P.memset("pool", ones1[:], 1.0, ["ones1"])
    rB = C.sb("rB", [128, 1536])
    with ExitStack() as es0:
        rowS = Ctx(nc, es0, C.pfx).sb("rowS", [1, 2048])
        P.dma(rowS[:], rows_d, writes=["rowS"])
        for i in range(3):
            P.mm(B[i][:], ones1[0:1, :], rowS[0:1, i * 512:(i + 1) * 512], True, True, ["ones1", "rowS"], [BK[i]])
            P.cp("act", rB[:, i * 512:(i + 1) * 512], B[i][:], [BK[i]], ["rB"])
        P.barrier()
    abB = rB[:, 0:256]
    gnB = rB[:, 256:512]
    qnB = rB[:, 512:1024]
    knB = rB[:, 1024:1152]
    dnB = rB[:, 1152:1408]
    lmB = rB[:, 1408:1536]
    lam = C.sb("lam", [128, 8])
    junk = C.sb("junk", [128, 32])
    P.op("dve", lambda e: e.scalar_tensor_tensor(junk[:, 0:32], lmB[:, 0:32], 1.0, lmB[:, 32:64], ALU.mult, ALU.mult,
                                                  accum_out=lam[:, 0:1]), ["rB"], ["n_junk", "lam"])
    P.op("dve", lambda e: e.scalar_tensor_tensor(junk[:, 0:32], lmB[:, 64:96], 1.0, lmB[:, 96:128], ALU.mult, ALU.mult,
                                                  accum_out=lam[:, 1:2]), ["rB"], ["n_junk", "lam"])
    P.act(lam[:, 2:4], lam[:, 0:2], AF.Exp, ["lam"], ["lam2"])
    P.tt("dve", lam[:, 4:5], lam[:, 3:4], lam[:, 2:3], ALU.subtract, ["lam2"], ["lam3"])
    P.ts("dve", lam[:, 5:6], lam[:, 4:5], -lam_init, None, ALU.add, None, ["lam3"], ["nlam"])
    nlam = lam[:, 5:6]
    P.ts("dve", rB[:, 512:1024], rB[:, 512:1024], 0.125, None, ALU.mult, None, ["rB"], ["rB"])
    P.ts("dve", rB[:, 1152:1408], rB[:, 1152:1408], 1.0 - lam_init, None, ALU.mult, None, ["rB"], ["rB"])

    KT = C.sb("KT", [64, 2, NKT * 128], BF16)
    Vg = C.sb("Vg", [128, NKT, 2, 65], BF16)
    KTd = C.sb("KTd", [128, 2, NKT * 128], BF16)
    Vd = C.sb("Vd", [128, NKT, 4, 65], BF16)
    P.memset("pool", Vg[:, :, :, 64:65], 1.0, ["Vg"])
    P.memset("pool", Vd[:, :, :, 64:65], 1.0, ["Vd"])
    glaout = C.sb("glaout", [128, 18, 256], BF16)
    ofS = glaout
    xt = C.sb("xt", [128, D])
    tmp = C.sb("tmp", [128, D])
    hf = C.sb("hf", [128, D])
    ss = C.sb("ss", [128, 4])
    hT = C.sb("hT", [128, 8, 128], BF16)
    sq = C.sb("sq", [128, 512])
    s8 = C.sb("s8", [128, 32])
    fa = C.sb("fa", [128, 512])
    fb = C.sb("fb", [128, 512])
    r1 = C.sb("r1", [128, 256])
    r2 = C.sb("r2", [128, 256])
    cG = C.sb("cG", [128, 64])
    cD = C.sb("cD", [128, 32])
    wst = [None, None]

    def load_w(dst, dcol, src_d, scol, width):
        for o in range(0, width, 256):
            w = min(256, width - o)
            k = load_w.n % 2
            load_w.n += 1
            P.dma(wst[k][:, :, 0:w], src_d.rearrange("(c p) n -> p c n", p=128)[:, :, scol + o:scol + o + w],
                  writes=["wst%d" % k])
            P.cp("pool", dst[:, :, dcol + o:dcol + o + w], wst[k][:, :, 0:w], ["wst%d" % k], ["W"])
    load_w.n = 0

    def load_h(src, j, rope):
        src_ap, src_key = src
        P.dma(xt[:], src_ap, reads=[src_key], writes=["xt"])
        if rope is not None:
            tabs, rope_row0 = rope
            P.dma(cG[:, 0:32], tabs[0][rope_row0:rope_row0 + 128, :], writes=["cG"])
            P.dma(cG[:, 32:64], tabs[1][rope_row0:rope_row0 + 128, :], writes=["cG"])
            P.dma(cD[:, 0:16], tabs[2][rope_row0:rope_row0 + 128, :], writes=["cD"])
            P.dma(cD[:, 16:32], tabs[3][rope_row0:rope_row0 + 128, :], writes=["cD"])
        emit_norm_mod(P, xt[:], "xt", modB[:, j, 1, :], modB[:, j, 0, :], MK, junk, ss, tmp, hf[:], "hf", "n_")
        for half in range(2):
            b = banks[3 + half]
            for c4 in range(4):
                c = half * 4 + c4
                P.tr(b[0][:, c4 * 128:(c4 + 1) * 128], hf[:, c * 128:(c + 1) * 128], ident[:], ["hf", "ident"], [b[1]])
            P.cp("act", hT[:, half * 4:(half + 1) * 4, :], b[0][:].rearrange("p (c t) -> p c t", c=4), [b[1]], ["hT"])

    def head_rstd(src_ps, src_key, H, dh, out_s8):
        P.act(sq[:, 0:H * dh], src_ps, AF.Square, [src_key], ["sq"])
        P.op("dve", lambda e: e.tensor_reduce(out=s8[:, 8:8 + H], in_=sq[:, 0:H * dh].rearrange("p (h d) -> p h d", h=H),
                                               axis=AX.X, op=ALU.add), ["sq"], ["s8a"])
        P.ts("dve", s8[:, 16:16 + H], s8[:, 8:8 + H], 1.0 / dh, 1e-6, ALU.mult, ALU.add, ["s8a"], ["s8b"])
        P.act(s8[:, 24:24 + H], s8[:, 16:16 + H], AF.Ln, ["s8b"], ["s8c"])
        P.act(out_s8, s8[:, 24:24 + H], AF.Exp, ["s8c"], ["s8"], scale=-0.5)

    with ExitStack() as es1:
        C1 = Ctx(nc, es1, C.pfx)
        wi1 = C1.sb("wi1", [128, 8, 1536], BF16)
        esw = ExitStack()
        Cw = Ctx(nc, esw, C.pfx)
        wst[0] = Cw.sb("wst0", [128, 8, 256])
        wst[1] = Cw.sb("wst1", [128, 8, 256])
        w4 = Cw.sb("w4", [16, 2, D])
        a2 = Cw.sb("a2", [16, 2, 128])
        P.dma(w4[:, 0, :], w4f_d, writes=["w4"])
        P.dma(w4[:, 1, :], w4b_d, writes=["w4"])
        P.dma(a2[:, 0, :], a2f_d, writes=["a2"])
        P.dma(a2[:, 1, :], a2b_d, writes=["a2"])
        for dr in range(2):
            for c in range(8):
                b = banks[c % 2]
                P.mm(b[0][:, 0:128], w4[:, dr, c * 128:(c + 1) * 128], a2[:, dr, :], True, True, ["w4", "a2"], [b[1]])
                P.cp("act", wi1[:, c, 256 + dr * 128:256 + (dr + 1) * 128], b[0][:, 0:128], [b[1]], ["W"])
        load_w(wi1, 0, win_d, 0, 256)
        load_w(wi1, 512, win_d, 256, 256)
        load_w(wi1, 768, win_d, 1312, 256)
        load_w(wi1, 1024, win_d, 2080, 256)
        load_w(wi1, 1280, win_d, 1824, 256)
        P.barrier()
        esw.close()
        NST = 18
        qkB = C1.sb("qkB", [128, NST, 2, 128], BF16)
        qkF = C1.sb("qkF", [128, 2, 128], BF16)
        kdB = C1.sb("kdB", [128, NST, 128], BF16)
        kdF = C1.sb("kdF", [128, 128], BF16)
        vS = C1.sb("vS", [128, NST, 256], BF16)
        dec = C1.sb("dec", [128, NST, 2])
        zt = C1.sb("zt", [128, 256])
        sp = C1.sb("sp", [128, 256])
        Eq = C1.sb("Eq", [128, 256])
        Ek = C1.sb("Ek", [128, 256])
        Ed = C1.sb("Ed", [128, 256])
        qk = C1.sb("qk", [128, 256])
        qdki = C1.sb("qdki", [128, 4, 128])
        kim = C1.sb("kim", [128, 4, 128], BF16)
        AT = C1.sb("AT", [128, 4, 128], BF16)
        KVm = C1.sb("KVm", [128, 256])
        S = [C1.sb("S%d" % i, [128, 256]) for i in range(2)]
        Sb = [C1.sb("Sb%d" % i, [128, 256], BF16) for i in range(2)]
        og = C1.sb("og", [128, 256])
        for i in range(2):
            P.memset("pool", S[i][:], 0.0, ["S%d" % i])
            P.memset("pool", Sb[i][:], 0.0, ["Sb%d" % i])

        def proj1():
            for g in range(3):
                for c in range(8):
                    P.mm(B[g][:], hT[:, c, :], wi1[:, c, g * 512:(g + 1) * 512], c == 0, c == 7, ["hT", "W"], [BK[g]])

        def gla_prep(st, full):
            P.tt("dve", zt[:], B[0][:, 256:512], abB, ALU.add, [BK[0], "rB"], ["zt"])
            P.act(Eq[:], zt[:], AF.Exp, ["zt"], ["Eq"], scale=-1.0)
            P.act(sp[:], Eq[:], AF.Ln, ["Eq"], ["sp"], bias=1.0)
            b3 = B[5]
            P.mm(b3[:, 0:128], msk[:, 0, :], sp[:, 0:128], True, True, ["msk", "sp"], [BK[5]])
            P.mm(b3[:, 128:256], msk[:, 1, :], sp[:, 128:256], True, True, ["msk", "sp"], [BK[5]])
            P.mm(b3[:, 256:384], msk[:, 2, :], sp[:, 0:128], True, True, ["msk", "sp"], [BK[5]])
            P.mm(b3[:, 384:512], msk[:, 3 if full else 2, :], sp[:, 128:256], True, True, ["msk", "sp"], [BK[5]])
            P.mm(B[6][:, 0:1], sp[:, 0:128], negcol[:], True, True, ["sp", "negcol"], [BK[6]])
            P.mm(B[6][:, 1:2], sp[:, 128:256], negcol[:], True, True, ["sp", "negcol"], [BK[6]])
            P.act(dec[:, st, :], B[6][:, 0:2], AF.Exp, [BK[6]], ["dec"])
            P.act(Ed[:], b3[:, 256:512], AF.Exp, [BK[5]], ["Ed"])
            P.cp("act", qk[:], B[0][:, 0:256], [BK[0]], ["qk"])
            P.tt("dve", kdB[:, st, :], qk[:, 128:256], Ed[:, 128:256], ALU.mult, ["qk", "Ed"], ["kdB"])
            if full:
                P.tt("dve", kdF[:], qk[:, 128:256], Ed[:, 0:128], ALU.mult, ["qk", "Ed"], ["kdF"])
            P.cp("act", vS[:, st, :], B[1][:, 0:256], [BK[1]], ["vS"])
            if not full:
                return
            P.act(Eq[:], b3[:, 0:256], AF.Exp, [BK[5]], ["Eq"])
            P.act(Ek[:], b3[:, 0:256], AF.Exp, [BK[5]], ["Ek"], scale=-1.0)
            for dr in range(2):
                P.stt(qdki[:, 2 * dr, :], qk[:, 0:128], 32.0 ** -0.5, Eq[:, dr * 128:(dr + 1) * 128], ALU.mult, ALU.mult,
                      ["qk", "Eq"], ["qdki"])
                P.tt("dve", qdki[:, 2 * dr + 1, :], qk[:, 128:256], Ek[:, dr * 128:(dr + 1) * 128], ALU.mult,
                     ["qk", "Ek"], ["qdki"])
            for i in range(4):
                P.tr(B[7][:, i * 128:(i + 1) * 128], qdki[:, i, :], ident[:], ["qdki", "ident"], [BK[7]])
            P.cp("act", qkF[:], B[7][:, 0:256].rearrange("p (i t) -> p i t", i=2), [BK[7]], ["qkF"])
            P.cp("act", qkB[:, st, :, :], B[7][:, 256:512].rearrange("p (i t) -> p i t", i=2), [BK[7]], ["qkB"])

        def kv_prep(kidx, rope):
            head_rstd(B[1][:, 256:384], BK[1], 2, 64, s8[:, 0:2])
            P.tt("dve", fa[:, 0:128].rearrange("p (h d) -> p h d", h=2), B[1][:, 256:384].rearrange("p (h d) -> p h d", h=2),
                 s8[:, 0:2].unsqueeze(2).to_broadcast([128, 2, 64]), ALU.mult, [BK[1], "s8"], ["fa"])
            P.tt("pool", fb[:, 0:128], fa[:, 0:128], knB, ALU.mult, ["fa", "rB"], ["fb"])
            src = fb
            if rope:
                emit_rope(P, "pool", fa[:, 0:128], fb[:, 0:128], cG[:, 0:32], cG[:, 32:64], 2, 16,
                          r1[:, 0:64], r2[:, 0:64], ["cG"], "fb", "fa")
                src = fa
            skey = "fa" if rope else "fb"
            for h in range(2):
                P.tr(B[7][0:64, h * 128:(h + 1) * 128], src[:, h * 64:(h + 1) * 64], ident[:], [skey, "ident"], [BK[7]])
            P.cp("act", KT[:, :, kidx * 128:(kidx + 1) * 128], B[7][0:64, 0:256].rearrange("p (h t) -> p h t", h=2),
                 [BK[7]], ["KT"])
            P.cp("act", Vg[:, kidx, :, 0:64], B[1][:, 384:512].rearrange("p (h d) -> p h d", h=2), [BK[1]], ["Vg"])
            P.cp("act", Vd[:, kidx, :, 0:64], B[2][:, 0:256].rearrange("p (h d) -> p h d", h=4), [BK[2]], ["Vd"])
            P.cp("act", fb[:, 256:512], B[2][:, 256:512], [BK[2]], ["fb2"])
            src2, k2 = fb[:, 256:512], "fb2"
            if rope:
                emit_rope(P, "pool", fa[:, 256:512], fb[:, 256:512], cD[:, 0:16], cD[:, 16:32], 8, 8,
                          r1[:, 0:128], r2[:, 0:128], ["cD"], "fb2", "fa2")
                src2, k2 = fa[:, 256:512], "fa2"
            for g in range(2):
                P.tr(B[7][:, 256 + g * 128:256 + (g + 1) * 128], src2[:, g * 128:(g + 1) * 128], ident[:],
                     [k2, "ident"], [BK[7]])
            P.cp("act", KTd[:, :, kidx * 128:(kidx + 1) * 128], B[7][:, 256:512].rearrange("p (g t) -> p g t", g=2),
                 [BK[7]], ["KTd"])

        def scan_step(dr, st, with_out, final_to=None, slot=None):
            Sd, Sbd = S[dr], Sb[dr]
            sk, sbk = "S%d" % dr, "Sb%d" % dr
            if dr == 0:
                qd_, ki_, kd_, qkk, kdk = qkF[:, 0, :], qkF[:, 1, :], kdF[:], "qkF", "kdF"
            else:
                qd_, ki_, kd_, qkk, kdk = qkB[:, st, 0, :], qkB[:, st, 1, :], kdB[:, st, :], "qkB", "kdB"
            if with_out:
                for h in range(4):
                    P.ts("pool", kim[:, h, :], ki_, bd[:, 64 * h:64 * h + 1], None, ALU.mult, None,
                         [qkk, "bd"], ["kim"])
                for h in range(4):
                    P.mm(B[5][:, h * 128:(h + 1) * 128], kim[:, h, :], qd_, True, True, ["kim", qkk], [BK[5]])
                P.tt("dve", AT[:], B[5][:].rearrange("p (h c) -> p h c", h=4),
                     msk[:, 4 + dr, :].unsqueeze(1).to_broadcast([128, 4, 128]), ALU.mult, [BK[5], "msk"], ["AT"])
                for h in range(4):
                    P.mm(B[7][:, h * 64:(h + 1) * 64], AT[:, h, :], vS[:, st, h * 64:(h + 1) * 64], True, False,
                         ["AT", "vS"], [BK[7]])
                    P.mm(B[7][:, h * 64:(h + 1) * 64], qd_, Sbd[:, h * 64:(h + 1) * 64], False, True,
                         [qkk, sbk], [BK[7]])
                if final_to is None:
                    P.cp("act", ofS[:, st, :], B[7][:, 0:256], [BK[7]], ["ofS"])
                else:
                    P.tt("dve", og[:], B[7][:, 0:256], ofS[:, st, :], ALU.add, [BK[7], "ofS"], ["og"])
                    P.act(sq[:, 0:256], og[:], AF.Square, ["og"], ["sq"])
                    P.op("dve", lambda e: e.tensor_reduce(out=s8[:, 8:12], in_=sq[:, 0:256].rearrange("p (h d) -> p h d", h=4),
                                                           axis=AX.X, op=ALU.add), ["sq"], ["s8a"])
                    P.ts("dve", s8[:, 16:20], s8[:, 8:12], 1.0 / 64, 1e-6, ALU.mult, ALU.add, ["s8a"], ["s8b"])
                    P.act(s8[:, 24:28], s8[:, 16:20], AF.Ln, ["s8b"], ["s8c"])
                    P.act(s8[:, 0:4], s8[:, 24:28], AF.Exp, ["s8c"], ["s8"], scale=-0.5)
                    P.tt("dve", og[:].rearrange("p (h d) -> p h d", h=4), og[:].rearrange("p (h d) -> p h d", h=4),
                         s8[:, 0:4].unsqueeze(2).to_broadcast([128, 4, 64]), ALU.mult, ["og", "s8"], ["og"])
                    P.tt("pool", glaout[:, final_to, :], og[:], gnB, ALU.mult, ["og", "rB"], ["glaout"])
            P.mm(B[6][:, 0:256], kd_, vS[:, st, :], True, True, [kdk, "vS"], [BK[6]])
            if slot is None:
                P.tt("dve", KVm[:], B[6][:, 0:256], bd[:], ALU.mult, [BK[6], "bd"], ["KVm"])
                P.stt(Sd[:], Sd[:], dec[:, st, dr:dr + 1], KVm[:], ALU.mult, ALU.add, [sk, "dec", "KVm"], [sk])
            else:
                sg_ = sel[:, slot:slot + 1]
                P.stt(KVm[:], B[6][:, 0:256], sg_, bd[:], ALU.mult, ALU.mult, [BK[6], "bd", "sel"], ["KVm"])
                P.ts("dve", sel[:, 2:3], dec[:, st, dr:dr + 1], -1.0, sg_, ALU.add, ALU.mult, ["dec", "sel"], ["sel2"])
                P.ts("dve", sel[:, 3:4], sel[:, 2:3], 1.0, None, ALU.add, None, ["sel2"], ["sel3"])
                P.stt(Sd[:], Sd[:], sel[:, 3:4], KVm[:], ALU.mult, ALU.add, [sk, "sel3", "KVm"], [sk])
            P.cp("act", Sbd[:], Sd[:], [sk], [sbk])

        for i in range(2):
            load_h(dd["ctx"][i], 1, None)
            proj1()
            gla_prep(16 + i, True)
            kv_prep(32 + i, False)
            scan_step(0, 16 + i, need_ctx, None)
        for i in (1, 0):
            scan_step(1, 16 + i, need_ctx, (16 + i) if need_ctx else None)
        for t in range(16):
            load_h(dd["xown"][t], 0, (ropeO, t * 128))
            proj1()
            gla_prep(t, True)
            kv_prep(t, True)
            scan_step(0, t, True, None)
        groups = [[0, 1], [2, 3], [4, 5], [6, 7]]
        X = dd["xch"]

        def allgather(src, dst, key):
            P.coll(lambda e: e.collective_compute("AllGather", ALU.bypass, replica_groups=groups,
                                                   ins=[src], outs=[dst]), [key + "_i"], [key + "_o"])
        P.dma(X["sx_i"], S[0][:], reads=["S0"], writes=["sx_i"])
        allgather(X["sx_i"], X["sx_o"], "sx")
        Sx = C1.sb("Sx", [128, 2, 256])
        P.dma(Sx[:], X["sx_o"].rearrange("(s p) n -> p s n", p=128), reads=["sx_o"], writes=["Sx"])
        P.ts("dve", KVm[:], Sx[:, 0, :], sel[:, 0:1], None, ALU.mult, None, ["Sx", "sel"], ["KVm"])
        P.stt(S[1][:], Sx[:, 1, :], sel[:, 1:2], KVm[:], ALU.mult, ALU.add, ["Sx", "sel", "KVm"], ["S1"])
        P.cp("act", Sb[1][:], S[1][:], ["S1"], ["Sb1"])
        P.dma(X["kt_i"].rearrange("p (k t) -> p k t", k=2), KT[:, :, 0:2048], reads=["KT"], writes=["kt_i"])
        P.dma(X["ktd_i"].rearrange("p (k t) -> p k t", k=2), KTd[:, :, 0:2048], reads=["KTd"], writes=["ktd_i"])
        P.dma(X["vg_i"].rearrange("p (t k d) -> p t k d", t=16, k=2), Vg[:, 0:16, :, :], reads=["Vg"], writes=["vg_i"])
        for hh in range(2):
            P.dma(X["vd_i%d" % hh].rearrange("p (t k d) -> p t k d", t=8, k=4), Vd[:, hh * 8:(hh + 1) * 8, :, :],
                  reads=["Vd"], writes=["vd%d_i" % hh])
        allgather(X["kt_i"], X["kt_o"], "kt")
        allgather(X["ktd_i"], X["ktd_o"], "ktd")
        allgather(X["vg_i"], X["vg_o"], "vg")
        for hh in range(2):
            allgather(X["vd_i%d" % hh], X["vd_o%d" % hh], "vd%d" % hh)
        for sl in range(2):
            P.dma(KT[:, :, sl * 2048:(sl + 1) * 2048], X["kt_o"][sl * 64:(sl + 1) * 64, :].rearrange("p (k t) -> p k t", k=2),
                  reads=["kt_o"], writes=["KT"])
            P.dma(KTd[:, :, sl * 2048:(sl + 1) * 2048], X["ktd_o"][sl * 128:(sl + 1) * 128, :].rearrange("p (k t) -> p k t", k=2),
                  reads=["ktd_o"], writes=["KTd"])
            P.dma(Vg[:, sl * 16:(sl + 1) * 16, :, :], X["vg_o"][sl * 128:(sl + 1) * 128, :].rearrange("p (t k d) -> p t k d", t=16, k=2),
                  reads=["vg_o"], writes=["Vg"])
            for hh in range(2):
                P.dma(Vd[:, sl * 16 + hh * 8:sl * 16 + (hh + 1) * 8, :, :],
                      X["vd_o%d" % hh][sl * 128:(sl + 1) * 128, :].rearrange("p (t k d) -> p t k d", t=8, k=4),
                      reads=["vd%d_o" % hh], writes=["Vd"])
        for t in range(15, -1, -1):
            scan_step(1, t, True, t)
        P.barrier()

    with ExitStack() as es2:
        C2 = Ctx(nc, es2, C.pfx)
        wi2 = C2.sb("wi2", [128, 8, 1024], BF16)
        wo = C2.sb("wo", [128, 8, 1024], BF16)
        with ExitStack() as esw2:
            Cw2 = Ctx(nc, esw2, C.pfx)
            wst[0] = Cw2.sb("wst0b", [128, 8, 256])
            wst[1] = Cw2.sb("wst1b", [128, 8, 256])
            load_w(wi2, 0, win_d, 800, 512)
            load_w(wi2, 512, win_d, 1568, 256)
            load_w(wi2, 768, win_d, 512, 256)
            load_w(wo, 0, wout_d, 0, 1024)
            P.barrier()
        QT2 = [C2.sb("QT%d" % i, [64, 8, 128], BF16) for i in range(2)]
        QTm2 = [C2.sb("QTm%d" % i, [128, 8, 128], BF16) for i in range(2)]
        cat2 = [C2.sb("cat%d" % i, [128, D]) for i in range(2)]
        xt2 = [xt, C2.sb("xtb", [128, D])]
        hT2 = [hT, C2.sb("hTb", [128, 8, 128], BF16)]
        QTd = C2.sb("QTd", [128, 2, 128], BF16)
        PT = [C2.sb("PT%d" % i, [128, 1024], BF16) for i in range(2)]
        catT = C2.sb("catT", [128, 8, 128], BF16)
        rs = C2.sb("rs", [128, 256])
        rc = C2.sb("rc", [128, 16])
        od = C2.sb("od", [128, 256])
        t0 = C2.sb("t0", [128, 64])
        xo = C2.sb("xo", [128, D])

        def prep_chunks(tile, p):
            (src_ap, src_key), j, rope_row0, gidx, keys, dst = tile
            xt_, hT_, QT_, QTm_, cat_ = xt2[p], hT2[p], QT2[p], QTm2[p], cat2[p]
            kx, kh, kq, kqm, kc = "xt%d" % p, "hT%d" % p, "QT%d" % p, "QTm%d" % p, "cat%d" % p
            rope = rope_row0 is not None

            def c0():
                P.dma(xt_[:], src_ap, reads=[src_key], writes=[kx])
                if rope:
                    P.dma(cG[:, 0:32], ropeO[0][rope_row0:rope_row0 + 128, :], writes=["cG"])
                    P.dma(cG[:, 32:64], ropeO[1][rope_row0:rope_row0 + 128, :], writes=["cG"])
                    P.dma(cD[:, 0:16], ropeO[2][rope_row0:rope_row0 + 128, :], writes=["cD"])
                    P.dma(cD[:, 16:32], ropeO[3][rope_row0:rope_row0 + 128, :], writes=["cD"])

            def c1():
                emit_norm_mod(P, xt_[:], kx, modB[:, j, 1, :], modB[:, j, 0, :], MK, None, ss, tmp, hf[:], "hf", "n_")

            def c2():
                for half in range(2):
                    b = banks[half]
                    for c4 in range(4):
                        c = half * 4 + c4
                        P.tr(b[0][:, c4 * 128:(c4 + 1) * 128], hf[:, c * 128:(c + 1) * 128], ident[:], ["hf", "ident"], [b[1]])
                    P.cp("act", hT_[:, half * 4:(half + 1) * 4, :], b[0][:].rearrange("p (c t) -> p c t", c=4), [b[1]], [kh])

            def c3():
                for g in range(2):
                    for c in range(8):
                        P.mm(B[g][:], hT_[:, c, :], wi2[:, c, g * 512:(g + 1) * 512], c == 0, c == 7, [kh, "W"], [BK[g]])

            def c4():
                head_rstd(B[0][:], BK[0], 8, 64, s8[:, 0:8])
                P.tt("dve", fa[:].rearrange("p (h d) -> p h d", h=8), B[0][:].rearrange("p (h d) -> p h d", h=8),
                     s8[:, 0:8].unsqueeze(2).to_broadcast([128, 8, 64]), ALU.mult, [BK[0], "s8"], ["fa"])
                P.tt("pool", fb[:], fa[:], qnB, ALU.mult, ["fa", "rB"], ["fb"])
                if rope:
                    emit_rope(P, "pool", fa[:], fb[:], cG[:, 0:32], cG[:, 32:64], 8, 16, r1[:], r2[:], ["cG"], "fb", "fa")
                P.act(sq[:, 0:256], B[1][:, 0:256], AF.Copy, [BK[1]], ["sq"], scale=32.0 ** -0.5)
                if rope:
                    emit_rope(P, "pool", sq[:, 256:512], sq[:, 0:256], cD[:, 0:16], cD[:, 16:32], 8, 8,
                              r1[:, 0:128], r2[:, 0:128], ["cD"], "sq", "sqr")
                P.act(rs[:], B[1][:, 256:512], AF.Silu, [BK[1]], ["rs"])
                P.tt("dve", cat_[:, 0:256], glaout[:, gidx, :], rs[:], ALU.mult, ["glaout", "rs"], [kc])

            def c5():
                src, skey = (fa, "fa") if rope else (fb, "fb")
                for hh in range(2):
                    b = banks[hh]
                    for h4 in range(4):
                        h = hh * 4 + h4
                        P.tr(b[0][0:64, h4 * 128:(h4 + 1) * 128], src[:, h * 64:(h + 1) * 64], ident[:], [skey, "ident"], [b[1]])
                    P.cp("act", QT_[:, hh * 4:(hh + 1) * 4, :], b[0][0:64, :].rearrange("p (h t) -> p h t", h=4), [b[1]], [kq])
                src2, k2 = (sq[:, 256:512], "sqr") if rope else (sq[:, 0:256], "sq")
                for g in range(2):
                    P.tr(B[0][:, g * 128:(g + 1) * 128], src2[:, g * 128:(g + 1) * 128], ident[:], [k2, "ident"], [BK[0]])
                P.cp("act", QTd[:], B[0][:, 0:256].rearrange("p (g t) -> p g t", g=2), [BK[0]], ["QTd"])
                for m in range(8):
                    P.ts("pool", QTm_[:, m, :], QTd[:, m // 4, :], bd[:, 64 * (m % 4):64 * (m % 4) + 1], None, ALU.mult, None,
                         ["QTd", "bd"], [kqm])
            return [c0, c1, c2, c3, c4, c5]

        SCHED = {1: 0, 6: 1, 15: 2, 21: 3, 23: 4, 46: 5}

        def attention(tile, p, nxt):
            (src_ap, src_key), j, rope_row0, gidx, keys, dst = tile
            QT_, QTm_, cat_ = QT2[p], QTm2[p], cat2[p]
            kq, kqm, kc = "QT%d" % p, "QTm%d" % p, "cat%d" % p
            done = [0]

            def gqa_post(kv):
                ob = banks[6 + kv]
                for g in range(4):
                    h = kv * 4 + g
                    P.op("dve", lambda e, g=g, ob=ob, h=h: e.reciprocal(rc[:, h:h + 1], ob[0][:, g * 65 + 64:g * 65 + 65]),
                         [ob[1]], ["rc"])
                    P.ts("dve", cat_[:, 256 + h * 64:256 + (h + 1) * 64], ob[0][:, g * 65:g * 65 + 64], rc[:, h:h + 1], None,
                         ALU.mult, None, [ob[1], "rc"], [kc])

            def diff_post(gg):
                ob = banks[6 + gg]
                for hh in range(2):
                    h = gg * 2 + hh
                    m0, m1 = 2 * hh, 2 * hh + 1
                    P.op("dve", lambda e, ob=ob, m0=m0: e.reciprocal(rc[:, 8:9], ob[0][:, m0 * 65 + 64:m0 * 65 + 65]),
                         [ob[1]], ["rc"])
                    P.op("dve", lambda e, ob=ob, m1=m1: e.reciprocal(rc[:, 9:10], ob[0][:, m1 * 65 + 64:m1 * 65 + 65]),
                         [ob[1]], ["rc"])
                    P.tt("dve", rc[:, 10:11], rc[:, 9:10], nlam, ALU.mult, ["rc", "nlam"], ["rc"])
                    P.ts("dve", t0[:], ob[0][:, m0 * 65:m0 * 65 + 64], rc[:, 8:9], None, ALU.mult, None, [ob[1], "rc"], ["t0"])
                    P.stt(od[:, h * 64:(h + 1) * 64], ob[0][:, m1 * 65:m1 * 65 + 64], rc[:, 10:11], t0[:], ALU.mult, ALU.add,
                          [ob[1], "rc", "t0"], ["od"])

            npair = len(keys) // 2
            items = [(typ, grp, pi_, (keys[2 * pi_], keys[2 * pi_ + 1]))
                     for typ in range(2) for grp in range(2) for pi_ in range(npair)]

            def emit_score(ix):
                typ, grp, pi_, pr = items[ix]
                for hx in range(2):
                    sbk = banks[2 + 2 * (ix % 2) + hx]
                    s = pr[hx]
                    if typ == 0:
                        P.mm(sbk[0][:], KT[:, grp, s * 128:(s + 1) * 128], QT_[:, grp * 4:(grp + 1) * 4, :], True, True,
                             ["KT", kq], [sbk[1]])
                    else:
                        P.mm(sbk[0][:], KTd[:, grp, s * 128:(s + 1) * 128], QTm_[:, grp * 4:(grp + 1) * 4, :], True, True,
                             ["KTd", kqm], [sbk[1]])

            def emit_rest(ix):
                typ, grp, pi_, pr = items[ix]
                k0, k1 = "bank%d" % (2 + 2 * (ix % 2)), "bank%d" % (3 + 2 * (ix % 2))
                pt = PT[ix % 2]
                ptk = "PT%d" % (ix % 2)
                ob = banks[6 + grp]
                P.act(pt[:], PS2[1 + ix % 2][:], AF.Exp, [k0, k1], [ptk])
                for hx in range(2):
                    s = pr[hx]
                    for g in range(4):
                        if typ == 0:
                            rhs, vk = Vg[:, s, grp, :], "Vg"
                        else:
                            rhs, vk = Vd[:, s, (grp * 4 + g) // 2, :], "Vd"
                        P.mm(ob[0][:, g * 65:(g + 1) * 65], pt[:, hx * 512 + g * 128:hx * 512 + (g + 1) * 128], rhs,
                             pi_ == 0 and hx == 0 and g == 0, pi_ == npair - 1 and hx == 1, [ptk, vk], [ob[1]], skip=True)
                if pi_ == npair - 1:
                    if typ == 0:
                        gqa_post(grp)
                    else:
                        diff_post(grp)

            emit_score(0)
            for ix in range(len(items)):
                if ix + 1 < len(items):
                    emit_score(ix + 1)
                emit_rest(ix)
                if ix in SCHED and SCHED[ix] == done[0] and done[0] < len(nxt):
                    nxt[done[0]]()
                    done[0] += 1
            while done[0] < len(nxt):
                nxt[done[0]]()
                done[0] += 1

        def post(tile, p):
            (src_ap, src_key), j, rope_row0, gidx, keys, dst = tile
            xt_, cat_ = xt2[p], cat2[p]
            kx, kc = "xt%d" % p, "cat%d" % p
            head_rstd(od[:], "od", 4, 64, s8[:, 0:4])
            P.tt("dve", od[:].rearrange("p (h d) -> p h d", h=4), od[:].rearrange("p (h d) -> p h d", h=4),
                 s8[:, 0:4].unsqueeze(2).to_broadcast([128, 4, 64]), ALU.mult, ["od", "s8"], ["od"])
            P.tt("pool", cat_[:, 768:1024], od[:], dnB, ALU.mult, ["od", "rB"], [kc])
            for half in range(2):
                b = banks[half]
                for c4 in range(4):
                    c = half * 4 + c4
                    P.tr(b[0][:, c4 * 128:(c4 + 1) * 128], cat_[:, c * 128:(c + 1) * 128], ident[:], [kc, "ident"], [b[1]])
                P.cp("act", catT[:, half * 4:(half + 1) * 4, :], b[0][:].rearrange("p (c t) -> p c t", c=4), [b[1]], ["catT"])
            for h2 in range(2):
                b = banks[h2]
                for c in range(8):
                    P.mm(b[0][:], catT[:, c, :], wo[:, c, h2 * 512:(h2 + 1) * 512], c == 0, c == 7, ["catT", "W"], [b[1]])
                cs = slice(h2 * 512, (h2 + 1) * 512)
                P.tt("dve", tmp[:, cs], b[0][:], modB[:, j, 2, cs], ALU.mult, [b[1]] + MK, ["n_tmp"])
                P.tt("pool", xo[:, cs], xt_[:, cs], tmp[:, cs], ALU.add, ["n_tmp", kx], ["xo"])
            P.dma(dst[0], xo[:], reads=["xo"], writes=[dst[1]])

        tiles = [(dd["xown"][t], 0, t * 128, t, list(range(NKT)), dd["yo"][t]) for t in range(16)]
        if need_ctx:
            tiles += [(dd["ctx"][i], 1, None, 16 + i, [32, 33], dd["yc"][i]) for i in range(2)]
        for ch in prep_chunks(tiles[0], 0):
            ch()
        for i, tile in enumerate(tiles):
            nxt = prep_chunks(tiles[i + 1], (i + 1) % 2) if i + 1 < len(tiles) else []
            attention(tile, i % 2, nxt)
            post(tile, i % 2)
        P.barrier()


LAM_INIT = [0.8 - 0.6 * math.exp(-0.3 * l) for l in range(2)]


def build_fused(stages=("m0", "f0", "cc", "m1", "f1")):
    nc = bass.Bass("TRN2", target_bir_lowering=False)
    es = ExitStack()
    C = Ctx(nc, es)
    P = Prog(nc, es)
    di = {}

    def din(name, shape):
        di[name] = C.din(name, shape)
        return di[name]

    din("xown", [2048, D]); din("ctx", [256, D]); din("ccT", [128, 16]); din("sel", [128, 2])
    ropeO = [din("cosGo", [2048, 32]), din("sinGo", [2048, 32]), din("cosDo", [2048, 16]), din("sinDo", [2048, 16])]
    for l in range(2):
        din("w_mod%d" % l, [D, 6 * D]); din("b_mod%d" % l, [1, 6 * D])
        din("norm_mix%d" % l, [1, D]); din("norm_ffn%d" % l, [1, D])
        din("w_in%d" % l, [D, 2336]); din("w_out%d" % l, [D, D])
        din("w4Tf%d" % l, [16, D]); din("w4Tb%d" % l, [16, D]); din("a2f%d" % l, [16, 128]); din("a2b%d" % l, [16, 128])
        din("rows%d" % l, [1, 2048])
    din("norm_f", [1, D])
    din("wg0", [1, D, 2816]); din("wu0", [1, D, 2816]); din("wd0", [1, 2816, D])
    din("wg1", [8, D, 3584]); din("wu1", [8, D, 3584]); din("wd1", [8, 3584, D]); din("wrT", [8, D])
    y_d = C.dout("y", [2048, D])
    xmid0 = nc.dram_tensor("xmid0", [2304, D], F32, kind="Internal").ap()
    x1own = nc.dram_tensor("x1own", [2048, D], F32, kind="Internal").ap()

    def xch(l):
        def t(name, shape, dt):
            return nc.dram_tensor("x%d_%s" % (l, name), shape, dt, kind="Internal").ap()
        return {"sx_i": t("sx_i", [128, 256], F32), "sx_o": t("sx_o", [256, 256], F32),
                "kt_i": t("kt_i", [64, 4096], BF16), "kt_o": t("kt_o", [128, 4096], BF16),
                "ktd_i": t("ktd_i", [128, 4096], BF16), "ktd_o": t("ktd_o", [256, 4096], BF16),
                "vg_i": t("vg_i", [128, 2080], BF16), "vg_o": t("vg_o", [256, 2080], BF16),
                "vd_i0": t("vd_i0", [128, 2080], BF16), "vd_o0": t("vd_o0", [256, 2080], BF16),
                "vd_i1": t("vd_i1", [128, 2080], BF16), "vd_o1": t("vd_o1", [256, 2080], BF16)}
    xc1 = nc.dram_tensor("xc1", [256, D], F32, kind="Internal").ap()
    xmid1 = nc.dram_tensor("xmid1", [2048, D], F32, kind="Internal").ap()
    PS2 = [C.ps("psd%d" % i, [128, 1024]) for i in range(4)]
    banks = [(PS2[i // 2][:, (i % 2) * 512:(i % 2 + 1) * 512], "bank%d" % i) for i in range(8)]
    banks[0] = banks[0] + (PS2,)

    def tl(ap, key, n, off=0):
        return [(ap[off + t * 128:off + (t + 1) * 128, :], key) for t in range(n)]

    def mixer(l, xown_t, ctx_t, yo_t, yc_t):
        with ExitStack() as st:
            Cs = Ctx(nc, st, "M%d_" % l)
            dd = {"ccT": di["ccT"], "sel": di["sel"], "ropeO": ropeO, "xch": xch(l),
                  "xown": xown_t, "ctx": ctx_t, "yo": yo_t, "yc": yc_t}
            for k in ("w_mod", "b_mod", "norm_mix", "w_in", "w_out", "w4Tf", "w4Tb", "a2f", "a2b", "rows"):
                dd[k] = di["%s%d" % (k, l)]
            emit_mixer(P, nc, Cs, banks, l == 0, LAM_INIT[l], dd)

    def ffn(l, tiles):
        with ExitStack() as st:
            Cs = Ctx(nc, st, "F%d_" % l)
            dd = {"ccT": di["ccT"], "w_mod": di["w_mod%d" % l], "b_mod": di["b_mod%d" % l],
                  "norm_ffn": di["norm_ffn%d" % l], "norm_f": di["norm_f"],
                  "wg": di["wg%d" % l], "wu": di["wu%d" % l], "wd": di["wd%d" % l], "wrT": di["wrT"]}
            emit_ffn(P, nc, Cs, banks, "dense" if l == 0 else "moe", tiles, dd, l == 1)

    if "m0" in stages:
      mixer(0, tl(di["xown"], "in", 16), tl(di["ctx"], "in", 2),
          tl(y_d if stages == ("m0",) else xmid0, "xmid0", 16), tl(xmid0, "xmid0", 2, 2048))
    t0 = [(xmid0[t * 128:(t + 1) * 128, :], "xmid0", x1own[t * 128:(t + 1) * 128, :], "x1own", 0) for t in range(16)]
    t0 += [(xmid0[2048 + i * 128:2048 + (i + 1) * 128, :], "xmid0", xc1[i * 128:(i + 1) * 128, :], "xc1", 1) for i in range(2)]
    if "f0" in stages:
        ffn(0, t0)
    if "m1" in stages:
      mixer(1, tl(x1own, "x1own", 16), tl(xc1, "xc1", 2), tl(xmid1, "xmid1", 16), [])
    t1 = [(xmid1[t * 128:(t + 1) * 128, :], "xmid1", y_d[t * 128:(t + 1) * 128, :], "yout", 0) for t in range(16)]
    if "f1" in stages:
        ffn(1, t1)
    P.emit()
    es.close()
    return nc


def _ccT(cb, cc):
    return np.ascontiguousarray(np.concatenate([cb.reshape(8, 128).T, cc.reshape(8, 128).T], axis=1), dtype=np.float32)


def _rope_tables():
    t = np.arange(4096)
    rows = (t // 64).astype(np.float64)
    cols = (t % 64).astype(np.float64)

    def tab(half):
        fr = 10000.0 ** (-np.arange(half, dtype=np.float64) / half)
        ang = np.concatenate([rows[:, None] * fr[None, :], cols[:, None] * fr[None, :]], axis=1)
        return np.cos(ang).astype(np.float32), np.sin(ang).astype(np.float32)
    cG, sG = tab(16)
    cD, sD = tab(8)
    return cG, sG, cD, sD


def core_inputs(inp, b, half, ropes):
    f = lambda a: np.ascontiguousarray(a, dtype=np.float32)
    cG, sG, cD, sD = ropes
    xb = inp["x"][b]
    if half == 0:
        oorder = np.arange(2048)
        ctxl = inp["ctx"][b]
        sel = np.array([0.0, 1.0], np.float32)
    else:
        oorder = np.arange(4095, 2047, -1)
        ctxl = inp["ctx"][b][::-1]
        sel = np.array([1.0, 0.0], np.float32)
    m = {"xown": f(xb[oorder]), "ctx": f(ctxl), "ccT": _ccT(inp["c"][b], inp["c_ctx"]),
         "sel": f(np.tile(sel[None, :], (128, 1))),
         "cosGo": f(cG[oorder]), "sinGo": f(sG[oorder]), "cosDo": f(cD[oorder]), "sinDo": f(sD[oorder]),
         "norm_f": f(inp["norm_f"][None, :]),
         "wg0": inp["ffn_gate"], "wu0": inp["ffn_up"], "wd0": inp["ffn_down"],
         "wg1": inp["moe_gate"][0], "wu1": inp["moe_up"][0], "wd1": inp["moe_down"][0],
         "wrT": f(inp["moe_router"][0].T)}
    for l in range(2):
        w_in = inp["w_in"][l]
        if half == 0:
            w4f, w4b = w_in[:, 768:784], w_in[:, 784:800]
            a2f, a2b = inp["gla_a2_f"][l], inp["gla_a2_b"][l]
            abf, abb = inp["gla_ab_f"][l], inp["gla_ab_b"][l]
        else:
            w4f, w4b = w_in[:, 784:800], w_in[:, 768:784]
            a2f, a2b = inp["gla_a2_b"][l], inp["gla_a2_f"][l]
            abf, abb = inp["gla_ab_b"][l], inp["gla_ab_f"][l]
        rows = np.concatenate([abf, abb, np.tile(inp["gla_norm"][l], 4), np.tile(inp["gqa_q_norm"][l], 8),
                               np.tile(inp["gqa_k_norm"][l], 2), np.tile(inp["diff_norm"][l], 4),
                               inp["diff_lam_q1"][l], inp["diff_lam_k1"][l], inp["diff_lam_q2"][l], inp["diff_lam_k2"][l],
                               np.zeros(512, np.float32)]).astype(np.float32)[None, :]
        m.update({"w_mod%d" % l: inp["w_mod"][l], "b_mod%d" % l: f(inp["b_mod"][l][None, :]),
                  "norm_mix%d" % l: f(inp["norm_mix"][l][None, :]), "norm_ffn%d" % l: f(inp["norm_ffn"][l][None, :]),
                  "w_in%d" % l: w_in, "w_out%d" % l: inp["w_out"][l], "w4Tf%d" % l: f(w4f.T), "w4Tb%d" % l: f(w4b.T),
                  "a2f%d" % l: f(a2f), "a2b%d" % l: f(a2b), "rows%d" % l: f(rows)})
    return m


def kernel(**inp):
    inp = {k: np.asarray(v) for k, v in inp.items()}
    ropes = _rope_tables()
    nc = build_fused()
    maps = [core_inputs(inp, c // 2, c % 2, ropes) for c in range(8)]
    res = run_bass_kernel_spmd(nc, maps, core_ids=list(range(8))).results
    Bn = inp["x"].shape[0]
    out = np.empty((Bn, 4096, D), np.float32)
    for c in range(8):
        b, half = c // 2, c % 2
        y = res[c]["y"]
        if half == 0:
            out[b, 0:2048] = y
        else:
            out[b, 2048:4096] = y[::-1]
    return out
```

```python
import math
from contextlib import ExitStack
import numpy as np
import concourse.bass as bass
import concourse.mybir as mybir
from concourse.bass_utils import run_bass_kernel_spmd

F32 = mybir.dt.float32
BF16 = mybir.dt.bfloat16
AF = mybir.ActivationFunctionType
ALU = mybir.AluOpType
AX = mybir.AxisListType

D = 1024
NSLOT = 8


class Prog:
    ENGS = ["pe", "act", "dve", "pool", "sp"]

    def __init__(self, nc, es):
        self.nc = nc
        self.es = es
        self.ncoll = 0
        self.stream = {e: [] for e in self.ENGS}
        self.cnt = {e: 0 for e in self.ENGS}
        self.known = {e: {} for e in self.ENGS}
        self.lastw = {}
        self.rds = {}
        self.dmacnt = {e: 0 for e in self.ENGS}
        self.sems = {}
        self.semmax = {}
        for e in ["pe", "act", "dve", "pool"]:
            self.sems[e] = es.enter_context(nc.semaphore("s_" + e))
        for q in ["sp", "act", "pool"]:
            for j in range(NSLOT):
                self.sems[(q, j)] = es.enter_context(nc.semaphore("d_%s_%d" % (q, j)))

    def _wait(self, eng, ev):
        if ev is None:
            return
        k, v = ev
        if k == eng and eng == "pe":
            return
        if self.known[eng].get(k, 0) >= v:
            return
        self.known[eng][k] = v
        self.stream[eng].append(("w", k, v))

    def _deps(self, eng, reads, writes):
        for r in reads:
            self._wait(eng, self.lastw.get(r))
        for w in writes:
            self._wait(eng, self.lastw.get(w))
            for k, v in self.rds.get(w, {}).items():
                self._wait(eng, (k, v))

    def _commit(self, ev, reads, writes):
        k, v = ev
        self.semmax[k] = max(self.semmax.get(k, 0), v)
        for r in reads:
            d = self.rds.setdefault(r, {})
            d[k] = max(d.get(k, 0), v)
        for w in writes:
            self.lastw[w] = ev
            self.rds[w] = {}

    def op(self, eng, fn, reads=(), writes=()):
        self._deps(eng, reads, writes)
        self.cnt[eng] += 1
        ev = (eng, self.cnt[eng])
        self.stream[eng].append(("c", fn))
        self._commit(ev, reads, writes)

    def dma(self, out, in_, reads=(), writes=(), q="sp", **kw):
        n = self.dmacnt[q]
        self.dmacnt[q] += 1
        slot = n % NSLOT
        val = 16 * (n // NSLOT + 1)
        if val > 16:
            self._wait(q, ((q, slot), val - 16))
        self._deps(q, reads, writes)
        self.stream[q].append(("d", out, in_, (q, slot), kw))
        self._commit(((q, slot), val), reads, writes)

    def coll(self, fn, reads=(), writes=()):
        key = ("cc", self.ncoll)
        self.ncoll += 1
        self.sems[key] = self.es.enter_context(self.nc.semaphore("cc_%d" % key[1]))
        self._deps("pool", reads, writes)
        self.stream["pool"].append(("x", fn, key))
        self._commit((key, 1), reads, writes)

    def barrier(self):
        for e in self.ENGS:
            for k, v in self.semmax.items():
                self._wait(e, (k, v))

    def mm(self, out, lhsT, rhs, start, stop, reads, writes, skip=False):
        if skip:
            self.op("pe", lambda e: e.matmul(out, lhsT=lhsT, rhs=rhs, start=start, stop=stop, skip_group_check=True),
                    reads, writes)
        else:
            self.op("pe", lambda e: e.matmul(out, lhsT=lhsT, rhs=rhs, start=start, stop=stop), reads, writes)

    def tr(self, out, in_, ident, reads, writes):
        self.op("pe", lambda e: e.transpose(out, in_, ident), reads, writes)

    def act(self, out, in_, func, reads, writes, bias=None, scale=None, accum_out=None):
        kw = {}
        if bias is not None:
            kw["bias"] = bias
        if scale is not None:
            kw["scale"] = scale
        if accum_out is not None:
            kw["accum_out"] = accum_out
        self.op("act", lambda e: e.activation(out, in_, func, **kw), reads, writes)

    def tt(self, eng, out, in0, in1, op, reads, writes):
        self.op(eng, lambda e: e.tensor_tensor(out, in0, in1, op), reads, writes)

    def ts(self, eng, out, in0, s1, s2, op0, op1, reads, writes, accum_out=None):
        if op1 is None:
            self.op(eng, lambda e: e.tensor_scalar(out, in0, s1, None, op0), reads, writes)
        elif accum_out is None:
            self.op(eng, lambda e: e.tensor_scalar(out, in0, s1, s2, op0, op1), reads, writes)
        else:
            self.op(eng, lambda e: e.tensor_scalar(out, in0, s1, s2, op0, op1, accum_out=accum_out), reads, writes)

    def stt(self, out, in0, scalar, in1, op0, op1, reads, writes):
        self.op("dve", lambda e: e.scalar_tensor_tensor(out, in0, scalar, in1, op0, op1), reads, writes)

    def cp(self, eng, out, in_, reads, writes):
        if eng == "act":
            self.op("act", lambda e: e.copy(out, in_), reads, writes)
        else:
            self.op(eng, lambda e: e.tensor_copy(out, in_), reads, writes)

    def memset(self, eng, ap, val, writes):
        self.op(eng, lambda e: e.memset(ap, val), (), writes)

    def prune(self):
        comp = ("pe", "act", "dve", "pool")
        need = {e: set() for e in comp}
        for e in self.ENGS:
            for it in self.stream[e]:
                if it[0] == "w" and it[1] in need:
                    need[it[1]].add(it[2])
        rank = {e: {n: i + 1 for i, n in enumerate(sorted(need[e]))} for e in comp}
        out = {}
        for e in self.ENGS:
            lst = []
            idx = 0
            for it in self.stream[e]:
                if it[0] == "w" and it[1] in rank:
                    lst.append(("w", it[1], rank[it[1]][it[2]]))
                elif it[0] == "c":
                    idx += 1
                    lst.append(("c", it[1], idx in need[e]))
                else:
                    lst.append(it)
            out[e] = lst
        self.pruned = out
        return out

    def emit(self):
        nc = self.nc
        self.barrier()
        sems = self.sems
        streams = self.prune()

        def run(name, eng):
            for it in streams[name]:
                if it[0] == "w":
                    eng.wait_ge(sems[it[1]], it[2])
                elif it[0] == "c":
                    ins = it[1](eng)
                    if it[2]:
                        ins.then_inc(sems[name], 1)
                elif it[0] == "x":
                    it[1](eng).then_inc(sems[it[2]], 1)
                else:
                    eng.dma_start(out=it[1], in_=it[2], **it[4]).then_inc(sems[it[3]], 16)

        with nc.Block() as block:
            @block.tensor
            def _(e):
                run("pe", e)

            @block.scalar
            def _(e):
                run("act", e)

            @block.vector
            def _(e):
                run("dve", e)

            @block.gpsimd
            def _(e):
                run("pool", e)

            @block.sync
            def _(e):
                run("sp", e)


class Ctx:
    def __init__(self, nc, es, pfx=""):
        self.nc = nc
        self.es = es
        self.pfx = pfx

    def sb(self, name, shape, dt=F32):
        return self.es.enter_context(self.nc.sbuf_tensor(self.pfx + name, list(shape), dt))

    def ps(self, name, shape, dt=F32):
        return self.es.enter_context(self.nc.psum_tensor(name, list(shape), dt))

    def din(self, name, shape, dt=F32):
        return self.nc.dram_tensor(name, list(shape), dt, kind="ExternalInput").ap()

    def dout(self, name, shape, dt=F32):
        return self.nc.dram_tensor(name, list(shape), dt, kind="ExternalOutput").ap()


def emit_mod(P, C, banks, ccT_d, wmod_d, bmod_d, groups, rows_d, tag):
    nc = P.nc
    ng = len(groups)
    modB = C.sb(tag + "modB", [128, 2, ng, 1024])
    rowB = C.sb(tag + "rowB", [128, max(1, len(rows_d)), 1024])
    with ExitStack() as es2:
        C2 = Ctx(nc, es2, C.pfx)
        ccT = C2.sb(tag + "ccT", [128, 16])
        scT = C2.sb(tag + "scT", [128, 16])
        ones = C2.sb(tag + "ones", [128, 128])
        crep = C2.sb(tag + "crep", [128, 16, 128])
        wm = [C2.sb(tag + "wm%d" % i, [128, 8, 512]) for i in range(2)]
        rowt = C2.sb(tag + "rowt", [1, 1024])
        brow = C2.sb(tag + "brow", [1, 1024])
        P.dma(ccT[:], ccT_d, writes=[tag + "ccT"])
        P.act(scT[:], ccT[:], AF.Silu, [tag + "ccT"], [tag + "scT"])
        P.memset("pool", ones[:], 1.0, [tag + "ones"])
        for jc in range(16):
            P.ts("dve", crep[:, jc, :], ones[:], scT[:, jc:jc + 1], None, ALU.mult, None,
                 [tag + "ones", tag + "scT"], [tag + "crep"])
        bi = 0
        for ri, rd in enumerate(rows_d):
            P.dma(rowt[:], rd, writes=[tag + "rowt"])
            for hf in range(2):
                b = banks[bi % 2]
                bi += 1
                P.mm(b[0][:], ones[0:1, :], rowt[0:1, hf * 512:(hf + 1) * 512], True, True,
                     [tag + "ones", tag + "rowt"], [b[1]])
                P.cp("act", rowB[:, ri, hf * 512:(hf + 1) * 512], b[0][:], [b[1]], [tag + "rowB"])
        wv = wmod_d.rearrange("(c p) n -> p c n", p=128)
        li = 0
        for gi, g in enumerate(groups):
            P.dma(brow[:], bmod_d[0:1, g * 1024:(g + 1) * 1024], writes=[tag + "brow"])
            for hf in range(2):
                col0 = g * 1024 + hf * 512
                w = wm[li % 2]
                wk = tag + "wm%d" % (li % 2)
                li += 1
                P.dma(w[:], wv[:, :, col0:col0 + 512], writes=[wk])
                for j in range(2):
                    b = banks[bi % 2]
                    bi += 1
                    for c in range(8):
                        P.mm(b[0][:], crep[:, j * 8 + c, :], w[:, c, :], c == 0, False,
                             [tag + "crep", wk], [b[1]])
                    P.mm(b[0][:], ones[0:1, :], brow[0:1, hf * 512:(hf + 1) * 512], False, True,
                         [tag + "ones", tag + "brow"], [b[1]])
                    P.cp("act", modB[:, j, gi, hf * 512:(hf + 1) * 512], b[0][:], [b[1]], [tag + "modB"])
        P.barrier()
    return modB, rowB


def emit_rstd(P, ss, tag, n=D):
    P.ts("dve", ss[:, 1:2], ss[:, 0:1], 1.0 / n, 1e-6, ALU.mult, ALU.add, [tag + "ss"], [tag + "ss1"])
    P.act(ss[:, 3:4], ss[:, 1:2], AF.Ln, [tag + "ss1"], [tag + "ss3"])
    P.act(ss[:, 2:3], ss[:, 3:4], AF.Exp, [tag + "ss3"], [tag + "ss2"], scale=-0.5)


def emit_norm_mod(P, xt, xkey, G, S, gskeys, junk, ss, tmp, hf_out, hkey, tag):
    P.act(tmp[:], xt, AF.Square, [xkey], [tag + "tmp", tag + "ss"], accum_out=ss[:, 0:1])
    emit_rstd(P, ss, tag)
    P.stt(tmp[:], xt, ss[:, 2:3], G, ALU.mult, ALU.mult, [xkey, tag + "ss2"] + gskeys, [tag + "tmp"])
    P.tt("pool", hf_out, tmp[:], S, ALU.add, [tag + "tmp"] + gskeys, [hkey])


def emit_ffn(P, nc, C, banks, kind, tiles_spec, dd, final_norm):
    ntile = len(tiles_spec)
    tl = tiles_spec
    FF = 2816 if kind == "dense" else 3584
    NE = 1 if kind == "dense" else 8
    ccT_d, wmod_d, bmod_d, nffn_d, nf_d = dd["ccT"], dd["w_mod"], dd["b_mod"], dd["norm_ffn"], dd["norm_f"]
    wg_d, wu_d, wd_d = dd["wg"], dd["wu"], dd["wd"]
    wrT_d = dd.get("wrT")

    xall = C.sb("xall", [128, ntile, D])
    hT = C.sb("hT", [128, 8, ntile * 128], BF16)
    gateB = C.sb("gateB", [128, 2, D])
    nfB = C.sb("nfB", [128, D]) if final_norm else None
    comb = C.sb("comb", [128, ntile, 8])
    ss = C.sb("ss", [128, 4])
    with ExitStack() as e1:
        C1 = Ctx(nc, e1, C.pfx)
        modB, rowB = emit_mod(P, C1, banks, ccT_d, wmod_d, bmod_d, [3, 4, 5], [nffn_d, nf_d], "m_")
        for j in range(2):
            P.stt(modB[:, j, 1, :], modB[:, j, 1, :], 1.0, rowB[:, 0, :], ALU.add, ALU.mult,
                  ["m_modB", "m_rowB"], ["m_modB"])
            P.cp("pool", gateB[:, j, :], modB[:, j, 2, :], ["m_modB"], ["gateB"])
        if final_norm:
            P.cp("pool", nfB[:], rowB[:, 1, :], ["m_rowB"], ["nfB"])
        ident = C1.sb("ident", [128, 128])
        P.memset("pool", ident[:], 1.0, ["ident"])
        _asel(P, ident[:], "ident", [[-1, 128]], ALU.is_equal, 0, 1)
        tmp = C1.sb("tmp", [128, D])
        hf = C1.sb("hf", [128, D])
        if kind == "moe":
            junk = C1.sb("junk", [128, D])
            wrB = C1.sb("wrB", [128, 8, D])
            ones1 = C1.sb("ones1", [1, 128])
            P.memset("pool", ones1[:], 1.0, ["ones1"])
            for e_ in range(8):
                P.dma(junk[0:1, :], wrT_d[e_:e_ + 1, :], writes=["n_junk"])
                for h2 in range(2):
                    b = banks[h2]
                    P.mm(b[0][:], ones1[0:1, :], junk[0:1, h2 * 512:(h2 + 1) * 512], True, True,
                         ["ones1", "n_junk"], [b[1]])
                    P.cp("act", wrB[:, e_, h2 * 512:(h2 + 1) * 512], b[0][:], [b[1]], ["wrB"])
            logit = C1.sb("logit", [128, 8])
            top8 = C1.sb("top8", [128, 8])
            cb2 = C1.sb("cb2", [128, 8])
            rt = C1.sb("rt", [128, 8])
        for t in range(ntile):
            j = tl[t][4]
            xk = "xall%d" % t
            P.dma(xall[:, t, :], tl[t][0], reads=[tl[t][1]], writes=[xk])
            emit_norm_mod(P, xall[:, t, :], xk, modB[:, j, 1, :], modB[:, j, 0, :], ["m_modB"],
                          None, ss, tmp, hf[:], "hf", "n_")
            if kind == "moe":
                for e_ in range(8):
                    P.op("dve", (lambda e, e_=e_: e.scalar_tensor_tensor(
                        junk[:], hf[:], 1.0, wrB[:, e_, :], ALU.mult, ALU.mult, accum_out=logit[:, e_:e_ + 1])),
                        ["hf", "wrB"], ["n_junk", "logit"])
                P.op("dve", lambda e: e.max(top8[:], logit[:]), ["logit"], ["top8"])
                P.tt("dve", rt[:, 0:1], top8[:, 1:2], top8[:, 0:1], ALU.subtract, ["top8"], ["rt"])
                P.act(rt[:, 1:2], rt[:, 0:1], AF.Exp, ["rt"], ["rt"])
                P.ts("dve", rt[:, 2:3], rt[:, 1:2], 1.0, None, ALU.add, None, ["rt"], ["rt"])
                P.op("dve", lambda e: e.reciprocal(rt[:, 3:4], rt[:, 2:3]), ["rt"], ["rt"])
                P.tt("dve", rt[:, 4:5], rt[:, 1:2], rt[:, 3:4], ALU.mult, ["rt"], ["rt"])
                P.ts("dve", comb[:, t, :], logit[:], top8[:, 0:1], rt[:, 3:4], ALU.is_equal, ALU.mult,
                     ["logit", "top8", "rt"], ["comb"])
                P.ts("dve", cb2[:], logit[:], top8[:, 1:2], rt[:, 4:5], ALU.is_equal, ALU.mult,
                     ["logit", "top8", "rt"], ["cb2"])
                P.tt("dve", comb[:, t, :], comb[:, t, :], cb2[:], ALU.add, ["comb", "cb2"], ["comb"])
            for half in range(2):
                b = banks[half]
                for c4 in range(4):
                    c = half * 4 + c4
                    P.tr(b[0][:, c4 * 128:(c4 + 1) * 128], hf[:, c * 128:(c + 1) * 128], ident[:],
                         ["hf", "ident"], [b[1]])
                P.cp("act", hT[:, half * 4:(half + 1) * 4, t * 128:(t + 1) * 128],
                     b[0][:].rearrange("p (c t) -> p c t", c=4), [b[1]], ["hT"])
        P.barrier()
    with ExitStack() as e2:
        C2 = Ctx(nc, e2, C.pfx)
        stg = [C2.sb("stg%d" % i, [128, 2048]) for i in range(3)]
        wgb = [C2.sb("wgb%d" % i, [128, 8, 512], BF16) for i in range(2)]
        wub = [C2.sb("wub%d" % i, [128, 8, 512], BF16) for i in range(2)]
        dbf = [C2.sb("dbf%d" % i, [128, 4, D], BF16) for i in range(2)]
        actT = [C2.sb("actT%d" % i, [128, 4, 512], BF16) for i in range(2)]
        sg = [C2.sb("sg%d" % i, [128, 512], BF16) for i in range(2)]
        tmp2 = C2.sb("tmp2", [128, 2, 512])
        nst = [0]
        cnt = {"g": 0, "a": 0, "b": 0, "t": 0}

        def stage_cast(src3, dst3, dkey, eng):
            k = nst[0] % 3
            nst[0] += 1
            a, w = src3.shape[1], src3.shape[2]
            v = stg[k][:].rearrange("p (a w) -> p a w", a=a)
            P.dma(v, src3, writes=["stg%d" % k])
            P.cp(eng, dst3, v, ["stg%d" % k], [dkey])

        def prefetch(pi, grp=None):
            calls = []
            sc = lambda *a: calls.append(a)
            _prefetch_list(pi, sc)
            n = len(calls)
            sel_ = range(n) if grp is None else range((grp * n) // 3, ((grp + 1) * n) // 3)
            for i in sel_:
                stage_cast(*calls[i])

        def _prefetch_list(pi, stage_cast):
            e_, g0, gw = parts[pi]
            pb = pi % 2
            nch = gw // 128
            wgv = wg_d[e_].rearrange("(c p) n -> p c n", p=128)
            wuv = wu_d[e_].rearrange("(c p) n -> p c n", p=128)
            wdv = wd_d[e_].rearrange("(c p) n -> p c n", p=128)
            if gw == 512:
                for c0 in range(0, 8, 4):
                    stage_cast(wgv[:, c0:c0 + 4, g0:g0 + 512], wgb[pb][:, c0:c0 + 4, :], "wgb%d" % pb, "act")
                    stage_cast(wuv[:, c0:c0 + 4, g0:g0 + 512], wub[pb][:, c0:c0 + 4, :], "wub%d" % pb, "act")
            else:
                stage_cast(wgv[:, :, g0:g0 + 256], wgb[pb][:, :, 0:256], "wgb%d" % pb, "act")
                stage_cast(wuv[:, :, g0:g0 + 256], wub[pb][:, :, 0:256], "wub%d" % pb, "act")
            for o in range(0, nch, 2):
                c0 = g0 // 128 + o
                stage_cast(wdv[:, c0:c0 + 2, :], dbf[pb][:, o:o + 2, :], "dbf%d" % pb, "pool")

        parts = [(e_, g0, min(512, FF - g0)) for e_ in range(NE) for g0 in range(0, FF, 512)]
        blocks = [list(range(b0, min(ntile, b0 + 4))) for b0 in range(0, ntile, 4)]
        pendB = [None]

        def phaseB(blk, ab, pb, nch, e_):
            for ti, t in enumerate(blk):
                j = tl[t][4]
                for h2 in range(2):
                    bi = cnt["b"] % 4
                    cnt["b"] += 1
                    b = banks[4 + bi]
                    for jj in range(nch):
                        P.mm(b[0][:], actT[ab][:, jj, ti * 128:(ti + 1) * 128], dbf[pb][:, jj, h2 * 512:(h2 + 1) * 512],
                             jj == 0, jj == nch - 1, ["actT%d" % ab, "dbf%d" % pb], [b[1]])
                    cs = slice(h2 * 512, (h2 + 1) * 512)
                    tk = cnt["t"] % 2
                    cnt["t"] += 1
                    if kind == "dense":
                        P.tt("dve", tmp2[:, tk, :], b[0][:], gateB[:, j, cs], ALU.mult, [b[1], "gateB"], ["tmp2_%d" % tk])
                    else:
                        P.stt(tmp2[:, tk, :], b[0][:], comb[:, t, e_:e_ + 1], gateB[:, j, cs], ALU.mult, ALU.mult,
                              [b[1], "gateB", "comb"], ["tmp2_%d" % tk])
                    P.tt("pool", xall[:, t, cs], xall[:, t, cs], tmp2[:, tk, :], ALU.add,
                         ["tmp2_%d" % tk, "xall%d" % t], ["xall%d" % t])

        prefetch(0)
        for pi, (e_, g0, gw) in enumerate(parts):
            pb = pi % 2
            nch = gw // 128
            for bix, blk in enumerate(blocks):
                if pi + 1 < len(parts) and 1 <= bix <= 3:
                    prefetch(pi + 1, bix - 1)
                nbt = len(blk) * 128
                t0_ = blk[0] * 128
                ab = cnt["a"] % 2
                cnt["a"] += 1
                for jj in range(nch):
                    gi = cnt["g"] % 2
                    cnt["g"] += 1
                    bG = banks[2 * gi]
                    bU = banks[2 * gi + 1]
                    for c in range(8):
                        P.mm(bG[0][:, 0:nbt], wgb[pb][:, c, jj * 128:(jj + 1) * 128], hT[:, c, t0_:t0_ + nbt],
                             c == 0, c == 7, ["wgb%d" % pb, "hT"], [bG[1]])
                    for c in range(8):
                        P.mm(bU[0][:, 0:nbt], wub[pb][:, c, jj * 128:(jj + 1) * 128], hT[:, c, t0_:t0_ + nbt],
                             c == 0, c == 7, ["wub%d" % pb, "hT"], [bU[1]])
                    P.act(sg[gi][:, 0:nbt], bG[0][:, 0:nbt], AF.Silu, [bG[1]], ["sg%d" % gi])
                    P.tt("dve", actT[ab][:, jj, 0:nbt], sg[gi][:, 0:nbt], bU[0][:, 0:nbt], ALU.mult,
                         ["sg%d" % gi, bU[1]], ["actT%d" % ab])
                if pendB[0] is not None:
                    phaseB(*pendB[0])
                pendB[0] = (blk, ab, pb, nch, e_)
        if pendB[0] is not None:
            phaseB(*pendB[0])
        for t in range(ntile):
            xk = "xall%d" % t
            if final_norm:
                P.act(tmp2[:].rearrange("p a b -> p (a b)"), xall[:, t, :], AF.Square, [xk], ["tmp2_0", "tmp2_1", "n_ss"],
                      accum_out=ss[:, 0:1])
                emit_rstd(P, ss, "n_")
                P.stt(xall[:, t, :], xall[:, t, :], ss[:, 2:3], nfB[:], ALU.mult, ALU.mult,
                      [xk, "n_ss2", "nfB"], [xk])
            P.dma(tl[t][2], xall[:, t, :], reads=[xk], writes=[tl[t][3]])
        P.barrier()


NKT = 34


def _asel(P, t, key, pattern, op, base, cm):
    P.op("pool", lambda e: e.affine_select(out=t, in_=t, pattern=pattern, compare_op=op, fill=0.0,
                                            base=base, channel_multiplier=cm), [key], [key])


def emit_rope(P, eng, dst, src, cs, sn, H, hw, r1, r2, rkeys, skey, dkey):
    X = src.rearrange("p (h a b i) -> p h a b i", h=H, a=2, b=2, i=hw)
    Y = dst.rearrange("p (h a b i) -> p h a b i", h=H, a=2, b=2, i=hw)
    R1 = r1.rearrange("p (h a i) -> p h a i", h=H, a=2, i=hw)
    R2 = r2.rearrange("p (h a i) -> p h a i", h=H, a=2, i=hw)
    cb = cs.rearrange("p (a i) -> p a i", a=2).unsqueeze(1).to_broadcast([128, H, 2, hw])
    sb = sn.rearrange("p (a i) -> p a i", a=2).unsqueeze(1).to_broadcast([128, H, 2, hw])
    x1 = X[:, :, :, 0, :]
    x2 = X[:, :, :, 1, :]
    P.tt(eng, R1, x1, cb, ALU.mult, [skey] + rkeys, ["rp1"])
    P.tt(eng, R2, x2, sb, ALU.mult, [skey] + rkeys, ["rp2"])
    P.tt(eng, Y[:, :, :, 0, :], R1, R2, ALU.subtract, ["rp1", "rp2"], [dkey])
    P.tt(eng, R1, x1, sb, ALU.mult, [skey] + rkeys, ["rp1"])
    P.tt(eng, R2, x2, cb, ALU.mult, [skey] + rkeys, ["rp2"])
    P.tt(eng, Y[:, :, :, 1, :], R1, R2, ALU.add, ["rp1", "rp2"], [dkey])


def emit_mixer(P, nc, C, banks, need_ctx, lam_init, dd):
    debug = False
    ccT_d, wmod_d, bmod_d, nmix_d = dd["ccT"], dd["w_mod"], dd["b_mod"], dd["norm_mix"]
    win_d, wout_d = dd["w_in"], dd["w_out"]
    w4f_d, w4b_d, a2f_d, a2b_d, rows_d = dd["w4Tf"], dd["w4Tb"], dd["a2f"], dd["a2b"], dd["rows"]
    ropeO = dd["ropeO"]
    PS2 = banks[0][2]
    B = [b[0] for b in banks]
    BK = [b[1] for b in banks]

    modB, rowB0 = emit_mod(P, C, banks, ccT_d, wmod_d, bmod_d, [0, 1, 2], [nmix_d], "m_")
    for j in range(2):
        P.stt(modB[:, j, 1, :], modB[:, j, 1, :], 1.0, rowB0[:, 0, :], ALU.add, ALU.mult,
              ["m_modB", "m_rowB"], ["m_modB"])
    MK = ["m_modB"]

    ident = C.sb("ident", [128, 128])
    P.memset("pool", ident[:], 1.0, ["ident"])
    _asel(P, ident[:], "ident", [[-1, 128]], ALU.is_equal, 0, 1)
    msk = C.sb("msk", [128, 6, 128])
    for i in range(4):
        P.memset("pool", msk[:, i, :], -1.0 / 16.0, ["msk"])
    for i in range(4, 6):
        P.memset("pool", msk[:, i, :], 1.0, ["msk"])
    _asel(P, msk[:, 0, :], "msk", [[1, 128]], ALU.is_ge, 0, -1)
    _asel(P, msk[:, 1, :], "msk", [[-1, 128]], ALU.is_ge, 0, 1)
    _asel(P, msk[:, 2, :], "msk", [[-1, 128]], ALU.is_gt, 0, 1)
    _asel(P, msk[:, 3, :], "msk", [[1, 128]], ALU.is_gt, 0, -1)
    _asel(P, msk[:, 4, :], "msk", [[1, 128]], ALU.is_ge, 0, -1)
    _asel(P, msk[:, 5, :], "msk", [[-1, 128]], ALU.is_ge, 0, 1)
    bd = C.sb("bd", [128, 256])
    P.memset("pool", bd[:], 1.0, ["bd"])
    for h in range(4):
        _asel(P, bd[:, h * 64:(h + 1) * 64], "bd", [[0, 64]], ALU.is_ge, -32 * h, 1)
        _asel(P, bd[:, h * 64:(h + 1) * 64], "bd", [[0, 64]], ALU.is_ge, 32 * h + 31, -1)
    sel = C.sb("sel", [128, 4])
    P.dma(sel[:, 0:2], dd["sel"], writes=["sel"])
    negcol = C.sb("negcol", [128, 1])
    P.memset("pool", negcol[:], -1.0 / 16.0, ["negcol"])
    ones1 = C.sb("ones1", [1, 128])
    P.memset("pool", ones1[:], 1.0, ["ones1"])
    rB = C.sb("rB", [128, 1536])
    with ExitStack() as es0:
        rowS = Ctx(nc, es0, C.pfx).sb("rowS", [1, 2048])
        P.dma(rowS[:], rows_d, writes=["rowS"])
        for i in range(3):
            P.mm(B[i][:], ones1[0:1, :], rowS[0:1, i * 512:(i + 1) * 512], True, True, ["ones1", "rowS"], [BK[i]])
            P.cp("act", rB[:, i * 512:(i + 1) * 512], B[i][:], [BK[i]], ["rB"])
        P.barrier()
    abB = rB[:, 0:256]
    gnB = rB[:, 256:512]
    qnB = rB[:, 512:1024]
    knB = rB[:, 1024:1152]
    dnB = rB[:, 1152:1408]
    lmB = rB[:, 1408:1536]
    lam = C.sb("lam", [128, 8])
    junk = C.sb("junk", [128, 32])
    P.op("dve", lambda e: e.scalar_tensor_tensor(junk[:, 0:32], lmB[:, 0:32], 1.0, lmB[:, 32:64], ALU.mult, ALU.mult,
                                                  accum_out=lam[:, 0:1]), ["rB"], ["n_junk", "lam"])
    P.op("dve", lambda e: e.scalar_tensor_tensor(junk[:, 0:32], lmB[:, 64:96], 1.0, lmB[:, 96:128], ALU.mult, ALU.mult,
                                                  accum_out=lam[:, 1:2]), ["rB"], ["n_junk", "lam"])
    P.act(lam[:, 2:4], lam[:, 0:2], AF.Exp, ["lam"], ["lam2"])
    P.tt("dve", lam[:, 4:5], lam[:, 3:4], lam[:, 2:3], ALU.subtract, ["lam2"], ["lam3"])
    P.ts("dve", lam[:, 5:6], lam[:, 4:5], -lam_init, None, ALU.add, None, ["lam3"], ["nlam"])
    nlam = lam[:, 5:6]
    P.ts("dve", rB[:, 512:1024], rB[:, 512:1024], 0.125, None, ALU.mult, None, ["rB"], ["rB"])
    P.ts("dve", rB[:, 1152:1408], rB[:, 1152:1408], 1.0 - lam_init, None, ALU.mult, None, ["rB"], ["rB"])

    KT = C.sb("KT", [64, 2, NKT * 128], BF16)
    Vg = C.sb("Vg", [128, NKT, 2, 65], BF16)
    KTd = C.sb("KTd", [128, 2, NKT * 128], BF16)
    Vd = C.sb("Vd", [128, NKT, 4, 65], BF16)
    P.memset("pool", Vg[:, :, :, 64:65], 1.0, ["Vg"])
    P.memset("pool", Vd[:, :, :, 64:65], 1.0, ["Vd"])
    glaout = C.sb("glaout", [128, 18, 256], BF16)
    ofS = glaout
    xt = C.sb("xt", [128, D])
    tmp = C.sb("tmp", [128, D])
    hf = C.sb("hf", [128, D])
    ss = C.sb("ss", [128, 4])
    hT = C.sb("hT", [128, 8, 128], BF16)
    sq = C.sb("sq", [128, 512])
    s8 = C.sb("s8", [128, 32])
    fa = C.sb("fa", [128, 512])
    fb = C.sb("fb", [128, 512])
    r1 = C.sb("r1", [128, 256])
    r2 = C.sb("r2", [128, 256])
    cG = C.sb("cG", [128, 64])
    cD = C.sb("cD", [128, 32])
    wst = [None, None]

    def load_w(dst, dcol, src_d, scol, width):
        for o in range(0, width, 256):
            w = min(256, width - o)
            k = load_w.n % 2
            load_w.n += 1
            P.dma(wst[k][:, :, 0:w], src_d.rearrange("(c p) n -> p c n", p=128)[:, :, scol + o:scol + o + w],
                  writes=["wst%d" % k])
            P.cp("pool", dst[:, :, dcol + o:dcol + o + w], wst[k][:, :, 0:w], ["wst%d" % k], ["W"])
    load_w.n = 0

    def load_h(src, j, rope):
        src_ap, src_key = src
        P.dma(xt[:], src_ap, reads=[src_key], writes=["xt"])
        if rope is not None:
            tabs, rope_row0 = rope
            P.dma(cG[:, 0:32], tabs[0][rope_row0:rope_row0 + 128, :], writes=["cG"])
            P.dma(cG[:, 32:64], tabs[1][rope_row0:rope_row0 + 128, :], writes=["cG"])
            P.dma(cD[:, 0:16], tabs[2][rope_row0:rope_row0 + 128, :], writes=["cD"])
            P.dma(cD[:, 16:32], tabs[3][rope_row0:rope_row0 + 128, :], writes=["cD"])
        emit_norm_mod(P, xt[:], "xt", modB[:, j, 1, :], modB[:, j, 0, :], MK, junk, ss, tmp, hf[:], "hf", "n_")
        for half in range(2):
            b = banks[3 + half]
            for c4 in range(4):
                c = half * 4 + c4
                P.tr(b[0][:, c4 * 128:(c4 + 1) * 128], hf[:, c * 128:(c + 1) * 128], ident[:], ["hf", "ident"], [b[1]])
            P.cp("act", hT[:, half * 4:(half + 1) * 4, :], b[0][:].rearrange("p (c t) -> p c t", c=4), [b[1]], ["hT"])

    def head_rstd(src_ps, src_key, H, dh, out_s8):
        P.act(sq[:, 0:H * dh], src_ps, AF.Square, [src_key], ["sq"])
        P.op("dve", lambda e: e.tensor_reduce(out=s8[:, 8:8 + H], in_=sq[:, 0:H * dh].rearrange("p (h d) -> p h d", h=H),
                                               axis=AX.X, op=ALU.add), ["sq"], ["s8a"])
        P.ts("dve", s8[:, 16:16 + H], s8[:, 8:8 + H], 1.0 / dh, 1e-6, ALU.mult, ALU.add, ["s8a"], ["s8b"])
        P.act(s8[:, 24:24 + H], s8[:, 16:16 + H], AF.Ln, ["s8b"], ["s8c"])
        P.act(out_s8, s8[:, 24:24 + H], AF.Exp, ["s8c"], ["s8"], scale=-0.5)

    with ExitStack() as es1:
        C1 = Ctx(nc, es1, C.pfx)
        wi1 = C1.sb("wi1", [128, 8, 1536], BF16)
        esw = ExitStack()
        Cw = Ctx(nc, esw, C.pfx)
        wst[0] = Cw.sb("wst0", [128, 8, 256])
        wst[1] = Cw.sb("wst1", [128, 8, 256])
        w4 = Cw.sb("w4", [16, 2, D])
        a2 = Cw.sb("a2", [16, 2, 128])
        P.dma(w4[:, 0, :], w4f_d, writes=["w4"])
        P.dma(w4[:, 1, :], w4b_d, writes=["w4"])
        P.dma(a2[:, 0, :], a2f_d, writes=["a2"])
        P.dma(a2[:, 1, :], a2b_d, writes=["a2"])
        for dr in range(2):
            for c in range(8):
                b = banks[c % 2]
                P.mm(b[0][:, 0:128], w4[:, dr, c * 128:(c + 1) * 128], a2[:, dr, :], True, True, ["w4", "a2"], [b[1]])
                P.cp("act", wi1[:, c, 256 + dr * 128:256 + (dr + 1) * 128], b[0][:, 0:128], [b[1]], ["W"])
        load_w(wi1, 0, win_d, 0, 256)
        load_w(wi1, 512, win_d, 256, 256)
        load_w(wi1, 768, win_d, 1312, 256)
        load_w(wi1, 1024, win_d, 2080, 256)
        load_w(wi1, 1280, win_d, 1824, 256)
        P.barrier()
        esw.close()
        NST = 18
        qkB = C1.sb("qkB", [128, NST, 2, 128], BF16)
        qkF = C1.sb("qkF", [128, 2, 128], BF16)
        kdB = C1.sb("kdB", [128, NST, 128], BF16)
        kdF = C1.sb("kdF", [128, 128], BF16)
        vS = C1.sb("vS", [128, NST, 256], BF16)
        dec = C1.sb("dec", [128, NST, 2])
        zt = C1.sb("zt", [128, 256])
        sp = C1.sb("sp", [128, 256])
        Eq = C1.sb("Eq", [128, 256])
        Ek = C1.sb("Ek", [128, 256])
        Ed = C1.sb("Ed", [128, 256])
        qk = C1.sb("qk", [128, 256])
        qdki = C1.sb("qdki", [128, 4, 128])
        kim = C1.sb("kim", [128, 4, 128], BF16)
        AT = C1.sb("AT", [128, 4, 128], BF16)
        KVm = C1.sb("KVm", [128, 256])
        S = [C1.sb("S%d" % i, [128, 256]) for i in range(2)]
        Sb = [C1.sb("Sb%d" % i, [128, 256], BF16) for i in range(2)]
        og = C1.sb("og", [128, 256])
        for i in range(2):
            P.memset("pool", S[i][:], 0.0, ["S%d" % i])
            P.memset("pool", Sb[i][:], 0.0, ["Sb%d" % i])

        def proj1():
            for g in range(3):
                for c in range(8):
                    P.mm(B[g][:], hT[:, c, :], wi1[:, c, g * 512:(g + 1) * 512], c == 0, c == 7, ["hT", "W"], [BK[g]])

        def gla_prep(st, full):
            P.tt("dve", zt[:], B[0][:, 256:512], abB, ALU.add, [BK[0], "rB"], ["zt"])
            P.act(Eq[:], zt[:], AF.Exp, ["zt"], ["Eq"], scale=-1.0)
            P.act(sp[:], Eq[:], AF.Ln, ["Eq"], ["sp"], bias=1.0)
            b3 = B[5]
            P.mm(b3[:, 0:128], msk[:, 0, :], sp[:, 0:128], True, True, ["msk", "sp"], [BK[5]])
            P.mm(b3[:, 128:256], msk[:, 1, :], sp[:, 128:256], True, True, ["msk", "sp"], [BK[5]])
            P.mm(b3[:, 256:384], msk[:, 2, :], sp[:, 0:128], True, True, ["msk", "sp"], [BK[5]])
            P.mm(b3[:, 384:512], msk[:, 3 if full else 2, :], sp[:, 128:256], True, True, ["msk", "sp"], [BK[5]])
            P.mm(B[6][:, 0:1], sp[:, 0:128], negcol[:], True, True, ["sp", "negcol"], [BK[6]])
            P.mm(B[6][:, 1:2], sp[:, 128:256], negcol[:], True, True, ["sp", "negcol"], [BK[6]])
            P.act(dec[:, st, :], B[6][:, 0:2], AF.Exp, [BK[6]], ["dec"])
            P.act(Ed[:], b3[:, 256:512], AF.Exp, [BK[5]], ["Ed"])
            P.cp("act", qk[:], B[0][:, 0:256], [BK[0]], ["qk"])
            P.tt("dve", kdB[:, st, :], qk[:, 128:256], Ed[:, 128:256], ALU.mult, ["qk", "Ed"], ["kdB"])
            if full:
                P.tt("dve", kdF[:], qk[:, 128:256], Ed[:, 0:128], ALU.mult, ["qk", "Ed"], ["kdF"])
            P.cp("act", vS[:, st, :], B[1][:, 0:256], [BK[1]], ["vS"])
            if not full:
                return
            P.act(Eq[:], b3[:, 0:256], AF.Exp, [BK[5]], ["Eq"])
            P.act(Ek[:], b3[:, 0:256], AF.Exp, [BK[5]], ["Ek"], scale=-1.0)
            for dr in range(2):
                P.stt(qdki[:, 2 * dr, :], qk[:, 0:128], 32.0 ** -0.5, Eq[:, dr * 128:(dr + 1) * 128], ALU.mult, ALU.mult,
                      ["qk", "Eq"], ["qdki"])
                P.tt("dve", qdki[:, 2 * dr + 1, :], qk[:, 128:256], Ek[:, dr * 128:(dr + 1) * 128], ALU.mult,
                     ["qk", "Ek"], ["qdki"])
            for i in range(4):
                P.tr(B[7][:, i * 128:(i + 1) * 128], qdki[:, i, :], ident[:], ["qdki", "ident"], [BK[7]])
            P.cp("act", qkF[:], B[7][:, 0:256].rearrange("p (i t) -> p i t", i=2), [BK[7]], ["qkF"])
            P.cp("act", qkB[:, st, :, :], B[7][:, 256:512].rearrange("p (i t) -> p i t", i=2), [BK[7]], ["qkB"])

        def kv_prep(kidx, rope):
            head_rstd(B[1][:, 256:384], BK[1], 2, 64, s8[:, 0:2])
            P.tt("dve", fa[:, 0:128].rearrange("p (h d) -> p h d", h=2), B[1][:, 256:384].rearrange("p (h d) -> p h d", h=2),
                 s8[:, 0:2].unsqueeze(2).to_broadcast([128, 2, 64]), ALU.mult, [BK[1], "s8"], ["fa"])
            P.tt("pool", fb[:, 0:128], fa[:, 0:128], knB, ALU.mult, ["fa", "rB"], ["fb"])
            src = fb
            if rope:
                emit_rope(P, "pool", fa[:, 0:128], fb[:, 0:128], cG[:, 0:32], cG[:, 32:64], 2, 16,
                          r1[:, 0:64], r2[:, 0:64], ["cG"], "fb", "fa")
                src = fa
            skey = "fa" if rope else "fb"
            for h in range(2):
                P.tr(B[7][0:64, h * 128:(h + 1) * 128], src[:, h * 64:(h + 1) * 64], ident[:], [skey, "ident"], [BK[7]])
            P.cp("act", KT[:, :, kidx * 128:(kidx + 1) * 128], B[7][0:64, 0:256].rearrange("p (h t) -> p h t", h=2),
                 [BK[7]], ["KT"])
            P.cp("act", Vg[:, kidx, :, 0:64], B[1][:, 384:512].rearrange("p (h d) -> p h d", h=2), [BK[1]], ["Vg"])
            P.cp("act", Vd[:, kidx, :, 0:64], B[2][:, 0:256].rearrange("p (h d) -> p h d", h=4), [BK[2]], ["Vd"])
            P.cp("act", fb[:, 256:512], B[2][:, 256:512], [BK[2]], ["fb2"])
            src2, k2 = fb[:, 256:512], "fb2"
            if rope:
                emit_rope(P, "pool", fa[:, 256:512], fb[:, 256:512], cD[:, 0:16], cD[:, 16:32], 8, 8,
                          r1[:, 0:128], r2[:, 0:128], ["cD"], "fb2", "fa2")
                src2, k2 = fa[:, 256:512], "fa2"
            for g in range(2):
                P.tr(B[7][:, 256 + g * 128:256 + (g + 1) * 128], src2[:, g * 128:(g + 1) * 128], ident[:],
                     [k2, "ident"], [BK[7]])
            P.cp("act", KTd[:, :, kidx * 128:(kidx + 1) * 128], B[7][:, 256:512].rearrange("p (g t) -> p g t", g=2),
                 [BK[7]], ["KTd"])

        def scan_step(dr, st, with_out, final_to=None, slot=None):
            Sd, Sbd = S[dr], Sb[dr]
            sk, sbk = "S%d" % dr, "Sb%d" % dr
            if dr == 0:
                qd_, ki_, kd_, qkk, kdk = qkF[:, 0, :], qkF[:, 1, :], kdF[:], "qkF", "kdF"
            else:
                qd_, ki_, kd_, qkk, kdk = qkB[:, st, 0, :], qkB[:, st, 1, :], kdB[:, st, :], "qkB", "kdB"
            if with_out:
                for h in range(4):
                    P.ts("pool", kim[:, h, :], ki_, bd[:, 64 * h:64 * h + 1], None, ALU.mult, None,
                         [qkk, "bd"], ["kim"])
                for h in range(4):
                    P.mm(B[5][:, h * 128:(h + 1) * 128], kim[:, h, :], qd_, True, True, ["kim", qkk], [BK[5]])
                P.tt("dve", AT[:], B[5][:].rearrange("p (h c) -> p h c", h=4),
                     msk[:, 4 + dr, :].unsqueeze(1).to_broadcast([128, 4, 128]), ALU.mult, [BK[5], "msk"], ["AT"])
                for h in range(4):
                    P.mm(B[7][:, h * 64:(h + 1) * 64], AT[:, h, :], vS[:, st, h * 64:(h + 1) * 64], True, False,
                         ["AT", "vS"], [BK[7]])
                    P.mm(B[7][:, h * 64:(h + 1) * 64], qd_, Sbd[:, h * 64:(h + 1) * 64], False, True,
                         [qkk, sbk], [BK[7]])
                if final_to is None:
                    P.cp("act", ofS[:, st, :], B[7][:, 0:256], [BK[7]], ["ofS"])
                else:
                    P.tt("dve", og[:], B[7][:, 0:256], ofS[:, st, :], ALU.add, [BK[7], "ofS"], ["og"])
                    P.act(sq[:, 0:256], og[:], AF.Square, ["og"], ["sq"])
                    P.op("dve", lambda e: e.tensor_reduce(out=s8[:, 8:12], in_=sq[:, 0:256].rearrange("p (h d) -> p h d", h=4),
                                                           axis=AX.X, op=ALU.add), ["sq"], ["s8a"])
                    P.ts("dve", s8[:, 16:20], s8[:, 8:12], 1.0 / 64, 1e-6, ALU.mult, ALU.add, ["s8a"], ["s8b"])
                    P.act(s8[:, 24:28], s8[:, 16:20], AF.Ln, ["s8b"], ["s8c"])
                    P.act(s8[:, 0:4], s8[:, 24:28], AF.Exp, ["s8c"], ["s8"], scale=-0.5)
                    P.tt("dve", og[:].rearrange("p (h d) -> p h d", h=4), og[:].rearrange("p (h d) -> p h d", h=4),
                         s8[:, 0:4].unsqueeze(2).to_broadcast([128, 4, 64]), ALU.mult, ["og", "s8"], ["og"])
                    P.tt("pool", glaout[:, final_to, :], og[:], gnB, ALU.mult, ["og", "rB"], ["glaout"])
            P.mm(B[6][:, 0:256], kd_, vS[:, st, :], True, True, [kdk, "vS"], [BK[6]])
            if slot is None:
                P.tt("dve", KVm[:], B[6][:, 0:256], bd[:], ALU.mult, [BK[6], "bd"], ["KVm"])
                P.stt(Sd[:], Sd[:], dec[:, st, dr:dr + 1], KVm[:], ALU.mult, ALU.add, [sk, "dec", "KVm"], [sk])
            else:
                sg_ = sel[:, slot:slot + 1]
                P.stt(KVm[:], B[6][:, 0:256], sg_, bd[:], ALU.mult, ALU.mult, [BK[6], "bd", "sel"], ["KVm"])
                P.ts("dve", sel[:, 2:3], dec[:, st, dr:dr + 1], -1.0, sg_, ALU.add, ALU.mult, ["dec", "sel"], ["sel2"])
                P.ts("dve", sel[:, 3:4], sel[:, 2:3], 1.0, None, ALU.add, None, ["sel2"], ["sel3"])
                P.stt(Sd[:], Sd[:], sel[:, 3:4], KVm[:], ALU.mult, ALU.add, [sk, "sel3", "KVm"], [sk])
            P.cp("act", Sbd[:], Sd[:], [sk], [sbk])

        for i in range(2):
            load_h(dd["ctx"][i], 1, None)
            proj1()
            gla_prep(16 + i, True)
            kv_prep(32 + i, False)
            scan_step(0, 16 + i, need_ctx, None)
        for i in (1, 0):
            scan_step(1, 16 + i, need_ctx, (16 + i) if need_ctx else None)
        for t in range(16):
            load_h(dd["xown"][t], 0, (ropeO, t * 128))
            proj1()
            gla_prep(t, True)
            kv_prep(t, True)
            scan_step(0, t, True, None)
        groups = [[0, 1], [2, 3], [4, 5], [6, 7]]
        X = dd["xch"]

        def allgather(src, dst, key):
            P.coll(lambda e: e.collective_compute("AllGather", ALU.bypass, replica_groups=groups,
                                                   ins=[src], outs=[dst]), [key + "_i"], [key + "_o"])
        P.dma(X["sx_i"], S[0][:], reads=["S0"], writes=["sx_i"])
        allgather(X["sx_i"], X["sx_o"], "sx")
        Sx = C1.sb("Sx", [128, 2, 256])
        P.dma(Sx[:], X["sx_o"].rearrange("(s p) n -> p s n", p=128), reads=["sx_o"], writes=["Sx"])
        P.ts("dve", KVm[:], Sx[:, 0, :], sel[:, 0:1], None, ALU.mult, None, ["Sx", "sel"], ["KVm"])
        P.stt(S[1][:], Sx[:, 1, :], sel[:, 1:2], KVm[:], ALU.mult, ALU.add, ["Sx", "sel", "KVm"], ["S1"])
        P.cp("act", Sb[1][:], S[1][:], ["S1"], ["Sb1"])
        P.dma(X["kt_i"].rearrange("p (k t) -> p k t", k=2), KT[:, :, 0:2048], reads=["KT"], writes=["kt_i"])
        P.dma(X["ktd_i"].rearrange("p (k t) -> p k t", k=2), KTd[:, :, 0:2048], reads=["KTd"], writes=["ktd_i"])
        P.dma(X["vg_i"].rearrange("p (t k d) -> p t k d", t=16, k=2), Vg[:, 0:16, :, :], reads=["Vg"], writes=["vg_i"])
        for hh in range(2):
            P.dma(X["vd_i%d" % hh].rearrange("p (t k d) -> p t k d", t=8, k=4), Vd[:, hh * 8:(hh + 1) * 8, :, :],
                  reads=["Vd"], writes=["vd%d_i" % hh])
        allgather(X["kt_i"], X["kt_o"], "kt")
        allgather(X["ktd_i"], X["ktd_o"], "ktd")
        allgather(X["vg_i"], X["vg_o"], "vg")
        for hh in range(2):
            allgather(X["vd_i%d" % hh], X["vd_o%d" % hh], "vd%d" % hh)
        for sl in range(2):
            P.dma(KT[:, :, sl * 2048:(sl + 1) * 2048], X["kt_o"][sl * 64:(sl + 1) * 64, :].rearrange("p (k t) -> p k t", k=2),
                  reads=["kt_o"], writes=["KT"])
            P.dma(KTd[:, :, sl * 2048:(sl + 1) * 2048], X["ktd_o"][sl * 128:(sl + 1) * 128, :].rearrange("p (k t) -> p k t", k=2),
                  reads=["ktd_o"], writes=["KTd"])
            P.dma(Vg[:, sl * 16:(sl + 1) * 16, :, :], X["vg_o"][sl * 128:(sl + 1) * 128, :].rearrange("p (t k d) -> p t k d", t=16, k=2),
                  reads=["vg_o"], writes=["Vg"])
            for hh in range(2):
                P.dma(Vd[:, sl * 16 + hh * 8:sl * 16 + (hh + 1) * 8, :, :],
                      X["vd_o%d" % hh][sl * 128:(sl + 1) * 128, :].rearrange("p (t k d) -> p t k d", t=8, k=4),
                      reads=["vd%d_o" % hh], writes=["Vd"])
        for t in range(15, -1, -1):
            scan_step(1, t, True, t)
        P.barrier()

    with ExitStack() as es2:
        C2 = Ctx(nc, es2, C.pfx)
        wi2 = C2.sb("wi2", [128, 8, 1024], BF16)
        wo = C2.sb("wo", [128, 8, 1024], BF16)
        with ExitStack() as esw2:
            Cw2 = Ctx(nc, esw2, C.pfx)
            wst[0] = Cw2.sb("wst0b", [128, 8, 256])
            wst[1] = Cw2.sb("wst1b", [128, 8, 256])
            load_w(wi2, 0, win_d, 800, 512)
            load_w(wi2, 512, win_d, 1568, 256)
            load_w(wi2, 768, win_d, 512, 256)
            load_w(wo, 0, wout_d, 0, 1024)
            P.barrier()
        QT2 = [C2.sb("QT%d" % i, [64, 8, 128], BF16) for i in range(2)]
        QTm2 = [C2.sb("QTm%d" % i, [128, 8, 128], BF16) for i in range(2)]
        cat2 = [C2.sb("cat%d" % i, [128, D]) for i in range(2)]
        xt2 = [xt, C2.sb("xtb", [128, D])]
        hT2 = [hT, C2.sb("hTb", [128, 8, 128], BF16)]
        QTd = C2.sb("QTd", [128, 2, 128], BF16)
        PT = [C2.sb("PT%d" % i, [128, 1024], BF16) for i in range(2)]
        catT = C2.sb("catT", [128, 8, 128], BF16)
        rs = C2.sb("rs", [128, 256])
        rc = C2.sb("rc", [128, 16])
        od = C2.sb("od", [128, 256])
        t0 = C2.sb("t0", [128, 64])
        xo = C2.sb("xo", [128, D])

        def prep_chunks(tile, p):
            (src_ap, src_key), j, rope_row0, gidx, keys, dst = tile
            xt_, hT_, QT_, QTm_, cat_ = xt2[p], hT2[p], QT2[p], QTm2[p], cat2[p]
            kx, kh, kq, kqm, kc = "xt%d" % p, "hT%d" % p, "QT%d" % p, "QTm%d" % p, "cat%d" % p
            rope = rope_row0 is not None

            def c0():
                P.dma(xt_[:], src_ap, reads=[src_key], writes=[kx])
                if rope:
                    P.dma(cG[:, 0:32], ropeO[0][rope_row0:rope_row0 + 128, :], writes=["cG"])
                    P.dma(cG[:, 32:64], ropeO[1][rope_row0:rope_row0 + 128, :], writes=["cG"])
                    P.dma(cD[:, 0:16], ropeO[2][rope_row0:rope_row0 + 128, :], writes=["cD"])
                    P.dma(cD[:, 16:32], ropeO[3][rope_row0:rope_row0 + 128, :], writes=["cD"])

            def c1():
                emit_norm_mod(P, xt_[:], kx, modB[:, j, 1, :], modB[:, j, 0, :], MK, None, ss, tmp, hf[:], "hf", "n_")

            def c2():
                for half in range(2):
                    b = banks[half]
                    for c4 in range(4):
                        c = half * 4 + c4
                        P.tr(b[0][:, c4 * 128:(c4 + 1) * 128], hf[:, c * 128:(c + 1) * 128], ident[:], ["hf", "ident"], [b[1]])
                    P.cp("act", hT_[:, half * 4:(half + 1) * 4, :], b[0][:].rearrange("p (c t) -> p c t", c=4), [b[1]], [kh])

            def c3():
                for g in range(2):
                    for c in range(8):
                        P.mm(B[g][:], hT_[:, c, :], wi2[:, c, g * 512:(g + 1) * 512], c == 0, c == 7, [kh, "W"], [BK[g]])

            def c4():
                head_rstd(B[0][:], BK[0], 8, 64, s8[:, 0:8])
                P.tt("dve", fa[:].rearrange("p (h d) -> p h d", h=8), B[0][:].rearrange("p (h d) -> p h d", h=8),
                     s8[:, 0:8].unsqueeze(2).to_broadcast([128, 8, 64]), ALU.mult, [BK[0], "s8"], ["fa"])
                P.tt("pool", fb[:], fa[:], qnB, ALU.mult, ["fa", "rB"], ["fb"])
                if rope:
                    emit_rope(P, "pool", fa[:], fb[:], cG[:, 0:32], cG[:, 32:64], 8, 16, r1[:], r2[:], ["cG"], "fb", "fa")
                P.act(sq[:, 0:256], B[1][:, 0:256], AF.Copy, [BK[1]], ["sq"], scale=32.0 ** -0.5)
                if rope:
                    emit_rope(P, "pool", sq[:, 256:512], sq[:, 0:256], cD[:, 0:16], cD[:, 16:32], 8, 8,
                              r1[:, 0:128], r2[:, 0:128], ["cD"], "sq", "sqr")
                P.act(rs[:], B[1][:, 256:512], AF.Silu, [BK[1]], ["rs"])
                P.tt("dve", cat_[:, 0:256], glaout[:, gidx, :], rs[:], ALU.mult, ["glaout", "rs"], [kc])

            def c5():
                src, skey = (fa, "fa") if rope else (fb, "fb")
                for hh in range(2):
                    b = banks[hh]
                    for h4 in range(4):
                        h = hh * 4 + h4
                        P.tr(b[0][0:64, h4 * 128:(h4 + 1) * 128], src[:, h * 64:(h + 1) * 64], ident[:], [skey, "ident"], [b[1]])
                    P.cp("act", QT_[:, hh * 4:(hh + 1) * 4, :], b[0][0:64, :].rearrange("p (h t) -> p h t", h=4), [b[1]], [kq])
                src2, k2 = (sq[:, 256:512], "sqr") if rope else (sq[:, 0:256], "sq")
                for g in range(2):
                    P.tr(B[0][:, g * 128:(g + 1) * 128], src2[:, g * 128:(g + 1) * 128], ident[:], [k2, "ident"], [BK[0]])
                P.cp("act", QTd[:], B[0][:, 0:256].rearrange("p (g t) -> p g t", g=2), [BK[0]], ["QTd"])
                for m in range(8):
                    P.ts("pool", QTm_[:, m, :], QTd[:, m // 4, :], bd[:, 64 * (m % 4):64 * (m % 4) + 1], None, ALU.mult, None,
                         ["QTd", "bd"], [kqm])
            return [c0, c1, c2, c3, c4, c5]

        SCHED = {1: 0, 6: 1, 15: 2, 21: 3, 23: 4, 46: 5}

        def attention(tile, p, nxt):
            (src_ap, src_key), j, rope_row0, gidx, keys, dst = tile
            QT_, QTm_, cat_ = QT2[p], QTm2[p], cat2[p]
            kq, kqm, kc = "QT%d" % p, "QTm%d" % p, "cat%d" % p
            done = [0]

            def gqa_post(kv):
                ob = banks[6 + kv]
                for g in range(4):
                    h = kv * 4 + g
                    P.op("dve", lambda e, g=g, ob=ob, h=h: e.reciprocal(rc[:, h:h + 1], ob[0][:, g * 65 + 64:g * 65 + 65]),
                         [ob[1]], ["rc"])
                    P.ts("dve", cat_[:, 256 + h * 64:256 + (h + 1) * 64], ob[0][:, g * 65:g * 65 + 64], rc[:, h:h + 1], None,
                         ALU.mult, None, [ob[1], "rc"], [kc])

            def diff_post(gg):
                ob = banks[6 + gg]
                for hh in range(2):
                    h = gg * 2 + hh
                    m0, m1 = 2 * hh, 2 * hh + 1
                    P.op("dve", lambda e, ob=ob, m0=m0: e.reciprocal(rc[:, 8:9], ob[0][:, m0 * 65 + 64:m0 * 65 + 65]),
                         [ob[1]], ["rc"])
                    P.op("dve", lambda e, ob=ob, m1=m1: e.reciprocal(rc[:, 9:10], ob[0][:, m1 * 65 + 64:m1 * 65 + 65]),
                         [ob[1]], ["rc"])
                    P.tt("dve", rc[:, 10:11], rc[:, 9:10], nlam, ALU.mult, ["rc", "nlam"], ["rc"])
                    P.ts("dve", t0[:], ob[0][:, m0 * 65:m0 * 65 + 64], rc[:, 8:9], None, ALU.mult, None, [ob[1], "rc"], ["t0"])
                    P.stt(od[:, h * 64:(h + 1) * 64], ob[0][:, m1 * 65:m1 * 65 + 64], rc[:, 10:11], t0[:], ALU.mult, ALU.add,
                          [ob[1], "rc", "t0"], ["od"])

            npair = len(keys) // 2
            items = [(typ, grp, pi_, (keys[2 * pi_], keys[2 * pi_ + 1]))
                     for typ in range(2) for grp in range(2) for pi_ in range(npair)]

            def emit_score(ix):
                typ, grp, pi_, pr = items[ix]
                for hx in range(2):
                    sbk = banks[2 + 2 * (ix % 2) + hx]
                    s = pr[hx]
                    if typ == 0:
                        P.mm(sbk[0][:], KT[:, grp, s * 128:(s + 1) * 128], QT_[:, grp * 4:(grp + 1) * 4, :], True, True,
                             ["KT", kq], [sbk[1]])
                    else:
                        P.mm(sbk[0][:], KTd[:, grp, s * 128:(s + 1) * 128], QTm_[:, grp * 4:(grp + 1) * 4, :], True, True,
                             ["KTd", kqm], [sbk[1]])

            def emit_rest(ix):
                typ, grp, pi_, pr = items[ix]
                k0, k1 = "bank%d" % (2 + 2 * (ix % 2)), "bank%d" % (3 + 2 * (ix % 2))
                pt = PT[ix % 2]
                ptk = "PT%d" % (ix % 2)
                ob = banks[6 + grp]
                P.act(pt[:], PS2[1 + ix % 2][:], AF.Exp, [k0, k1], [ptk])
                for hx in range(2):
                    s = pr[hx]
                    for g in range(4):
                        if typ == 0:
                            rhs, vk = Vg[:, s, grp, :], "Vg"
                        else:
                            rhs, vk = Vd[:, s, (grp * 4 + g) // 2, :], "Vd"
                        P.mm(ob[0][:, g * 65:(g + 1) * 65], pt[:, hx * 512 + g * 128:hx * 512 + (g + 1) * 128], rhs,
                             pi_ == 0 and hx == 0 and g == 0, pi_ == npair - 1 and hx == 1, [ptk, vk], [ob[1]], skip=True)
                if pi_ == npair - 1:
                    if typ == 0:
                        gqa_post(grp)
                    else:
                        diff_post(grp)

            emit_score(0)
            for ix in range(len(items)):
                if ix + 1 < len(items):
                    emit_score(ix + 1)
                emit_rest(ix)
                if ix in SCHED and SCHED[ix] == done[0] and done[0] < len(nxt):
                    nxt[done[0]]()
                    done[0] += 1
            while done[0] < len(nxt):
                nxt[done[0]]()
                done[0] += 1

        def post(tile, p):
            (src_ap, src_key), j, rope_row0, gidx, keys, dst = tile
            xt_, cat_ = xt2[p], cat2[p]
            kx, kc = "xt%d" % p, "cat%d" % p
            head_rstd(od[:], "od", 4, 64, s8[:, 0:4])
            P.tt("dve", od[:].rearrange("p (h d) -> p h d", h=4), od[:].rearrange("p (h d) -> p h d", h=4),
                 s8[:, 0:4].unsqueeze(2).to_broadcast([128, 4, 64]), ALU.mult, ["od", "s8"], ["od"])
            P.tt("pool", cat_[:, 768:1024], od[:], dnB, ALU.mult, ["od", "rB"], [kc])
            for half in range(2):
                b = banks[half]
                for c4 in range(4):
                    c = half * 4 + c4
                    P.tr(b[0][:, c4 * 128:(c4 + 1) * 128], cat_[:, c * 128:(c + 1) * 128], ident[:], [kc, "ident"], [b[1]])
                P.cp("act", catT[:, half * 4:(half + 1) * 4, :], b[0][:].rearrange("p (c t) -> p c t", c=4), [b[1]], ["catT"])
            for h2 in range(2):
                b = banks[h2]
                for c in range(8):
                    P.mm(b[0][:], catT[:, c, :], wo[:, c, h2 * 512:(h2 + 1) * 512], c == 0, c == 7, ["catT", "W"], [b[1]])
                cs = slice(h2 * 512, (h2 + 1) * 512)
                P.tt("dve", tmp[:, cs], b[0][:], modB[:, j, 2, cs], ALU.mult, [b[1]] + MK, ["n_tmp"])
                P.tt("pool", xo[:, cs], xt_[:, cs], tmp[:, cs], ALU.add, ["n_tmp", kx], ["xo"])
            P.dma(dst[0], xo[:], reads=["xo"], writes=[dst[1]])

        tiles = [(dd["xown"][t], 0, t * 128, t, list(range(NKT)), dd["yo"][t]) for t in range(16)]
        if need_ctx:
            tiles += [(dd["ctx"][i], 1, None, 16 + i, [32, 33], dd["yc"][i]) for i in range(2)]
        for ch in prep_chunks(tiles[0], 0):
            ch()
        for i, tile in enumerate(tiles):
            nxt = prep_chunks(tiles[i + 1], (i + 1) % 2) if i + 1 < len(tiles) else []
            attention(tile, i % 2, nxt)
            post(tile, i % 2)
        P.barrier()


LAM_INIT = [0.8 - 0.6 * math.exp(-0.3 * l) for l in range(2)]


def build_fused(stages=("m0", "f0", "cc", "m1", "f1")):
    nc = bass.Bass("TRN2", target_bir_lowering=False)
    es = ExitStack()
    C = Ctx(nc, es)
    P = Prog(nc, es)
    di = {}

    def din(name, shape):
        di[name] = C.din(name, shape)
        return di[name]

    din("xown", [2048, D]); din("ctx", [256, D]); din("ccT", [128, 16]); din("sel", [128, 2])
    ropeO = [din("cosGo", [2048, 32]), din("sinGo", [2048, 32]), din("cosDo", [2048, 16]), din("sinDo", [2048, 16])]
    for l in range(2):
        din("w_mod%d" % l, [D, 6 * D]); din("b_mod%d" % l, [1, 6 * D])
        din("norm_mix%d" % l, [1, D]); din("norm_ffn%d" % l, [1, D])
        din("w_in%d" % l, [D, 2336]); din("w_out%d" % l, [D, D])
        din("w4Tf%d" % l, [16, D]); din("w4Tb%d" % l, [16, D]); din("a2f%d" % l, [16, 128]); din("a2b%d" % l, [16, 128])
        din("rows%d" % l, [1, 2048])
    din("norm_f", [1, D])
    din("wg0", [1, D, 2816]); din("wu0", [1, D, 2816]); din("wd0", [1, 2816, D])
    din("wg1", [8, D, 3584]); din("wu1", [8, D, 3584]); din("wd1", [8, 3584, D]); din("wrT", [8, D])
    y_d = C.dout("y", [2048, D])
    xmid0 = nc.dram_tensor("xmid0", [2304, D], F32, kind="Internal").ap()
    x1own = nc.dram_tensor("x1own", [2048, D], F32, kind="Internal").ap()

    def xch(l):
        def t(name, shape, dt):
            return nc.dram_tensor("x%d_%s" % (l, name), shape, dt, kind="Internal").ap()
        return {"sx_i": t("sx_i", [128, 256], F32), "sx_o": t("sx_o", [256, 256], F32),
                "kt_i": t("kt_i", [64, 4096], BF16), "kt_o": t("kt_o", [128, 4096], BF16),
                "ktd_i": t("ktd_i", [128, 4096], BF16), "ktd_o": t("ktd_o", [256, 4096], BF16),
                "vg_i": t("vg_i", [128, 2080], BF16), "vg_o": t("vg_o", [256, 2080], BF16),
                "vd_i0": t("vd_i0", [128, 2080], BF16), "vd_o0": t("vd_o0", [256, 2080], BF16),
                "vd_i1": t("vd_i1", [128, 2080], BF16), "vd_o1": t("vd_o1", [256, 2080], BF16)}
    xc1 = nc.dram_tensor("xc1", [256, D], F32, kind="Internal").ap()
    xmid1 = nc.dram_tensor("xmid1", [2048, D], F32, kind="Internal").ap()
    PS2 = [C.ps("psd%d" % i, [128, 1024]) for i in range(4)]
    banks = [(PS2[i // 2][:, (i % 2) * 512:(i % 2 + 1) * 512], "bank%d" % i) for i in range(8)]
    banks[0] = banks[0] + (PS2,)

    def tl(ap, key, n, off=0):
        return [(ap[off + t * 128:off + (t + 1) * 128, :], key) for t in range(n)]

    def mixer(l, xown_t, ctx_t, yo_t, yc_t):
        with ExitStack() as st:
            Cs = Ctx(nc, st, "M%d_" % l)
            dd = {"ccT": di["ccT"], "sel": di["sel"], "ropeO": ropeO, "xch": xch(l),
                  "xown": xown_t, "ctx": ctx_t, "yo": yo_t, "yc": yc_t}
            for k in ("w_mod", "b_mod", "norm_mix", "w_in", "w_out", "w4Tf", "w4Tb", "a2f", "a2b", "rows"):
                dd[k] = di["%s%d" % (k, l)]
            emit_mixer(P, nc, Cs, banks, l == 0, LAM_INIT[l], dd)

    def ffn(l, tiles):
        with ExitStack() as st:
            Cs = Ctx(nc, st, "F%d_" % l)
            dd = {"ccT": di["ccT"], "w_mod": di["w_mod%d" % l], "b_mod": di["b_mod%d" % l],
                  "norm_ffn": di["norm_ffn%d" % l], "norm_f": di["norm_f"],
                  "wg": di["wg%d" % l], "wu": di["wu%d" % l], "wd": di["wd%d" % l], "wrT": di["wrT"]}
            emit_ffn(P, nc, Cs, banks, "dense" if l == 0 else "moe", tiles, dd, l == 1)

    if "m0" in stages:
      mixer(0, tl(di["xown"], "in", 16), tl(di["ctx"], "in", 2),
          tl(y_d if stages == ("m0",) else xmid0, "xmid0", 16), tl(xmid0, "xmid0", 2, 2048))
    t0 = [(xmid0[t * 128:(t + 1) * 128, :], "xmid0", x1own[t * 128:(t + 1) * 128, :], "x1own", 0) for t in range(16)]
    t0 += [(xmid0[2048 + i * 128:2048 + (i + 1) * 128, :], "xmid0", xc1[i * 128:(i + 1) * 128, :], "xc1", 1) for i in range(2)]
    if "f0" in stages:
        ffn(0, t0)
    if "m1" in stages:
      mixer(1, tl(x1own, "x1own", 16), tl(xc1, "xc1", 2), tl(xmid1, "xmid1", 16), [])
    t1 = [(xmid1[t * 128:(t + 1) * 128, :], "xmid1", y_d[t * 128:(t + 1) * 128, :], "yout", 0) for t in range(16)]
    if "f1" in stages:
        ffn(1, t1)
    P.emit()
    es.close()
    return nc


def _ccT(cb, cc):
    return np.ascontiguousarray(np.concatenate([cb.reshape(8, 128).T, cc.reshape(8, 128).T], axis=1), dtype=np.float32)


def _rope_tables():
    t = np.arange(4096)
    rows = (t // 64).astype(np.float64)
    cols = (t % 64).astype(np.float64)

    def tab(half):
        fr = 10000.0 ** (-np.arange(half, dtype=np.float64) / half)
        ang = np.concatenate([rows[:, None] * fr[None, :], cols[:, None] * fr[None, :]], axis=1)
        return np.cos(ang).astype(np.float32), np.sin(ang).astype(np.float32)
    cG, sG = tab(16)
    cD, sD = tab(8)
    return cG, sG, cD, sD


def core_inputs(inp, b, half, ropes):
    f = lambda a: np.ascontiguousarray(a, dtype=np.float32)
    cG, sG, cD, sD = ropes
    xb = inp["x"][b]
    if half == 0:
        oorder = np.arange(2048)
        ctxl = inp["ctx"][b]
        sel = np.array([0.0, 1.0], np.float32)
    else:
        oorder = np.arange(4095, 2047, -1)
        ctxl = inp["ctx"][b][::-1]
        sel = np.array([1.0, 0.0], np.float32)
    m = {"xown": f(xb[oorder]), "ctx": f(ctxl), "ccT": _ccT(inp["c"][b], inp["c_ctx"]),
         "sel": f(np.tile(sel[None, :], (128, 1))),
         "cosGo": f(cG[oorder]), "sinGo": f(sG[oorder]), "cosDo": f(cD[oorder]), "sinDo": f(sD[oorder]),
         "norm_f": f(inp["norm_f"][None, :]),
         "wg0": inp["ffn_gate"], "wu0": inp["ffn_up"], "wd0": inp["ffn_down"],
         "wg1": inp["moe_gate"][0], "wu1": inp["moe_up"][0], "wd1": inp["moe_down"][0],
         "wrT": f(inp["moe_router"][0].T)}
    for l in range(2):
        w_in = inp["w_in"][l]
        if half == 0:
            w4f, w4b = w_in[:, 768:784], w_in[:, 784:800]
            a2f, a2b = inp["gla_a2_f"][l], inp["gla_a2_b"][l]
            abf, abb = inp["gla_ab_f"][l], inp["gla_ab_b"][l]
        else:
            w4f, w4b = w_in[:, 784:800], w_in[:, 768:784]
            a2f, a2b = inp["gla_a2_b"][l], inp["gla_a2_f"][l]
            abf, abb = inp["gla_ab_b"][l], inp["gla_ab_f"][l]
        rows = np.concatenate([abf, abb, np.tile(inp["gla_norm"][l], 4), np.tile(inp["gqa_q_norm"][l], 8),
                               np.tile(inp["gqa_k_norm"][l], 2), np.tile(inp["diff_norm"][l], 4),
                               inp["diff_lam_q1"][l], inp["diff_lam_k1"][l], inp["diff_lam_q2"][l], inp["diff_lam_k2"][l],
                               np.zeros(512, np.float32)]).astype(np.float32)[None, :]
        m.update({"w_mod%d" % l: inp["w_mod"][l], "b_mod%d" % l: f(inp["b_mod"][l][None, :]),
                  "norm_mix%d" % l: f(inp["norm_mix"][l][None, :]), "norm_ffn%d" % l: f(inp["norm_ffn"][l][None, :]),
                  "w_in%d" % l: w_in, "w_out%d" % l: inp["w_out"][l], "w4Tf%d" % l: f(w4f.T), "w4Tb%d" % l: f(w4b.T),
                  "a2f%d" % l: f(a2f), "a2b%d" % l: f(a2b), "rows%d" % l: f(rows)})
    return m


def kernel(**inp):
    inp = {k: np.asarray(v) for k, v in inp.items()}
    ropes = _rope_tables()
    nc = build_fused()
    maps = [core_inputs(inp, c // 2, c % 2, ropes) for c in range(8)]
    res = run_bass_kernel_spmd(nc, maps, core_ids=list(range(8))).results
    Bn = inp["x"].shape[0]
    out = np.empty((Bn, 4096, D), np.float32)
    for c in range(8):
        b, half = c // 2, c % 2
        y = res[c]["y"]
        if half == 0:
            out[b, 0:2048] = y
        else:
            out[b, 2048:4096] = y[::-1]
    return out
```
